# Optimizing a Trainium2 kernel written in Bass

```python
import math
import jax
import jax.numpy as jnp
from jax import lax
import numpy as np

D_MODEL = 1024
BATCH = 1
SEQ = 16384
DEPTH = 1

D_MIX = D_MODEL
GM_HEADS = 8
GM_WIDTH = D_MIX // 2
GM_HEAD_DIM = GM_WIDTH // GM_HEADS
GM_CHUNK = 128
DN_HEADS = 4
DN_WIDTH = D_MIX - GM_WIDTH
DN_HEAD_DIM = DN_WIDTH // DN_HEADS
DN_CHUNK = 64
CONV_K = 4
COL_GM = 0
COL_Q = COL_GM + 2 * GM_WIDTH
COL_K = COL_Q + DN_WIDTH
COL_V = COL_K + DN_WIDTH
COL_Z = COL_V + DN_WIDTH
COL_A = COL_Z + DN_WIDTH
COL_B = COL_A + DN_HEADS
D_IN = COL_B + DN_HEADS
PEER_HEADS = 8
N_KEYS = 128
N_EXPERTS = N_KEYS * N_KEYS
PEER_QUERY_DIM = 256
PEER_HALF = PEER_QUERY_DIM // 2
PEER_TOPK = 16
PEER_BLOCK = 128
EPS = 1e-6

kernel_name = 'hybrid_gmlp_gdn_peer_adaln'


def rms_norm(x, w):
    xf = x.astype(jnp.float32)
    y = xf * lax.rsqrt(jnp.mean(xf * xf, axis=-1, keepdims=True) + EPS)
    return (y * w.astype(jnp.float32)).astype(x.dtype)


def l2_norm(x):
    return x * lax.rsqrt(jnp.sum(x * x, axis=-1, keepdims=True) + EPS)


def modulate(h, shift, scale):
    return h * (1 + scale[:, None, :]) + shift[:, None, :]


def spatial_gating(u, v, norm_w, ws, bs):
    B, S, _ = u.shape
    v = rms_norm(v, norm_w).reshape(B, S // GM_CHUNK, GM_CHUNK, GM_HEADS, GM_HEAD_DIM)
    causal = jnp.tril(jnp.ones((GM_CHUNK, GM_CHUNK), dtype=bool))
    w = jnp.where(causal, ws, 0.0)
    s = jnp.einsum('hts,bnshd->bnthd', w, v) + bs.T[:, :, None]
    return u * s.reshape(B, S, GM_WIDTH)


def short_conv(x, w):
    return lax.conv_general_dilated(
        x, w[:, None, :], window_strides=(1,), padding=[(CONV_K - 1, 0)],
        dimension_numbers=('NWC', 'WIO', 'NWC'), feature_group_count=x.shape[-1])


def gated_delta_rule(q, k, v, g, beta):
    B, S, H, Dk = q.shape
    Dv = v.shape[-1]
    C = DN_CHUNK
    N = S // C

    def chunked(t):
        return t.reshape(B, N, C, H, -1).transpose(0, 3, 1, 2, 4)

    q = chunked(q) * (Dk ** -0.5)
    k = chunked(k)
    v = chunked(v)
    g = g.reshape(B, N, C, H).transpose(0, 3, 1, 2)
    beta = beta.reshape(B, N, C, H).transpose(0, 3, 1, 2)
    gc = jnp.cumsum(g, axis=-1)
    incl = jnp.tril(jnp.ones((C, C), dtype=bool))
    strict = jnp.tril(jnp.ones((C, C), dtype=bool), -1)
    decay = jnp.exp(jnp.where(incl, gc[..., :, None] - gc[..., None, :], -jnp.inf))
    kb = k * beta[..., None]
    kk = jnp.einsum('bhnid,bhnjd->bhnij', kb, k) * decay
    a_mat = jnp.eye(C, dtype=jnp.float32) + jnp.where(strict, kk, 0.0)
    rhs = jnp.concatenate([v * beta[..., None], kb * jnp.exp(gc)[..., None]], axis=-1)
    sol = lax.linalg.triangular_solve(a_mat, rhs, left_side=True, lower=True, unit_diagonal=True)
    u_val, w = sol[..., :Dv], sol[..., Dv:]
    attn = jnp.einsum('bhnid,bhnjd->bhnij', q, k) * decay
    q_dec = q * jnp.exp(gc)[..., None]
    g_last = gc[..., -1]
    k_dec = k * jnp.exp(g_last[..., None] - gc)[..., None]

    def step(state, xs):
        q_c, a_c, w_c, u_c, k_c, gl = xs
        v_new = u_c - jnp.einsum('bhck,bhkv->bhcv', w_c, state)
        o = jnp.einsum('bhck,bhkv->bhcv', q_c, state) + jnp.einsum('bhij,bhjv->bhiv', a_c, v_new)
        state = state * jnp.exp(gl)[..., None, None] + jnp.einsum('bhck,bhcv->bhkv', k_c, v_new)
        return state, o

    xs = tuple(jnp.moveaxis(t, 2, 0) for t in (q_dec, attn, w, u_val, k_dec, g_last))
    state0 = jnp.zeros((B, H, Dk, Dv), jnp.float32)
    _, o = lax.scan(step, state0, xs)
    return o.transpose(1, 0, 3, 2, 4).reshape(B, S, H, Dv)


def hybrid_mixer(p, gm_norm_w, gm_ws, gm_bs, conv_w, a_log, dt_bias, o_norm_w):
    B, S, _ = p.shape
    uv = jax.nn.gelu(p[..., COL_GM:COL_Q])
    y_a = spatial_gating(uv[..., :GM_WIDTH], uv[..., GM_WIDTH:], gm_norm_w, gm_ws, gm_bs)
    qkv = jax.nn.silu(short_conv(p[..., COL_Q:COL_Z], conv_w)).astype(jnp.float32)
    q = l2_norm(qkv[..., :DN_WIDTH].reshape(B, S, DN_HEADS, DN_HEAD_DIM))
    k = l2_norm(qkv[..., DN_WIDTH:2 * DN_WIDTH].reshape(B, S, DN_HEADS, DN_HEAD_DIM))
    v = qkv[..., 2 * DN_WIDTH:].reshape(B, S, DN_HEADS, DN_HEAD_DIM)
    z = p[..., COL_Z:COL_A].astype(jnp.float32).reshape(B, S, DN_HEADS, DN_HEAD_DIM)
    a = p[..., COL_A:COL_B].astype(jnp.float32)
    b = p[..., COL_B:D_IN].astype(jnp.float32)
    g = -jnp.exp(a_log.astype(jnp.float32)) * jax.nn.softplus(a + dt_bias.astype(jnp.float32))
    beta = jax.nn.sigmoid(b)
    o = gated_delta_rule(q, k, v, g, beta)
    o = rms_norm(o, o_norm_w) * jax.nn.silu(z)
    y_b = o.reshape(B, S, DN_WIDTH).astype(p.dtype)
    return jnp.concatenate([y_a, y_b], axis=-1)


def peer_routing(h, wq, keys1, keys2):
    T = h.shape[0]
    q = (h @ wq).astype(jnp.float32).reshape(T, PEER_HEADS, 2, PEER_HALF)
    s1 = jnp.einsum('thd,hkd->thk', q[:, :, 0], keys1.astype(jnp.float32))
    s2 = jnp.einsum('thd,hkd->thk', q[:, :, 1], keys2.astype(jnp.float32))
    v1, i1 = lax.top_k(s1, PEER_TOPK)
    v2, i2 = lax.top_k(s2, PEER_TOPK)
    cand = (v1[..., :, None] + v2[..., None, :]).reshape(T, PEER_HEADS, PEER_TOPK * PEER_TOPK)
    cand_id = (i1[..., :, None] * N_KEYS + i2[..., None, :]).reshape(T, PEER_HEADS, PEER_TOPK * PEER_TOPK)
    score, pos = lax.top_k(cand, PEER_TOPK)
    ids = jnp.take_along_axis(cand_id, pos, axis=-1)
    gates = jax.nn.softmax(score, axis=-1)
    return ids, gates


def peer_apply(h, ids, gates, expert_u, expert_v):
    T, D = h.shape
    nb = T // PEER_BLOCK

    def block(args):
        hb, ib, gb = args
        act = jax.nn.gelu(jnp.einsum('pd,phkd->phk', hb, expert_u[ib]))
        return jnp.einsum('phk,phkd->pd', gb.astype(hb.dtype) * act, expert_v[ib])

    out = lax.map(block, (h.reshape(nb, PEER_BLOCK, D),
                          ids.reshape(nb, PEER_BLOCK, PEER_HEADS, PEER_TOPK),
                          gates.reshape(nb, PEER_BLOCK, PEER_HEADS, PEER_TOPK)))
    return out.reshape(T, D)


def setup_inputs(seed: int = 0) -> dict:
    key = jax.random.key(seed)
    ks = jax.random.split(key, 21)
    f32 = jnp.float32
    L, D = DEPTH, D_MODEL
    dt = jnp.exp(jax.random.uniform(ks[11], (L, DN_HEADS), f32, math.log(1e-3), math.log(1e-1)))
    return {
        'x': jax.random.normal(ks[0], (BATCH, SEQ, D), f32),
        'c': jax.random.normal(ks[1], (BATCH, D), f32),
        'w_ada': jax.random.normal(ks[2], (L, D, 6 * D), f32) * (0.5 * D ** -0.5),
        'b_ada': jax.random.normal(ks[3], (L, 6 * D), f32) * 0.01,
        'norm1_w': 1.0 + 0.01 * jax.random.normal(ks[4], (L, D), f32),
        'w_in': jax.random.normal(ks[5], (L, D, D_IN), f32) * D ** -0.5,
        'gm_norm_w': 1.0 + 0.01 * jax.random.normal(ks[6], (L, GM_WIDTH), f32),
        'gm_ws': jax.random.normal(ks[7], (L, GM_HEADS, GM_CHUNK, GM_CHUNK), f32) * GM_CHUNK ** -0.5,
        'gm_bs': 1.0 + 0.01 * jax.random.normal(ks[8], (L, GM_HEADS, GM_CHUNK), f32),
        'conv_w': jax.random.normal(ks[9], (L, CONV_K, 3 * DN_WIDTH), f32) * CONV_K ** -0.5,
        'a_log': jnp.log(jax.random.uniform(ks[10], (L, DN_HEADS), f32, 1.0, 16.0)),
        'dt_bias': dt + jnp.log(-jnp.expm1(-dt)),
        'o_norm_w': 1.0 + 0.01 * jax.random.normal(ks[12], (L, DN_HEAD_DIM), f32),
        'w_out': jax.random.normal(ks[13], (L, D_MIX, D), f32) * D_MIX ** -0.5,
        'norm2_w': 1.0 + 0.01 * jax.random.normal(ks[14], (L, D), f32),
        'peer_wq': jax.random.normal(ks[15], (L, D, PEER_HEADS * PEER_QUERY_DIM), f32) * D ** -0.5,
        'peer_keys1': jax.random.normal(ks[16], (L, PEER_HEADS, N_KEYS, PEER_HALF), f32) * PEER_HALF ** -0.5,
        'peer_keys2': jax.random.normal(ks[17], (L, PEER_HEADS, N_KEYS, PEER_HALF), f32) * PEER_HALF ** -0.5,
        'peer_u': jax.random.normal(ks[18], (L, N_EXPERTS, D), f32) * D ** -0.5,
        'peer_v': jax.random.normal(ks[19], (L, N_EXPERTS, D), f32) * PEER_HEADS ** -0.5,
        'final_norm_w': 1.0 + 0.01 * jax.random.normal(ks[20], (D,), f32),
    }


def reference(x, c, w_ada, b_ada, norm1_w, w_in, gm_norm_w, gm_ws, gm_bs, conv_w, a_log, dt_bias,
              o_norm_w, w_out, norm2_w, peer_wq, peer_keys1, peer_keys2, peer_u, peer_v, final_norm_w):
    B, S, D = x.shape
    for l in range(DEPTH):
        mod = jax.nn.silu(c) @ w_ada[l] + b_ada[l]
        sh1, sc1, gt1, sh2, sc2, gt2 = jnp.split(mod, 6, axis=-1)
        h = modulate(rms_norm(x, norm1_w[l]), sh1, sc1)
        p = h @ w_in[l]
        mix = hybrid_mixer(p, gm_norm_w[l], gm_ws[l], gm_bs[l], conv_w[l], a_log[l], dt_bias[l], o_norm_w[l])
        x = x + gt1[:, None, :] * (mix @ w_out[l])
        h2 = modulate(rms_norm(x, norm2_w[l]), sh2, sc2).reshape(B * S, D)
        ids, gates = peer_routing(h2, peer_wq[l], peer_keys1[l], peer_keys2[l])
        ff = peer_apply(h2, ids, gates, peer_u[l], peer_v[l]).reshape(B, S, D)
        x = x + gt2[:, None, :] * ff
    return rms_norm(x, final_norm_w)
```

```python
import numpy as np
from contextlib import ExitStack
import concourse.bass as bass
import concourse.mybir as mybir
from concourse.bass_utils import run_bass_kernel_spmd

F32 = mybir.dt.float32
BF16 = mybir.dt.bfloat16
ALU = mybir.AluOpType
AF = mybir.ActivationFunctionType

ENGS = ["sync", "scalar", "vector", "gpsimd", "tensor"]
SEM_CHUNK = 4000
EPS = 1e-6


class Buf:
    __slots__ = ("name", "w", "r", "dsem", "dcnt", "excl")

    def __init__(self, name, excl=False):
        self.excl = excl
        self.name = name
        self.w = None
        self.r = {}
        self.dsem = None
        self.dcnt = 0


class Op:
    __slots__ = ("eng", "fn", "deps", "dma", "sig", "need", "dbuf", "dval", "inc")

    def __init__(self, eng, fn, dma):
        self.eng = eng
        self.fn = fn
        self.deps = []
        self.dma = dma
        self.sig = None
        self.need = False
        self.dbuf = None
        self.dval = 0
        self.inc = 16


class Prog:
    def __init__(self, nc):
        self.nc = nc
        self.q = {e: [] for e in ENGS}
        self.dma_bufs = []
        self.last = {e: None for e in ENGS}

    def op(self, eng, fn, r=(), w=(), dma_buf=None, extra=()):
        deps = list(extra)
        for b in r:
            if b.w is not None:
                deps.append(b.w)
            if b.excl:
                for o in b.r.values():
                    if o.eng != eng:
                        deps.append(o)
        for b in w:
            if b.w is not None and not (dma_buf is not None and b.w.dma and b.w.dbuf is dma_buf):
                deps.append(b.w)
            for o in b.r.values():
                deps.append(o)
        dma = dma_buf is not None
        rec = Op(eng, fn, dma)
        seen = set()
        for d in deps:
            if id(d) in seen:
                continue
            seen.add(id(d))
            if (not d.dma) and d.eng == eng:
                if eng == "tensor":
                    continue
                if not any(b.w is d for b in r) and d not in extra:
                    continue
            d.need = True
            rec.deps.append(d)
        if dma:
            if dma_buf.dsem is None:
                dma_buf.dsem = len(self.dma_bufs)
                self.dma_bufs.append(dma_buf)
            dma_buf.dcnt += 16
            rec.dbuf = dma_buf
            rec.dval = dma_buf.dcnt
        self.q[eng].append(rec)
        if not dma:
            self.last[eng] = rec
        for b in r:
            b.r[eng if not dma else ("dma", id(rec))] = rec
        for b in w:
            b.w = rec
            b.r = {}
        return rec

    def barrier(self, bufs):
        deps = [o for o in self.last.values() if o is not None]
        for b in bufs:
            if b.w is not None:
                deps.append(b.w)
            deps.extend(b.r.values())
        for e in ENGS:
            self.op(e, lambda en: en.nop(), extra=[d for d in deps if not (d.eng == e and not d.dma)])

    def emit(self):
        nc = self.nc
        nsig = {}
        for e in ENGS:
            c = 0
            for rec in self.q[e]:
                if rec.dma:
                    continue
                if rec.need:
                    c += 1
                    rec.sig = c
            nsig[e] = c
        with ExitStack() as es:
            esems = {}
            for e in ENGS:
                n = (nsig[e] + SEM_CHUNK - 1) // SEM_CHUNK
                esems[e] = [es.enter_context(nc.semaphore(f"s_{e}_{i}")) for i in range(n)]
            dsems = [es.enter_context(nc.semaphore(f"d_{i}")) for i in range(len(self.dma_bufs))]
            block = es.enter_context(nc.Block())
            prog = self

            def make(e):
                def body(eng):
                    known = {}
                    maxchunk = {}
                    for rec in prog.q[e]:
                        for d in rec.deps:
                            if d.dma:
                                key = ("d", d.dbuf.dsem)
                                val = d.dval
                                sem = dsems[d.dbuf.dsem]
                            else:
                                ci = (d.sig - 1) // SEM_CHUNK
                                if maxchunk.get(d.eng, -1) > ci:
                                    continue
                                key = (d.eng, ci)
                                val = d.sig - ci * SEM_CHUNK
                                sem = esems[d.eng][ci]
                            if known.get(key, 0) >= val:
                                continue
                            eng.wait_ge(sem, val)
                            known[key] = val
                            if not d.dma:
                                maxchunk[d.eng] = max(maxchunk.get(d.eng, -1), ci)
                        ins = rec.fn(eng)
                        if rec.dma:
                            ins.then_inc(dsems[rec.dbuf.dsem], 16)
                        elif rec.need:
                            ci = (rec.sig - 1) // SEM_CHUNK
                            ins.then_inc(esems[e][ci], 1)
                return body

            block.sync(make("sync"))
            block.scalar(make("scalar"))
            block.vector(make("vector"))
            block.gpsimd(make("gpsimd"))
            block.tensor(make("tensor"))


def run_interleaved(gens):
    active = list(gens)
    while active:
        for g in list(active):
            try:
                next(g)
            except StopIteration:
                active.remove(g)


C_ID, C_TRIU, C_BLK, C_NEGU, C_SU, C_CAUS, C_SEL, C_ONES, C_N = 0, 128, 256, 384, 512, 640, 768, 1280, 1408


def make_consts():
    c = np.zeros((128, C_N), np.float32)
    idx = np.arange(128)
    j = idx[:, None]
    i = idx[None, :]
    same = (j // 64) == (i // 64)
    c[:, C_ID:C_ID + 128] = np.eye(128)
    c[:, C_TRIU:C_TRIU + 128] = ((i >= j) & same)
    c[:, C_BLK:C_BLK + 128] = same
    c[:, C_NEGU:C_NEGU + 128] = np.where((i >= j) & same, 0.0, -30000.0)
    c[:, C_SU:C_SU + 128] = ((i > j) & same)
    c[:, C_CAUS:C_CAUS + 128] = (j <= i)
    for h in range(4):
        c[32 + h, C_SEL + h * 128:C_SEL + (h + 1) * 128] = 1.0
    c[:, C_ONES:C_ONES + 128] = 1.0
    return c


def build(n_cores, NT, dbg=False, phase=9):
    NPREV = (n_cores - 1) * NT
    NTOK = NT * 128
    GT = min(4, NT)
    NG = NT // GT
    NB = 16
    nc = bass.Bass("TRN2", target_bir_lowering=False)

    def din(name, shape, dt=F32):
        return nc.dram_tensor(name, shape, dt, kind="ExternalInput").ap()

    x_own = din("x_own", [NTOK, 1024])
    x_prev = din("x_prev", [max(NPREV, 1) * 128, 1024])
    pmask = din("pmask", [128, max(NPREV, 1)])
    c_col = din("c_col", [128, 8])
    w_ada = din("w_ada", [1024, 6144])
    b_ada = din("b_ada", [1, 6144])
    n1w = din("n1w", [128, 1024])
    n2w = din("n2w", [128, 1024])
    fnw = din("fnw", [128, 1024])
    gmnw = din("gmnw", [128, 512])
    onw = din("onw", [128, 128])
    w_tok = din("w_tok", [1024, 1536])
    w_qkv = din("w_qkv", [1024, 1536])
    w_ab = din("w_ab", [1024, 64])
    wsT = din("wsT", [128, 8 * 128])
    bs_bc = din("bs_bc", [128, 512])
    convw = din("convw", [128, 48])
    gpar = din("gpar", [128, 8])
    w_out = din("w_out", [1024, 1024])
    wq = din("wq", [1024, 2048])
    keysT = din("keysT", [128, 16 * 128])
    UT = din("UT", [1024, 16384])
    V = din("V", [16384, 1024])
    consts = din("consts", [128, C_N])
    y = nc.dram_tensor("y", [NTOK, 1024], F32, kind="ExternalOutput").ap()
    x1s = nc.dram_tensor("x1s", [NTOK, 1024], F32).ap()
    dbg_t = nc.dram_tensor("dbg", [NTOK, 2048], F32, kind="ExternalOutput").ap() if dbg else None

    P = Prog(nc)
    top = ExitStack()

    _names = {}

    def sbt(es, name, shape, dt=F32):
        n = _names.get(name, 0)
        _names[name] = n + 1
        if n:
            name = f"{name}_{n}"
        return es.enter_context(nc.sbuf_tensor(name, shape, dt))

    ps = [top.enter_context(nc.psum_tensor(f"ps{i}", [128, 512], F32)) for i in range(8)]
    bps = [Buf(f"ps{i}", excl=True) for i in range(8)]

    cst = sbt(top, "cst", [128, C_N]); bcst = Buf("cst")
    idb = sbt(top, "idb", [128, 128], BF16); bidb = Buf("idb")
    modscr = nc.dram_tensor("modscr", [3 * 128, 1024], F32).ap()
    bmodscr = Buf("modscr")
    epsc = sbt(top, "epsc", [128, 1]); bepsc = Buf("epsc")
    by = Buf("y"); bx1s = Buf("x1s"); bdbg = Buf("dbg")

    ident = cst[:, C_ID:C_ID + 128]
    triU = cst[:, C_TRIU:C_TRIU + 128]
    blk = cst[:, C_BLK:C_BLK + 128]
    negU = cst[:, C_NEGU:C_NEGU + 128]
    sU = cst[:, C_SU:C_SU + 128]
    caus = cst[:, C_CAUS:C_CAUS + 128]
    ones = cst[:, C_ONES:C_ONES + 128]

    def V_(fn, r=(), w=()):
        return P.op("vector", fn, r, w)

    def A_(fn, r=(), w=()):
        return P.op("scalar", fn, r, w)

    def G_(fn, r=(), w=()):
        return P.op("gpsimd", fn, r, w)

    def T_(fn, r=(), w=()):
        return P.op("tensor", fn, r, w)

    def D_(fn, r=(), w=(), buf=None, eng="sync"):
        return P.op(eng, fn, r, w, dma_buf=buf)

    def bc3(ap2d, n, axis):
        k = ap2d.shape[1]
        if axis == 2:
            return ap2d.unsqueeze(2).to_broadcast([ap2d.shape[0], k, n])
        return ap2d.unsqueeze(1).to_broadcast([ap2d.shape[0], n, k])

    D_(lambda e: e.dma_start(out=cst[:], in_=consts[:, :]), w=[bcst], buf=bcst)
    V_(lambda e: e.tensor_copy(out=idb[:], in_=ident), r=[bcst], w=[bidb])
    V_(lambda e: e.memset(epsc[:], EPS), w=[bepsc])

    mx = ExitStack()
    A1 = sbt(mx, "A1", [128, 1024]); B1 = sbt(mx, "B1", [128, 1024]); GT1 = sbt(mx, "GT1", [128, 1024])
    bA1, bB1, bGT1 = Buf("A1"), Buf("B1"), Buf("GT1")
    wtok = sbt(mx, "wtok", [128, 8, 1536], BF16); bwtok = Buf("wtok")
    wout = sbt(mx, "wout", [128, 8, 1024], BF16); bwout = Buf("wout")
    wqkv = sbt(mx, "wqkv", [128, 8, 1536], BF16); bwqkv = Buf("wqkv")
    wab = sbt(mx, "wab", [128, 8, 64], BF16); bwab = Buf("wab")
    wsm = sbt(mx, "wsm", [128, 8, 128]); bwsm = Buf("wsm")
    bsb = sbt(mx, "bsb", [128, 512]); bbsb = Buf("bsb")
    gmn = sbt(mx, "gmn", [128, 512]); bgmn = Buf("gmn")
    onb = sbt(mx, "onb", [128, 128]); bonb = Buf("onb")
    cw = sbt(mx, "cw", [128, 12, 4]); bcw = Buf("cw")
    gp = sbt(mx, "gp", [128, 12]); bgp = Buf("gp")

    with ExitStack() as su:
        ccl = sbt(su, "ccl", [128, 8]); bccl = Buf("ccl")
        scl = sbt(su, "scl", [128, 8]); bscl = Buf("scl")
        wad = [sbt(su, f"wad{i}", [128, 8, 512]) for i in range(2)]
        bwad = [Buf(f"wad{i}") for i in range(2)]
        bad = sbt(su, "bad", [1, 6144]); bbad = Buf("bad")
        tn1 = sbt(su, "tn1", [128, 1024]); btn1 = Buf("tn1")
        tn2 = sbt(su, "tn2", [128, 1024]); btn2 = Buf("tn2")
        D_(lambda e: e.dma_start(out=ccl[:], in_=c_col[:, :]), w=[bccl], buf=bccl)
        D_(lambda e: e.dma_start(out=bad[:], in_=b_ada[:, :]), w=[bbad], buf=bbad)
        A_(lambda e: e.activation(out=scl[:], in_=ccl[:], func=AF.Silu), r=[bccl], w=[bscl])
        D_(lambda e: e.dma_start(out=wqkv[:], in_=w_qkv.rearrange("(kc p) n -> p kc n", p=128)), w=[bwqkv], buf=bwqkv, eng="gpsimd")
        D_(lambda e: e.dma_start(out=wab[:], in_=w_ab.rearrange("(kc p) n -> p kc n", p=128)), w=[bwab], buf=bwab, eng="gpsimd")
        D_(lambda e: e.dma_start(out=wtok[:], in_=w_tok.rearrange("(kc p) n -> p kc n", p=128)), w=[bwtok], buf=bwtok, eng="gpsimd")
        D_(lambda e: e.dma_start(out=wout[:], in_=w_out.rearrange("(kc p) n -> p kc n", p=128)), w=[bwout], buf=bwout, eng="gpsimd")
        D_(lambda e: e.dma_start(out=wsm[:], in_=wsT.rearrange("p (h t) -> p h t", h=8)), w=[bwsm], buf=bwsm)
        D_(lambda e: e.dma_start(out=bsb[:], in_=bs_bc[:, :]), w=[bbsb], buf=bbsb)
        D_(lambda e: e.dma_start(out=gmn[:], in_=gmnw[:, :]), w=[bgmn], buf=bgmn)
        D_(lambda e: e.dma_start(out=onb[:], in_=onw[:, :]), w=[bonb], buf=bonb)
        D_(lambda e: e.dma_start(out=cw[:], in_=convw.rearrange("p (c k) -> p c k", k=4)), w=[bcw], buf=bcw)
        D_(lambda e: e.dma_start(out=gp[:, 0:8], in_=gpar[:, :]), w=[bgp], buf=bgp)
        V_(lambda e: e.tensor_tensor(out=wsm[:], in0=wsm[:], in1=bc3(caus, 8, 1), op=ALU.mult), r=[bwsm, bcst], w=[bwsm])
        A_(lambda e: e.activation(out=gp[:, 8:12], in_=gp[:, 0:4], func=AF.Exp), r=[bgp], w=[bgp])
        V_(lambda e: e.tensor_scalar(out=gp[:, 8:12], in0=gp[:, 8:12], scalar1=-1.0, scalar2=None, op0=ALU.mult), r=[bgp], w=[bgp])
        dests = [(B1, bB1), (None, None), (GT1, bGT1), (tn2, btn2), (None, None), (tn2, btn2)]
        scr_row = {3: 1, 4: 0, 5: 2}
        for j in range(12):
            sl = j % 2
            D_(lambda e, j=j, sl=sl: e.dma_start(out=wad[sl][:], in_=w_ada[:, j * 512:(j + 1) * 512].rearrange("(kc p) n -> p kc n", p=128)),
               w=[bwad[sl]], buf=bwad[sl])
            pb = j % 2
            for kc in range(8):
                T_(lambda e, kc=kc, sl=sl, pb=pb: e.matmul(ps[pb][:, :], lhsT=scl[:, kc:kc + 1].to_broadcast([128, 128]), rhs=wad[sl][:, kc, :],
                                                           start=(kc == 0), stop=False), r=[bscl, bwad[sl]], w=[bps[pb]])
            T_(lambda e, j=j, pb=pb: e.matmul(ps[pb][:, :], lhsT=ones[0:1, :], rhs=bad[0:1, j * 512:(j + 1) * 512], start=False, stop=True),
               r=[bcst, bbad], w=[bps[pb]])
            vec, half = j // 2, j % 2
            cs = slice(half * 512, (half + 1) * 512)
            if vec in (1, 4):
                dst, bdst, nw = (A1, bA1, n1w) if vec == 1 else (tn2, btn2, n2w)
                if half == 0:
                    D_(lambda e, nw=nw: e.dma_start(out=tn1[:], in_=nw[:, :]), w=[btn1], buf=btn1)
                V_(lambda e, dst=dst, cs=cs, pb=pb: e.scalar_tensor_tensor(out=dst[:, cs], in0=ps[pb][:, :], scalar=1.0, in1=tn1[:, cs],
                                                                         op0=ALU.add, op1=ALU.mult), r=[bps[pb], btn1], w=[bdst])
            else:
                dst, bdst = dests[vec]
                A_(lambda e, dst=dst, cs=cs, pb=pb: e.activation(out=dst[:, cs], in_=ps[pb][:, :], func=AF.Copy), r=[bps[pb]], w=[bdst])
            if vec >= 3 and half == 1:
                rr0 = scr_row[vec] * 128
                D_(lambda e, rr0=rr0: e.dma_start(out=modscr[rr0:rr0 + 128, :], in_=tn2[:]), r=[btn2], w=[bmodscr], buf=bmodscr)
        P.barrier([bwad[0], bwad[1], btn1, btn2, bbad, bscl, bccl, bmodscr])

    if phase == 0:
        P.op("sync", lambda e: e.nop())
        P.emit(); mx.close(); top.close()
        return nc
    NBUF = 2
    xt = [sbt(mx, f"xt{i}", [128, 1024]) for i in range(NBUF)]; bxt = [Buf(f"xt{i}") for i in range(NBUF)]
    pmk = sbt(mx, "pmk", [128, max(NPREV, 1)]); bpmk = Buf("pmk")
    junk = sbt(mx, "junk", [128, 1024]); bjunk = Buf("junk")
    junk2 = sbt(mx, "junk2", [128, 512]); bjunk2 = Buf("junk2")
    st = [sbt(mx, f"st{i}", [128, 8]) for i in range(NBUF)]; bst = [Buf(f"st{i}") for i in range(NBUF)]
    hb = [sbt(mx, f"hb{i}", [128, 1024], BF16) for i in range(NBUF)]; bhb = [Buf(f"hb{i}") for i in range(NBUF)]
    hT = [sbt(mx, f"hT{i}", [128, 8, 128], BF16) for i in range(NBUF)]; bhT = [Buf(f"hT{i}") for i in range(NBUF)]
    cin = sbt(mx, "cin", [128, 12, 131]); bcin = Buf("cin")
    cacc = sbt(mx, "cacc", [128, 12, 128]); bcacc = Buf("cacc")
    bcaccs = [Buf(f"cacc_c{i}") for i in range(12)]
    qkv = cacc; bqkv = bcacc
    sq = sbt(mx, "sq", [128, 8, 128]); bsq = Buf("sq")
    rs = sbt(mx, "rs", [128, 8, 128]); brs = Buf("rs")
    abr = sbt(mx, "abr", [128, 16]); babr = Buf("abr")
    gbt = [sbt(mx, f"gbt{i}", [128, 24]) for i in range(NBUF)]; bgbt = [Buf(f"gbt{i}") for i in range(NBUF)]
    kT = [sbt(mx, f"kT{i}", [128, 4, 128]) for i in range(NBUF)]; bkT = [Buf(f"kT{i}") for i in range(NBUF)]
    kdec = [sbt(mx, f"kdec{i}", [128, 4, 128], BF16) for i in range(NBUF)]; bkdec = [Buf(f"kdec{i}") for i in range(NBUF)]
    ck = [sbt(mx, f"ck{i}", [128, 4, 128], BF16) for i in range(NBUF)]; bck = [Buf(f"ck{i}") for i in range(NBUF)]
    vb = [sbt(mx, f"vb{i}", [128, 4, 128], BF16) for i in range(NBUF)]; bvb = [Buf(f"vb{i}") for i in range(NBUF)]
    qdT = [sbt(mx, f"qdT{i}", [128, 4, 128]) for i in range(NBUF)]; bqdT = [Buf(f"qdT{i}") for i in range(NBUF)]
    LT = [sbt(mx, f"LT{i}", [128, 4, 128], BF16) for i in range(NBUF)]; bLT = [Buf(f"LT{i}") for i in range(NBUF)]
    Erow = [sbt(mx, f"Erow{i}", [128, 4, 128]) for i in range(NBUF)]; bErow = [Buf(f"Erow{i}") for i in range(NBUF)]
    DTi = sbt(mx, "DTi", [128, 4, 128]); bDTi = Buf("DTi")
    BU = sbt(mx, "BU", [128, 4, 128]); bBU = Buf("BU")
    atT = [sbt(mx, f"atT{i}", [128, 4, 128]) for i in range(NBUF)]; batT = [Buf(f"atT{i}") for i in range(NBUF)]
    Pm = [sbt(mx, f"Pm{i}", [128, 4, 128], BF16) for i in range(2)]; bPm = [Buf(f"Pm{i}") for i in range(2)]
    PTm = [sbt(mx, f"PTm{i}", [128, 4, 128], BF16) for i in range(2)]; bPTm = [Buf(f"PTm{i}") for i in range(2)]
    Xm = [sbt(mx, f"Xm{i}", [128, 4, 128], BF16) for i in range(2)]; bXm = [Buf(f"Xm{i}") for i in range(2)]
    A1m = sbt(mx, "A1m", [128, 4, 128], BF16); bA1m = Buf("A1m")
    U1m = sbt(mx, "U1m", [128, 4, 128], BF16); bU1m = Buf("U1m")
    MT = [sbt(mx, f"MT{i}", [128, 4, 128]) for i in range(2)]; bMT = [Buf(f"MT{i}") for i in range(2)]
    Cm = [sbt(mx, f"Cm{i}", [128, 4, 128]) for i in range(2)]; bCm = [Buf(f"Cm{i}") for i in range(2)]
    eI = sbt(mx, "eI", [128, 4, 128]); beI = Buf("eI")
    Sst = [sbt(mx, f"Sst{i}", [128, 4, 128]) for i in range(2)]; bSst = [Buf(f"Sst{i}") for i in range(2)]
    rr = sbt(mx, "rr", [128, 4, 128], BF16); brr = Buf("rr")
    vn = sbt(mx, "vn", [128, 4, 128]); bvn = Buf("vn")
    osb = sbt(mx, "osb", [128, 4, 128]); bosb = Buf("osb")
    uu = sbt(mx, "uu", [128, 512]); buu = Buf("uu")
    vv = sbt(mx, "vv", [128, 512]); bvv = Buf("vv")
    zz = sbt(mx, "zz", [128, 512]); bzz = Buf("zz")
    mix = sbt(mx, "mix", [128, 1024], BF16); bmix = Buf("mix")
    mixT = sbt(mx, "mixT", [128, 8, 128], BF16); bmixT = Buf("mixT")
    x1t = sbt(mx, "x1t", [128, 1024]); bx1t = Buf("x1t")

    if NPREV > 0:
        D_(lambda e: e.dma_start(out=pmk[:], in_=pmask[:, :]), w=[bpmk], buf=bpmk)
    G_(lambda e: e.memset(cin[:], 0.0), w=[bcin])
    G_(lambda e: e.memset(Sst[0][:], 0.0), w=[bSst[0]])
    scur = [0]
    mtc = [0]

    def stage1(ti, own, g):
        b = g % NBUF
        need_q = own or (ti == NPREV - 1)
        c0 = 0 if need_q else 4
        src = x_own[ti * 128:(ti + 1) * 128, :] if own else x_prev[ti * 128:(ti + 1) * 128, :]
        D_(lambda e: e.dma_start(out=xt[b][:], in_=src), w=[bxt[b]], buf=bxt[b])
        yield
        A_(lambda e: e.activation(out=junk[:], in_=xt[b][:], func=AF.Square, accum_out=st[b][:, 0:1]), r=[bxt[b]], w=[bjunk, bst[b]])
        A_(lambda e: e.activation(out=st[b][:, 1:2], in_=st[b][:, 0:1], func=AF.Ln, bias=epsc[:, 0:1], scale=1.0 / 1024), r=[bst[b], bepsc], w=[bst[b]])
        A_(lambda e: e.activation(out=st[b][:, 2:3], in_=st[b][:, 1:2], func=AF.Exp, scale=-0.5), r=[bst[b]], w=[bst[b]])
        yield
        V_(lambda e: e.scalar_tensor_tensor(out=junk[:], in0=xt[b][:], scalar=st[b][:, 2:3], in1=A1[:], op0=ALU.mult, op1=ALU.mult),
           r=[bxt[b], bst[b], bA1], w=[bjunk])
        if own:
            V_(lambda e: e.tensor_tensor(out=hb[b][:], in0=junk[:], in1=B1[:], op=ALU.add), r=[bjunk, bB1], w=[bhb[b]])
        else:
            V_(lambda e: e.scalar_tensor_tensor(out=hb[b][:], in0=B1[:], scalar=pmk[:, ti:ti + 1], in1=junk[:], op0=ALU.mult, op1=ALU.add),
               r=[bjunk, bB1, bpmk], w=[bhb[b]])
        yield
        for kc in range(8):
            T_(lambda e, kc=kc: e.matmul(ps[kc // 4][:, (kc % 4) * 128:(kc % 4 + 1) * 128],
                                         lhsT=hb[b][:, kc * 128:(kc + 1) * 128], rhs=idb[:], start=True, stop=True), r=[bhb[b], bidb], w=[bps[kc // 4]])
        A_(lambda e: e.activation(out=hT[b][:, 0:4, :], in_=ps[0][:, :].rearrange("p (a c) -> p a c", a=4), func=AF.Copy), r=[bps[0]], w=[bhT[b]])
        V_(lambda e: e.tensor_copy(out=hT[b][:, 4:8, :], in_=ps[1][:, :].rearrange("p (a c) -> p a c", a=4)), r=[bps[1]], w=[bhT[b]])
        yield
        for ch in range(c0, 12):
            bank = ch // 4
            for kc in range(8):
                T_(lambda e, ch=ch, kc=kc, bank=bank: e.matmul(ps[bank][:, (ch % 4) * 128:(ch % 4 + 1) * 128], lhsT=wqkv[:, kc, ch * 128:(ch + 1) * 128],
                                                              rhs=hT[b][:, kc, :], start=(kc == 0), stop=(kc == 7)), r=[bwqkv, bhT[b]], w=[bps[bank]])
            if ch % 4 == 3:
                eng = A_ if bank != 1 else V_
                if bank != 1:
                    A_(lambda e, bank=bank: e.activation(out=cin[:, bank * 4:(bank + 1) * 4, 3:131], in_=ps[bank][:, :].rearrange("p (a c) -> p a c", a=4), func=AF.Copy),
                       r=[bps[bank]], w=[bcin])
                else:
                    V_(lambda e, bank=bank: e.tensor_copy(out=cin[:, bank * 4:(bank + 1) * 4, 3:131], in_=ps[bank][:, :].rearrange("p (a c) -> p a c", a=4)),
                       r=[bps[bank]], w=[bcin])
                yield
        for kc in range(8):
            T_(lambda e, kc=kc: e.matmul(ps[3][:, 0:64], lhsT=hT[b][:, kc, :], rhs=wab[:, kc, :], start=(kc == 0), stop=(kc == 7)), r=[bwab, bhT[b]], w=[bps[3]])
        V_(lambda e: e.tensor_tensor(out=abr[:, 0:4], in0=ps[3][:, 0:4], in1=gp[:, 4:8], op=ALU.add), r=[bps[3], bgp], w=[babr])
        A_(lambda e: e.activation(out=abr[:, 0:4], in_=abr[:, 0:4], func=AF.Exp), r=[babr], w=[babr])
        A_(lambda e: e.activation(out=abr[:, 4:8], in_=ps[3][:, 32:36], func=AF.Exp, scale=-1.0), r=[bps[3]], w=[babr])
        A_(lambda e: e.activation(out=abr[:, 8:12], in_=abr[:, 0:4], func=AF.Ln, bias=1.0, scale=1.0), r=[babr], w=[babr])
        V_(lambda e: e.tensor_tensor(out=gbt[b][:, 0:4], in0=abr[:, 8:12], in1=gp[:, 8:12], op=ALU.mult), r=[babr, bgp], w=[bgbt[b]])
        V_(lambda e: e.tensor_scalar(out=abr[:, 12:16], in0=abr[:, 4:8], scalar1=1.0, scalar2=None, op0=ALU.add), r=[babr], w=[babr])
        V_(lambda e: e.reciprocal(out=gbt[b][:, 4:8], in_=abr[:, 12:16]), r=[babr], w=[bgbt[b]])
        yield
        for k in range(4):
            for ch in range(c0, 12):
                if k == 0:
                    V_(lambda e, ch=ch: e.tensor_scalar(out=cacc[:, ch, :], in0=cin[:, ch, 0:128], scalar1=cw[:, ch, 0:1], scalar2=None, op0=ALU.mult),
                       r=[bcin, bcw], w=[bcaccs[ch], bcacc])
                else:
                    V_(lambda e, ch=ch, k=k: e.scalar_tensor_tensor(out=cacc[:, ch, :], in0=cin[:, ch, k:k + 128], scalar=cw[:, ch, k:k + 1], in1=cacc[:, ch, :],
                                                                    op0=ALU.mult, op1=ALU.add), r=[bcin, bcw, bcaccs[ch]], w=[bcaccs[ch]])
            yield
        G_(lambda e: e.tensor_copy(out=cin[:, :, 0:3], in_=cin[:, :, 128:131]), r=[bcin], w=[bcin])
        A_(lambda e: e.activation(out=qkv[:, c0:12, :], in_=cacc[:, c0:12, :], func=AF.Silu), r=[bcacc] + bcaccs[c0:12], w=[bcacc])
        yield
        A_(lambda e: e.activation(out=sq[:, c0:8, :], in_=qkv[:, c0:8, :], func=AF.Square), r=[bqkv], w=[bsq])
        for hf in range(c0 // 4, 2):
            T_(lambda e, hf=hf: e.matmul(ps[hf][:, :], lhsT=ones, rhs=sq[:, hf * 4:(hf + 1) * 4, :].rearrange("p a c -> p (a c)"), start=True, stop=True),
               r=[bcst, bsq], w=[bps[hf]])
            A_(lambda e, hf=hf: e.activation(out=rs[:, hf * 4:(hf + 1) * 4, :].rearrange("p a c -> p (a c)"), in_=ps[hf][:, :], func=AF.Ln, bias=epsc[:, 0:1], scale=1.0),
               r=[bps[hf], bepsc], w=[brs])
        A_(lambda e: e.activation(out=rs[:, c0:8, :], in_=rs[:, c0:8, :], func=AF.Exp, scale=-0.5), r=[brs], w=[brs])
        yield
        T_(lambda e: e.matmul(ps[2][:, 8:12], lhsT=triU, rhs=gbt[b][:, 0:4], start=True, stop=True), r=[bcst, bgbt[b]], w=[bps[2]])
        T_(lambda e: e.matmul(ps[2][:, 12:16], lhsT=blk, rhs=gbt[b][:, 0:4], start=True, stop=True), r=[bcst, bgbt[b]], w=[bps[2]])
        V_(lambda e: e.tensor_copy(out=gbt[b][:, 8:16], in_=ps[2][:, 8:16]), r=[bps[2]], w=[bgbt[b]])
        for h in range(4):
            T_(lambda e, h=h: e.matmul(ps[3][:, h * 128:(h + 1) * 128], lhsT=gbt[b][:, h:h + 1].to_broadcast([128, 128]), rhs=triU, start=True, stop=True),
               r=[bgbt[b], bcst], w=[bps[3]])
        yield
        import os as _os; _sk = _os.environ.get('KSKIP', '')
        if 'cols' not in _sk:
            A_(lambda e: e.activation(out=gbt[b][:, 16:20], in_=gbt[b][:, 8:12], func=AF.Exp), r=[bgbt[b]], w=[bgbt[b]])
            V_(lambda e: e.scalar_tensor_tensor(out=gbt[b][:, 16:20], in0=gbt[b][:, 16:20], scalar=-1.0, in1=gbt[b][:, 4:8], op0=ALU.mult, op1=ALU.mult),
               r=[bgbt[b]], w=[bgbt[b]])
            V_(lambda e: e.tensor_tensor(out=gbt[b][:, 20:24], in0=gbt[b][:, 12:16], in1=gbt[b][:, 8:12], op=ALU.subtract), r=[bgbt[b]], w=[bgbt[b]])
            A_(lambda e: e.activation(out=gbt[b][:, 20:24], in_=gbt[b][:, 20:24], func=AF.Exp), r=[bgbt[b]], w=[bgbt[b]])
        if 'erow' not in _sk:
            A_(lambda e: e.activation(out=Erow[b][:].rearrange("p a c -> p (a c)"), in_=ps[3][:, :], func=AF.Exp), r=[bps[3]], w=[bErow[b]])
        if 'dti' not in _sk:
            V_(lambda e: e.tensor_tensor(out=DTi[:], in0=ps[3][:, :].rearrange("p (a c) -> p a c", a=4), in1=bc3(negU, 4, 1), op=ALU.add), r=[bps[3], bcst], w=[bDTi])
            V_(lambda e: e.tensor_tensor(out=DTi[:], in0=DTi[:], in1=bc3(gbt[b][:, 8:12], 128, 2), op=ALU.subtract), r=[bDTi, bgbt[b]], w=[bDTi])
            A_(lambda e: e.activation(out=DTi[:], in_=DTi[:], func=AF.Exp), r=[bDTi], w=[bDTi])
            yield
        for h in range(4):
            T_(lambda e, h=h: e.matmul(ps[2][:, h * 128:(h + 1) * 128], lhsT=gbt[b][:, 4 + h:5 + h].to_broadcast([128, 128]), rhs=ident, start=True, stop=True),
               r=[bcst, bgbt[b]], w=[bps[2]])
        V_(lambda e: e.tensor_tensor(out=BU[:], in0=ps[2][:, :].rearrange("p (a c) -> p a c", a=4), in1=bc3(sU, 4, 1), op=ALU.mult), r=[bps[2], bcst], w=[bBU])
        V_(lambda e: e.tensor_tensor(out=BU[:], in0=BU[:], in1=DTi[:], op=ALU.mult), r=[bBU, bDTi], w=[bBU])
        V_(lambda e: e.tensor_tensor(out=kT[b][:], in0=qkv[:, 4:8, :], in1=rs[:, 4:8, :], op=ALU.mult), r=[bqkv, brs], w=[bkT[b]])
        if own:
            V_(lambda e: e.scalar_tensor_tensor(out=qdT[b][:], in0=qkv[:, 0:4, :], scalar=128.0 ** -0.5, in1=rs[:, 0:4, :], op0=ALU.mult, op1=ALU.mult),
               r=[bqkv, brs], w=[bqdT[b]])
        yield
        for h in range(4):
            T_(lambda e, h=h: e.matmul(ps[0][:, h * 128:(h + 1) * 128], lhsT=kT[b][:, h, :], rhs=kT[b][:, h, :], start=True, stop=True), r=[bkT[b]], w=[bps[0]])
        if own:
            for h in range(4):
                T_(lambda e, h=h: e.matmul(ps[1][:, h * 128:(h + 1) * 128], lhsT=kT[b][:, h, :], rhs=qdT[b][:, h, :], start=True, stop=True), r=[bkT[b], bqdT[b]], w=[bps[1]])
        V_(lambda e: e.tensor_tensor(out=LT[b][:], in0=ps[0][:, :].rearrange("p (a c) -> p a c", a=4), in1=BU[:], op=ALU.mult), r=[bps[0], bBU], w=[bLT[b]])
        if own:
            V_(lambda e: e.tensor_tensor(out=atT[b][:], in0=ps[1][:, :].rearrange("p (a c) -> p a c", a=4), in1=DTi[:], op=ALU.mult), r=[bps[1], bDTi], w=[batT[b]])
            G_(lambda e: e.tensor_tensor(out=qdT[b][:], in0=qdT[b][:], in1=Erow[b][:], op=ALU.mult), r=[bqdT[b], bErow[b]], w=[bqdT[b]])
        yield
        for h in range(4):
            T_(lambda e, h=h: e.matmul(ps[2][:, h * 128:(h + 1) * 128], lhsT=kT[b][:, h, :], rhs=ident, start=True, stop=True), r=[bkT[b], bcst], w=[bps[2]])
        for h in range(4):
            T_(lambda e, h=h: e.matmul(ps[3][:, h * 128:(h + 1) * 128], lhsT=qkv[:, 8 + h, :], rhs=ident, start=True, stop=True), r=[bqkv, bcst], w=[bps[3]])
        V_(lambda e: e.tensor_tensor(out=kdec[b][:], in0=ps[2][:, :].rearrange("p (a c) -> p a c", a=4), in1=bc3(gbt[b][:, 20:24], 128, 2), op=ALU.mult),
           r=[bps[2], bgbt[b]], w=[bkdec[b]])
        V_(lambda e: e.tensor_tensor(out=ck[b][:], in0=ps[2][:, :].rearrange("p (a c) -> p a c", a=4), in1=bc3(gbt[b][:, 16:20], 128, 2), op=ALU.mult),
           r=[bps[2], bgbt[b]], w=[bck[b]])
        V_(lambda e: e.tensor_tensor(out=vb[b][:], in0=ps[3][:, :].rearrange("p (a c) -> p a c", a=4), in1=bc3(gbt[b][:, 4:8], 128, 2), op=ALU.mult),
           r=[bps[3], bgbt[b]], w=[bvb[b]])
        yield

    def stage2(ti, own, g):
        b = g % NBUF
        for h in range(4):
            T_(lambda e, h=h: e.matmul(ps[4][:, h * 128:(h + 1) * 128], lhsT=LT[b][:, h, :], rhs=idb[:], start=True, stop=True), r=[bLT[b], bidb], w=[bps[4]])
        A_(lambda e: e.activation(out=Pm[0][:].rearrange("p a c -> p (a c)"), in_=ps[4][:, :], func=AF.Copy), r=[bps[4]], w=[bPm[0]])
        G_(lambda e: e.tensor_tensor(out=Xm[0][:], in0=bc3(idb[:], 4, 1), in1=LT[b][:], op=ALU.subtract), r=[bidb, bLT[b]], w=[bXm[0]])
        yield
        cur = 0
        PTl = [LT[b], PTm[1]]; bPTl = [bLT[b], bPTm[1]]
        for lvl in range(5):
            nxt = 1 - cur
            if lvl == 1:
                PTl[0] = PTm[0]; bPTl[0] = bPTm[0]
            for h in range(4):
                T_(lambda e, h=h, cur=cur, pt=PTl[cur]: e.matmul(ps[4][:, h * 128:(h + 1) * 128], lhsT=pt[:, h, :], rhs=Pm[cur][:, h, :], start=True, stop=True),
                   r=[bPTl[cur], bPm[cur]], w=[bps[4]])
            if lvl < 4:
                for h in range(4):
                    T_(lambda e, h=h, cur=cur, pt=PTl[cur]: e.matmul(ps[5][:, h * 128:(h + 1) * 128], lhsT=Pm[cur][:, h, :], rhs=pt[:, h, :], start=True, stop=True),
                       r=[bPTl[cur], bPm[cur]], w=[bps[5]])
            A_(lambda e, nxt=nxt: e.activation(out=Pm[nxt][:].rearrange("p a c -> p (a c)"), in_=ps[4][:, :], func=AF.Copy), r=[bps[4]], w=[bPm[nxt]])
            if lvl < 4:
                V_(lambda e, nxt=nxt: e.tensor_copy(out=PTm[nxt][:].rearrange("p a c -> p (a c)"), in_=ps[5][:, :]), r=[bps[5]], w=[bPTm[nxt]])
                PTl[nxt] = PTm[nxt]; bPTl[nxt] = bPTm[nxt]
            yield
            for h in range(4):
                T_(lambda e, h=h, cur=cur: e.matmul(ps[4][:, h * 128:(h + 1) * 128], lhsT=idb[:], rhs=Xm[cur][:, h, :], start=True, stop=False),
                   r=[bidb, bXm[cur]], w=[bps[4]])
                T_(lambda e, h=h, cur=cur, nxt=nxt: e.matmul(ps[4][:, h * 128:(h + 1) * 128], lhsT=Pm[nxt][:, h, :], rhs=Xm[cur][:, h, :], start=False, stop=True),
                   r=[bPm[nxt], bXm[cur]], w=[bps[4]])
            V_(lambda e, nxt=nxt: e.tensor_copy(out=Xm[nxt][:].rearrange("p a c -> p (a c)"), in_=ps[4][:, :]), r=[bps[4]], w=[bXm[nxt]])
            yield
            cur = nxt
        TT = Xm[cur]; bTT = bXm[cur]
        for h in range(4):
            T_(lambda e, h=h: e.matmul(ps[4][:, h * 128:(h + 1) * 128], lhsT=TT[:, h, :], rhs=ck[b][:, h, :], start=True, stop=True), r=[bTT, bck[b]], w=[bps[4]])
        for h in range(4):
            T_(lambda e, h=h: e.matmul(ps[5][:, h * 128:(h + 1) * 128], lhsT=TT[:, h, :], rhs=vb[b][:, h, :], start=True, stop=True), r=[bTT, bvb[b]], w=[bps[5]])
        A_(lambda e: e.activation(out=A1m[:].rearrange("p a c -> p (a c)"), in_=ps[4][:, :], func=AF.Copy), r=[bps[4]], w=[bA1m])
        V_(lambda e: e.tensor_copy(out=U1m[:].rearrange("p a c -> p (a c)"), in_=ps[5][:, :]), r=[bps[5]], w=[bU1m])
        yield
        res = []
        for c in range(2):
            m = mtc[0] % 2
            mtc[0] += 1
            rows = slice(64 * c, 64 * c + 64)
            for h in range(4):
                T_(lambda e, h=h, rows=rows: e.matmul(ps[4][:, h * 128:(h + 1) * 128], lhsT=A1m[rows, h, :], rhs=kdec[b][rows, h, :], start=True, stop=True),
                   r=[bA1m, bkdec[b]], w=[bps[4]])
            for h in range(4):
                T_(lambda e, h=h, rows=rows: e.matmul(ps[5][:, h * 128:(h + 1) * 128], lhsT=kdec[b][rows, h, :], rhs=U1m[rows, h, :], start=True, stop=True),
                   r=[bU1m, bkdec[b]], w=[bps[5]])
            col = 64 * c + 63
            G_(lambda e, col=col: e.tensor_tensor(out=eI[:], in0=bc3(ident, 4, 1), in1=Erow[b][:, :, col:col + 1].to_broadcast([128, 4, 128]), op=ALU.mult),
               r=[bcst, bErow[b]], w=[beI])
            V_(lambda e, m=m: e.tensor_tensor(out=MT[m][:], in0=ps[4][:, :].rearrange("p (a c) -> p a c", a=4), in1=eI[:], op=ALU.add), r=[bps[4], beI], w=[bMT[m]])
            A_(lambda e, m=m: e.activation(out=Cm[m][:].rearrange("p a c -> p (a c)"), in_=ps[5][:, :], func=AF.Copy), r=[bps[5]], w=[bCm[m]])
            res.append(m)
            yield
        stage2.out[g] = (res, TT, bTT)
    stage2.out = {}

    def stage3(ti, own, g):
        b = g % NBUF
        res, TT, bTT = stage2.out[g]
        for c in range(2):
            m = res[c]
            rows = slice(64 * c, 64 * c + 64)
            s0 = scur[0]
            s1 = 1 - s0
            if own:
                for h in range(4):
                    T_(lambda e, h=h, s0=s0: e.matmul(ps[6][:, h * 128:(h + 1) * 128], lhsT=kT[b][:, h, :], rhs=Sst[s0][:, h, :], start=True, stop=True),
                       r=[bkT[b], bSst[s0]], w=[bps[6]])
                V_(lambda e, rows=rows: e.tensor_tensor(out=rr[rows], in0=ps[6][rows, :].rearrange("p (a c) -> p a c", a=4), in1=bc3(gbt[b][rows, 16:20], 128, 2), op=ALU.mult),
                   r=[bps[6], bgbt[b]], w=[brr])
                G_(lambda e, rows=rows: e.tensor_tensor(out=rr[rows], in0=rr[rows], in1=vb[b][rows], op=ALU.add), r=[brr, bvb[b]], w=[brr])
                yield
                for h in range(4):
                    T_(lambda e, h=h, rows=rows: e.matmul(ps[6][:, h * 128:(h + 1) * 128], lhsT=TT[rows, h, :], rhs=rr[rows, h, :], start=True, stop=True),
                       r=[bTT, brr], w=[bps[6]])
                A_(lambda e, rows=rows: e.activation(out=vn[rows].rearrange("p a c -> p (a c)"), in_=ps[6][rows, :], func=AF.Copy), r=[bps[6]], w=[bvn])
                yield
                for h in range(4):
                    T_(lambda e, h=h, s0=s0: e.matmul(ps[6][:, h * 128:(h + 1) * 128], lhsT=qdT[b][:, h, :], rhs=Sst[s0][:, h, :], start=True, stop=False),
                       r=[bqdT[b], bSst[s0]], w=[bps[6]])
                    T_(lambda e, h=h, rows=rows: e.matmul(ps[6][:, h * 128:(h + 1) * 128], lhsT=atT[b][rows, h, :], rhs=vn[rows, h, :], start=False, stop=True),
                       r=[batT[b], bvn], w=[bps[6]])
                A_(lambda e, rows=rows: e.activation(out=osb[rows].rearrange("p a c -> p (a c)"), in_=ps[6][rows, :], func=AF.Copy), r=[bps[6]], w=[bosb])
                yield
            for h in range(4):
                T_(lambda e, h=h, m=m: e.matmul(ps[7][:, h * 128:(h + 1) * 128], lhsT=ident, rhs=Cm[m][:, h, :], start=True, stop=False), r=[bcst, bCm[m]], w=[bps[7]])
                T_(lambda e, h=h, m=m, s0=s0: e.matmul(ps[7][:, h * 128:(h + 1) * 128], lhsT=MT[m][:, h, :], rhs=Sst[s0][:, h, :], start=False, stop=True),
                   r=[bMT[m], bSst[s0]], w=[bps[7]])
            V_(lambda e, s1=s1: e.tensor_copy(out=Sst[s1][:].rearrange("p a c -> p (a c)"), in_=ps[7][:, :]), r=[bps[7]], w=[bSst[s1]])
            scur[0] = s1
            yield

    def stage_tok(ti, g):
        b = g % NBUF
        for gi in range(3):
            for kc in range(8):
                T_(lambda e, gi=gi, kc=kc: e.matmul(ps[gi][:, :], lhsT=hT[b][:, kc, :], rhs=wtok[:, kc, gi * 512:(gi + 1) * 512], start=(kc == 0), stop=(kc == 7)),
                   r=[bhT[b], bwtok], w=[bps[gi]])
        A_(lambda e: e.activation(out=uu[:], in_=ps[0][:, :], func=AF.Gelu_apprx_tanh), r=[bps[0]], w=[buu])
        A_(lambda e: e.activation(out=vv[:], in_=ps[1][:, :], func=AF.Gelu_apprx_tanh), r=[bps[1]], w=[bvv])
        A_(lambda e: e.activation(out=zz[:], in_=ps[2][:, :], func=AF.Silu), r=[bps[2]], w=[bzz])
        yield
        A_(lambda e: e.activation(out=junk2[:], in_=vv[:], func=AF.Square, accum_out=st[b][:, 4:5]), r=[bvv], w=[bjunk2, bst[b]])
        A_(lambda e: e.activation(out=st[b][:, 5:6], in_=st[b][:, 4:5], func=AF.Sqrt, bias=epsc[:, 0:1], scale=1.0 / 512), r=[bst[b], bepsc], w=[bst[b]])
        V_(lambda e: e.reciprocal(out=st[b][:, 6:7], in_=st[b][:, 5:6]), r=[bst[b]], w=[bst[b]])
        V_(lambda e: e.scalar_tensor_tensor(out=vv[:], in0=vv[:], scalar=st[b][:, 6:7], in1=gmn[:], op0=ALU.mult, op1=ALU.mult), r=[bvv, bst[b], bgmn], w=[bvv])
        yield
        for h in range(8):
            T_(lambda e, h=h: e.matmul(ps[0][:, h * 64:(h + 1) * 64], lhsT=wsm[:, h, :], rhs=vv[:, h * 64:(h + 1) * 64], start=True, stop=True), r=[bwsm, bvv], w=[bps[0]])
        V_(lambda e: e.tensor_tensor(out=junk2[:], in0=ps[0][:, :], in1=bsb[:], op=ALU.add), r=[bps[0], bbsb], w=[bjunk2])
        V_(lambda e: e.tensor_tensor(out=mix[:, 0:512], in0=junk2[:], in1=uu[:], op=ALU.mult), r=[bjunk2, buu], w=[bmix])
        yield

    def stage_out(ti, g):
        b = g % NBUF
        for h in range(4):
            A_(lambda e, h=h: e.activation(out=junk2[:, h * 128:(h + 1) * 128], in_=osb[:, h, :], func=AF.Square, accum_out=st[b][:, 4 + h:5 + h]), r=[bosb], w=[bjunk2, bst[b]])
        A_(lambda e: e.activation(out=st[b][:, 4:8], in_=st[b][:, 4:8], func=AF.Sqrt, bias=epsc[:, 0:1], scale=1.0 / 128), r=[bst[b], bepsc], w=[bst[b]])
        V_(lambda e: e.reciprocal(out=st[b][:, 4:8], in_=st[b][:, 4:8]), r=[bst[b]], w=[bst[b]])
        V_(lambda e: e.tensor_tensor(out=osb[:], in0=osb[:], in1=bc3(st[b][:, 4:8], 128, 2), op=ALU.mult), r=[bosb, bst[b]], w=[bosb])
        G_(lambda e: e.tensor_tensor(out=osb[:], in0=osb[:], in1=bc3(onb[:], 4, 1), op=ALU.mult), r=[bosb, bonb], w=[bosb])
        V_(lambda e: e.tensor_tensor(out=mix[:, 512:1024], in0=osb[:].rearrange("p a c -> p (a c)"), in1=zz[:], op=ALU.mult), r=[bosb, bzz], w=[bmix])
        yield
        for kc in range(8):
            T_(lambda e, kc=kc: e.matmul(ps[kc // 4][:, (kc % 4) * 128:(kc % 4 + 1) * 128], lhsT=mix[:, kc * 128:(kc + 1) * 128], rhs=idb[:], start=True, stop=True),
               r=[bmix, bidb], w=[bps[kc // 4]])
        A_(lambda e: e.activation(out=mixT[:, 0:4, :], in_=ps[0][:, :].rearrange("p (a c) -> p a c", a=4), func=AF.Copy), r=[bps[0]], w=[bmixT])
        V_(lambda e: e.tensor_copy(out=mixT[:, 4:8, :], in_=ps[1][:, :].rearrange("p (a c) -> p a c", a=4)), r=[bps[1]], w=[bmixT])
        yield
        for hf in range(2):
            for kc in range(8):
                T_(lambda e, hf=hf, kc=kc: e.matmul(ps[2 + hf][:, :], lhsT=mixT[:, kc, :], rhs=wout[:, kc, hf * 512:(hf + 1) * 512], start=(kc == 0), stop=(kc == 7)),
                   r=[bmixT, bwout], w=[bps[2 + hf]])
            cs = slice(hf * 512, (hf + 1) * 512)
            V_(lambda e, hf=hf, cs=cs: e.tensor_tensor(out=x1t[:, cs], in0=ps[2 + hf][:, :], in1=GT1[:, cs], op=ALU.mult), r=[bps[2 + hf], bGT1], w=[bx1t])
        G_(lambda e: e.tensor_tensor(out=x1t[:], in0=x1t[:], in1=xt[b][:], op=ALU.add), r=[bx1t, bxt[b]], w=[bx1t])
        D_(lambda e: e.dma_start(out=x1s[ti * 128:(ti + 1) * 128, :], in_=x1t[:]), r=[bx1t], w=[bx1s], buf=bx1s)
        yield

    tiles = [(t, False) for t in range(NPREV)] + [(t, True) for t in range(NT)]

    def tile_rest(ti, own, g):
        if phase == 11:
            return
        yield from stage2(ti, own, g)
        if own and phase not in (12, 13):
            yield from stage_tok(ti, g)
        if phase == 12:
            return
        yield from stage3(ti, own, g)
        if own and phase not in (13, 14):
            yield from stage_out(ti, g)

    import os, itertools
    CUT = int(os.environ.get("KCUT", "1000"))
    _stage1 = stage1
    stage1 = lambda a, b_, c_: itertools.islice(_stage1(a, b_, c_), CUT)
    run_interleaved([stage1(tiles[0][0], tiles[0][1], 0)])
    for g, (ti, own) in enumerate(tiles):
        gens = [tile_rest(ti, own, g)]
        if g + 1 < len(tiles):
            gens.append(stage1(tiles[g + 1][0], tiles[g + 1][1], g + 1))
        run_interleaved(gens)

    P.barrier([bx1s, bcin, bSst[0], bSst[1], bpmk])
    if phase in (1, 11, 12, 13, 14):
        P.op("sync", lambda e: e.nop(), r=[bx1s])
        P.emit(); mx.close(); top.close()
        return nc
    mx.close()

    pe = ExitStack()
    import os as _os3
    KDBG = _os3.environ.get("KDBG", "")
    s2a = sbt(pe, "s2a", [128, GT, 8, 128]); bs2a = [Buf(f"s2a{i}") for i in range(GT)]
    aa = sbt(pe, "aa", [128, GT, 8, 128]); baa = [Buf(f"aa{i}") for i in range(GT)]
    kap = sbt(pe, "kap", [128, GT, 8]); bkap = [Buf(f"kap{i}") for i in range(GT)]
    tau = sbt(pe, "tau", [128, GT, 8])
    h2T = sbt(pe, "h2T", [128, 8, GT * 128], BF16); bh2T = Buf("h2T")
    acc = sbt(pe, "acc", [128, GT, 1024]); bacc = [Buf(f"acc{i}") for i in range(GT)]
    x1g = sbt(pe, "x1g", [128, GT, 1024]); bx1g = [Buf(f"x1g{i}") for i in range(GT)]
    pst = sbt(pe, "pst", [128, 8]); bpst = Buf("pst")
    Ub = [sbt(pe, f"Ub{i}", [128, 8, 1024], BF16) for i in range(2)]; bUb = [Buf(f"Ub{i}") for i in range(2)]

    def load_U(bi):
        sl = bi % 2
        D_(lambda e: e.dma_start(out=Ub[sl][:], in_=UT[:, bi * 1024:(bi + 1) * 1024].rearrange("(kc p) n -> p kc n", p=128)), w=[bUb[sl]], buf=bUb[sl], eng="gpsimd")
    A2 = sbt(pe, "A2", [128, 1024]); B2 = sbt(pe, "B2", [128, 1024])
    GT2 = sbt(pe, "GT2", [128, 1024]); FNW = sbt(pe, "FNW", [128, 1024])
    bA2, bB2, bGT2, bFNW = Buf("A2"), Buf("B2"), Buf("GT2"), Buf("FNW")
    D_(lambda e: e.dma_start(out=A2[:], in_=modscr[0:128, :]), r=[bmodscr], w=[bA2], buf=bA2)
    D_(lambda e: e.dma_start(out=B2[:], in_=modscr[128:256, :]), r=[bmodscr], w=[bB2], buf=bB2)
    D_(lambda e: e.dma_start(out=GT2[:], in_=modscr[256:384, :]), r=[bmodscr], w=[bGT2], buf=bGT2)
    D_(lambda e: e.dma_start(out=FNW[:], in_=fnw[:, :]), w=[bFNW], buf=bFNW)

    for grp in range(NG):
        with ExitStack() as rt:
            if KDBG != 'rt':
                load_U(0)
                load_U(1)
            wqs = [sbt(rt, f"wqs{i}", [128, 8, 256]) for i in range(2)]; bwqs = [Buf(f"wqs{i}") for i in range(2)]
            h2f = sbt(rt, "h2f", [128, 8, GT * 128]); bh2f = Buf("h2f")
            kys = sbt(rt, "kys", [128, 16, 128]); bkys = Buf("kys")
            h2 = sbt(rt, "h2", [128, 1024]); bh2 = Buf("h2")
            jk = sbt(rt, "jk", [128, 1024]); bjk = Buf("jk")
            e16 = sbt(rt, "e16", [128, 8, 16]); be16 = Buf("e16")
            zz8 = sbt(rt, "zz8", [128, 16]); bzz8 = Buf("zz8")
            D_(lambda e: e.dma_start(out=kys[:], in_=keysT.rearrange("p (c k) -> p c k", c=16)), w=[bkys], buf=bkys)
            for tt in range(GT):
                ti = grp * GT + tt
                D_(lambda e, tt=tt, ti=ti: e.dma_start(out=x1g[:, tt, :], in_=x1s[ti * 128:(ti + 1) * 128, :]), r=[bx1s], w=[bx1g[tt]], buf=bx1g[tt])
                A_(lambda e, tt=tt: e.activation(out=jk[:], in_=x1g[:, tt, :], func=AF.Square, accum_out=pst[:, 0:1]), r=[bx1g[tt]], w=[bjk, bpst])
                A_(lambda e: e.activation(out=pst[:, 1:2], in_=pst[:, 0:1], func=AF.Sqrt, bias=epsc[:, 0:1], scale=1.0 / 1024), r=[bpst, bepsc], w=[bpst])
                V_(lambda e: e.reciprocal(out=pst[:, 2:3], in_=pst[:, 1:2]), r=[bpst], w=[bpst])
                V_(lambda e, tt=tt: e.scalar_tensor_tensor(out=jk[:], in0=x1g[:, tt, :], scalar=pst[:, 2:3], in1=A2[:], op0=ALU.mult, op1=ALU.mult),
                   r=[bx1g[tt], bpst, bA2], w=[bjk])
                V_(lambda e: e.tensor_tensor(out=h2[:], in0=jk[:], in1=B2[:], op=ALU.add), r=[bjk, bB2], w=[bh2])
                for kc in range(8):
                    T_(lambda e, kc=kc: e.matmul(ps[kc // 4][:, (kc % 4) * 128:(kc % 4 + 1) * 128], lhsT=h2[:, kc * 128:(kc + 1) * 128], rhs=ident, start=True, stop=True),
                       r=[bh2, bcst], w=[bps[kc // 4]])
                for hf in range(2):
                    A_(lambda e, hf=hf, tt=tt: e.activation(out=h2f[:, hf * 4:(hf + 1) * 4, tt * 128:(tt + 1) * 128], in_=ps[hf][:, :].rearrange("p (a c) -> p a c", a=4), func=AF.Copy),
                       r=[bps[hf]], w=[bh2f])
                    V_(lambda e, hf=hf, tt=tt: e.tensor_copy(out=h2T[:, hf * 4:(hf + 1) * 4, tt * 128:(tt + 1) * 128], in_=ps[hf][:, :].rearrange("p (a c) -> p a c", a=4)),
                       r=[bps[hf]], w=[bh2T])
            NW = GT * 128
            qTh = [sbt(rt, f"qTh{i}", [128, 2, NW]) for i in range(2)]; bqTh = [Buf(f"qTh{i}") for i in range(2)]
            c16g = sbt(rt, "c16g", [128, GT, 8, 16]); bc16g = [Buf(f"c16g{i}") for i in range(GT)]
            v12h = [sbt(rt, f"v12h{i}", [128, 32]) for i in range(2)]; bv12h = [Buf(f"v12h{i}") for i in range(2)]
            cdh = [sbt(rt, f"cdh{i}", [128, 256]) for i in range(2)]; bcdh = [Buf(f"cdh{i}") for i in range(2)]
            tkh = [sbt(rt, f"tkh{i}", [128, 128]) for i in range(2)]; btkh = [Buf(f"tkh{i}") for i in range(2)]
            bsc = [[Buf(f"sc{t_}_{h_}") for h_ in range(8)] for t_ in range(GT)]
            rc = 0
            for h in range(8):
                sl = h % 2
                D_(lambda e, h=h, sl=sl: e.dma_start(out=wqs[sl][:], in_=wq[:, h * 256:(h + 1) * 256].rearrange("(kc p) n -> p kc n", p=128)), w=[bwqs[sl]], buf=bwqs[sl])
                for cc in range(2):
                    pb = 2 + cc
                    for kc in range(8):
                        T_(lambda e, cc=cc, kc=kc, sl=sl, pb=pb: e.matmul(ps[pb][:, 0:NW], lhsT=wqs[sl][:, kc, cc * 128:(cc + 1) * 128], rhs=h2f[:, kc, :],
                                                                          start=(kc == 0), stop=(kc == 7)), r=[bwqs[sl], bh2f], w=[bps[pb]])
                    A_(lambda e, cc=cc, sl=sl, pb=pb: e.activation(out=qTh[sl][:, cc, :], in_=ps[pb][:, 0:NW], func=AF.Copy), r=[bps[pb]], w=[bqTh[sl]])
                for tt in range(GT):
                    tsl = slice(tt * 128, (tt + 1) * 128)
                    w_ = rc % 2
                    rc += 1
                    bk = 4 + w_
                    for half in range(2):
                        T_(lambda e, half=half, bk=bk, h=h, tsl=tsl, sl=sl: e.matmul(ps[bk][:, half * 128:(half + 1) * 128], lhsT=qTh[sl][:, half, tsl], rhs=kys[:, 2 * h + half, :],
                                                                                 start=True, stop=True), r=[bqTh[sl], bkys], w=[bps[bk]])
                    A_(lambda e, tt=tt, h=h, bk=bk: e.activation(out=aa[:, tt, h, :], in_=ps[bk][:, 0:128], func=AF.Copy), r=[bps[bk]], w=[bsc[tt][h]])
                    A_(lambda e, tt=tt, h=h, bk=bk: e.activation(out=s2a[:, tt, h, :], in_=ps[bk][:, 128:256], func=AF.Copy), r=[bps[bk]], w=[bsc[tt][h]])
                    for half, src in enumerate((aa[:, tt, h, :], s2a[:, tt, h, :])):
                        o0 = half * 16
                        V_(lambda e, src=src, o0=o0, w_=w_: e.max(out=v12h[w_][:, o0:o0 + 8], in_=src), r=[bsc[tt][h]], w=[bv12h[w_]])
                        V_(lambda e, src=src, o0=o0, w_=w_: e.match_replace(out=tkh[w_][:], in_to_replace=v12h[w_][:, o0:o0 + 8], in_values=src, imm_value=-1e30),
                           r=[bsc[tt][h], bv12h[w_]], w=[btkh[w_]])
                        V_(lambda e, o0=o0, w_=w_: e.max(out=v12h[w_][:, o0 + 8:o0 + 16], in_=tkh[w_][:]), r=[btkh[w_]], w=[bv12h[w_]])
                    V_(lambda e, w_=w_: e.tensor_tensor(out=cdh[w_][:].rearrange("p (a b) -> p a b", a=16), in0=v12h[w_][:, 0:16].unsqueeze(2).to_broadcast([128, 16, 16]),
                                                        in1=v12h[w_][:, 16:32].unsqueeze(1).to_broadcast([128, 16, 16]), op=ALU.add), r=[bv12h[w_]], w=[bcdh[w_]])
                    V_(lambda e, tt=tt, h=h, w_=w_: e.max(out=c16g[:, tt, h, 0:8], in_=cdh[w_][:]), r=[bcdh[w_]], w=[bc16g[tt]])
                    V_(lambda e, tt=tt, h=h, w_=w_: e.match_replace(out=cdh[w_][:], in_to_replace=c16g[:, tt, h, 0:8], in_values=cdh[w_][:], imm_value=-1e30),
                       r=[bcdh[w_], bc16g[tt]], w=[bcdh[w_]])
                    V_(lambda e, tt=tt, h=h, w_=w_: e.max(out=c16g[:, tt, h, 8:16], in_=cdh[w_][:]), r=[bcdh[w_]], w=[bc16g[tt]])
            for tt in range(GT):
                V_(lambda e, tt=tt: e.tensor_copy(out=tau[:, tt, :], in_=c16g[:, tt, :, 15]), r=[bc16g[tt]], w=[bkap[tt]])
                V_(lambda e, tt=tt: e.tensor_tensor(out=e16[:], in0=c16g[:, tt], in1=c16g[:, tt, :, 0:1].to_broadcast([128, 8, 16]), op=ALU.subtract), r=[bc16g[tt]], w=[be16])
                A_(lambda e: e.activation(out=e16[:], in_=e16[:], func=AF.Exp), r=[be16], w=[be16])
                V_(lambda e: e.tensor_reduce(out=zz8[:, 0:8], in_=e16[:], axis=mybir.AxisListType.X, op=ALU.add), r=[be16], w=[bzz8])
                A_(lambda e: e.activation(out=zz8[:, 8:16], in_=zz8[:, 0:8], func=AF.Ln), r=[bzz8], w=[bzz8])
                V_(lambda e, tt=tt: e.tensor_tensor(out=kap[:, tt, :], in0=c16g[:, tt, :, 0], in1=zz8[:, 8:16], op=ALU.add), r=[bc16g[tt], bzz8], w=[bkap[tt]])
                V_(lambda e, tt=tt: e.tensor_scalar(out=kap[:, tt, :], in0=kap[:, tt, :], scalar1=-1.0, scalar2=None, op0=ALU.mult), r=[bkap[tt]], w=[bkap[tt]])
                G_(lambda e, tt=tt: e.memset(acc[:, tt, :], 0.0), w=[bacc[tt]])
            P.barrier([bh2f, bkys, bwqs[0], bwqs[1], be16, bzz8, bh2, bjk] + bqTh + bv12h + bcdh + btkh + bc16g)

        with ExitStack() as ex:
            Vb = [sbt(ex, f"Vb{i}", [128, 8, 1024], BF16) for i in range(2)]; bVb = [Buf(f"Vb{i}") for i in range(2)]
            Ag = [sbt(ex, f"Ag{i}", [128, 8, GT * 128], BF16) for i in range(2)]; bAg = [Buf(f"Ag{i}") for i in range(2)]
            NSP, NEB = 4, 3
            Sp = [sbt(ex, f"Sp{i}", [128, 8, 128]) for i in range(NSP)]; bSp = [Buf(f"Sp{i}") for i in range(NSP)]
            Eb = [sbt(ex, f"Eb{i}", [128, 8, 128], BF16) for i in range(NEB)]; bEb = [Buf(f"Eb{i}") for i in range(NEB)]
            Mb = [sbt(ex, f"Mb{i}", [128, 8, 128], BF16) for i in range(NEB)]; bMb = [Buf(f"Mb{i}") for i in range(NEB)]
            Wt = [sbt(ex, f"Wt{i}", [128, 8, 128], BF16) for i in range(2)]; bWt = [Buf(f"Wt{i}") for i in range(2)]
            NW = GT * 128
            NDVE = 8
            NBX = NB if KDBG != 'rt' else 0

            def load_V(bi):
                sl = bi % 2
                D_(lambda e: e.dma_start(out=Vb[sl][:], in_=V[bi * 1024:(bi + 1) * 1024, :].rearrange("(c p) n -> p c n", p=128)), w=[bVb[sl]], buf=bVb[sl], eng="gpsimd")

            def act_mm(bi, ch):
                sl = bi % 2
                pb = ch % 2
                for kc in range(8):
                    T_(lambda e, kc=kc: e.matmul(ps[pb][:, 0:NW], lhsT=Ub[sl][:, kc, ch * 128:(ch + 1) * 128], rhs=h2T[:, kc, :], start=(kc == 0), stop=(kc == 7)),
                       r=[bUb[sl], bh2T], w=[bps[pb]])

            def act_gelu(bi, ch):
                sl = bi % 2
                pb = ch % 2
                A_(lambda e: e.activation(out=Ag[sl][:, ch, :], in_=ps[pb][:, 0:NW], func=AF.Gelu_apprx_tanh), r=[bps[pb]], w=[bAg[sl]])

            def act_chunk(bi, ch):
                act_mm(bi, ch)
                act_gelu(bi, ch)

            spc = [0]
            ebc = [0]
            deferred = {}

            def unit(bi, tt, uidx):
                sl = bi % 2
                gpair = 2 + 2 * (uidx % 2)
                w3 = uidx % 2
                slots = {}

                def emit_sp(h):
                    s_ = spc[0] % NSP
                    spc[0] += 1
                    slots[h] = s_
                    eng = V_ if h < NDVE else G_
                    eng(lambda e: e.tensor_tensor(out=Sp[s_][:], in0=aa[:, tt, h, bi * 8:(bi + 1) * 8].unsqueeze(2).to_broadcast([128, 8, 128]),
                                                  in1=s2a[:, tt, h, :].unsqueeze(1).to_broadcast([128, 8, 128]), op=ALU.add),
                        r=[baa[tt], bs2a[tt]], w=[bSp[s_]])

                emit_sp(0)
                emit_sp(1)
                if 'wt' in deferred:
                    deferred.pop('wt')()
                emit_sp(2)
                emit_sp(3)
                cpu = 8 // GT
                for h in range(8):
                    s_ = slots[h]
                    w2 = ebc[0] % NEB
                    ebc[0] += 1
                    A_(lambda e, h=h, s_=s_, w2=w2: e.activation(out=Eb[w2][:], in_=Sp[s_][:], func=AF.Exp, bias=kap[:, tt, h:h + 1], scale=1.0),
                       r=[bSp[s_], bkap[tt]], w=[bEb[w2]])
                    V_(lambda e, h=h, s_=s_, w2=w2: e.scalar_tensor_tensor(out=Mb[w2][:], in0=Sp[s_][:], scalar=tau[:, tt, h:h + 1], in1=Eb[w2][:], op0=ALU.is_ge, op1=ALU.mult),
                       r=[bSp[s_], bEb[w2], bkap[tt]], w=[bMb[w2]])
                    for ch in range(8):
                        bank = gpair + ch // 4
                        T_(lambda e, ch=ch, bank=bank, h=h, w2=w2: e.matmul(ps[bank][:, (ch % 4) * 128:(ch % 4 + 1) * 128], lhsT=Mb[w2][:, ch, :], rhs=idb[:],
                                                                           start=(h == 0 and ch % 4 == 0), stop=(h == 7), skip_group_check=True), r=[bMb[w2], bidb], w=[bps[bank]])
                    if h + 4 < 8:
                        emit_sp(h + 4)
                    if h == 2 and 'acc' in deferred:
                        deferred.pop('acc')()
                    if bi + 1 < NBX:
                        pairs = max(1, cpu // 2)
                        step = 8 // pairs
                        for p_ in range(pairs):
                            h_mm = 1 if pairs == 1 else step * p_
                            h_ge = 6 if pairs == 1 else step * p_ + step - 1
                            c0 = tt * cpu + 2 * p_
                            if h == h_mm:
                                act_mm(bi + 1, c0)
                                act_mm(bi + 1, c0 + 1)
                            if h == h_ge:
                                act_gelu(bi + 1, c0)
                                act_gelu(bi + 1, c0 + 1)

                def do_wt():
                    for q in range(2):
                        V_(lambda e, q=q: e.tensor_tensor(out=Wt[w3][:, q * 4:(q + 1) * 4, :], in0=ps[gpair + q][:, :].rearrange("p (a c) -> p a c", a=4),
                                                          in1=Ag[sl][:, q * 4:(q + 1) * 4, tt * 128:(tt + 1) * 128], op=ALU.mult), r=[bps[gpair + q], bAg[sl]], w=[bWt[w3]])
                    for hf in range(2):
                        for ch in range(8):
                            T_(lambda e, hf=hf, ch=ch: e.matmul(ps[6 + hf][:, :], lhsT=Wt[w3][:, ch, :], rhs=Vb[sl][:, ch, hf * 512:(hf + 1) * 512], start=(ch == 0), stop=(ch == 7)),
                               r=[bWt[w3], bVb[sl]], w=[bps[6 + hf]])

                def do_acc():
                    for hf in range(2):
                        V_(lambda e, hf=hf: e.tensor_tensor(out=acc[:, tt, hf * 512:(hf + 1) * 512], in0=acc[:, tt, hf * 512:(hf + 1) * 512], in1=ps[6 + hf][:, :], op=ALU.add),
                           r=[bps[6 + hf], bacc[tt]], w=[bacc[tt]])

                deferred['wt'] = do_wt
                deferred['acc'] = do_acc

            if NBX > 0:
                load_V(0)
                for ch in range(8):
                    act_chunk(0, ch)
            uidx = 0
            for bi in range(NBX):
                for tt in range(GT):
                    unit(bi, tt, uidx)
                    uidx += 1
                    if tt == min(1, GT - 1) and bi + 1 < NBX:
                        load_V(bi + 1)
                    if tt == min(2, GT - 1) and bi + 2 < NBX:
                        load_U(bi + 2)
            if 'wt' in deferred:
                deferred.pop('wt')()
            if 'acc' in deferred:
                deferred.pop('acc')()
            for tt in range(GT):
                ti = grp * GT + tt
                import os as _os2
                if _os2.environ.get("KDBG", "") in ("ff", "rt"):
                    pass
                elif _os2.environ.get("KDBG", "") == "x1":
                    V_(lambda e, tt=tt: e.tensor_copy(out=acc[:, tt, :], in_=x1g[:, tt, :]), r=[bacc[tt], bx1g[tt]], w=[bacc[tt]])
                else:
                    V_(lambda e, tt=tt: e.tensor_tensor(out=acc[:, tt, :], in0=acc[:, tt, :], in1=GT2[:], op=ALU.mult), r=[bacc[tt], bGT2], w=[bacc[tt]])
                    V_(lambda e, tt=tt: e.tensor_tensor(out=acc[:, tt, :], in0=acc[:, tt, :], in1=x1g[:, tt, :], op=ALU.add), r=[bacc[tt], bx1g[tt]], w=[bacc[tt]])
                if _os2.environ.get('KDBG', '') not in ('ff', 'rt'):
                    A_(lambda e, tt=tt: e.activation(out=x1g[:, tt, :], in_=acc[:, tt, :], func=AF.Square, accum_out=pst[:, 4:5]), r=[bacc[tt]], w=[bx1g[tt], bpst])
                    A_(lambda e: e.activation(out=pst[:, 5:6], in_=pst[:, 4:5], func=AF.Sqrt, bias=epsc[:, 0:1], scale=1.0 / 1024), r=[bpst, bepsc], w=[bpst])
                    V_(lambda e: e.reciprocal(out=pst[:, 6:7], in_=pst[:, 5:6]), r=[bpst], w=[bpst])
                    V_(lambda e, tt=tt: e.scalar_tensor_tensor(out=acc[:, tt, :], in0=acc[:, tt, :], scalar=pst[:, 6:7], in1=FNW[:], op0=ALU.mult, op1=ALU.mult),
                       r=[bacc[tt], bpst, bFNW], w=[bacc[tt]])
                D_(lambda e, tt=tt, ti=ti: e.dma_start(out=y[ti * 128:(ti + 1) * 128, :], in_=acc[:, tt, :]), r=[bacc[tt]], w=[by], buf=by)
            P.barrier([bUb[0], bUb[1], bVb[0], bVb[1], bWt[0], bWt[1], by] + bAg + bSp + bEb + bMb + bacc + bx1g)
    P.op("sync", lambda e: e.nop(), r=[by])
    P.emit()
    pe.close()
    top.close()
    return nc


def prep_inputs(inp, n_cores, NT):
    f = lambda a: np.ascontiguousarray(np.asarray(a, dtype=np.float32))
    x = f(inp["x"])[0]
    S = x.shape[0]
    assert S == n_cores * NT * 128
    NPREV = (n_cores - 1) * NT
    w_in = f(inp["w_in"])[0]
    w_tok = np.ascontiguousarray(np.concatenate([w_in[:, 0:1024], w_in[:, 2560:3072]], axis=1))
    w_qkv = np.ascontiguousarray(w_in[:, 1024:2560])
    w_ab = np.zeros((1024, 64), np.float32)
    w_ab[:, 0:4] = w_in[:, 3072:3076]
    w_ab[:, 32:36] = w_in[:, 3076:3080]
    rep = lambda v, n=128: np.ascontiguousarray(np.broadcast_to(v[None, :], (n, v.shape[0])))
    gm_ws = f(inp["gm_ws"])[0]
    wsT = np.ascontiguousarray(gm_ws.transpose(2, 0, 1).reshape(128, 8 * 128))
    gm_bs = f(inp["gm_bs"])[0]
    bs_bc = np.ascontiguousarray(np.repeat(gm_bs.T[:, :, None], 64, axis=2).reshape(128, 512))
    conv_w = f(inp["conv_w"])[0]
    convw = np.ascontiguousarray(conv_w.reshape(4, 12, 128).transpose(2, 1, 0).reshape(128, 48))
    gpar = np.zeros((128, 8), np.float32)
    gpar[:, 0:4] = f(inp["a_log"])[0][None, :]
    gpar[:, 4:8] = f(inp["dt_bias"])[0][None, :]
    k1 = f(inp["peer_keys1"])[0]
    k2 = f(inp["peer_keys2"])[0]
    keysT = np.zeros((128, 16, 128), np.float32)
    for h in range(8):
        keysT[:, 2 * h, :] = k1[h].T
        keysT[:, 2 * h + 1, :] = k2[h].T
    keysT = keysT.reshape(128, 16 * 128)
    UT = np.ascontiguousarray(f(inp["peer_u"])[0].T)
    Vv = f(inp["peer_v"])[0]
    c = f(inp["c"])[0]
    shared = {
        "c_col": np.ascontiguousarray(c.reshape(8, 128).T),
        "w_ada": f(inp["w_ada"])[0], "b_ada": f(inp["b_ada"])[0][None, :],
        "n1w": rep(f(inp["norm1_w"])[0]), "n2w": rep(f(inp["norm2_w"])[0]), "fnw": rep(f(inp["final_norm_w"])),
        "gmnw": rep(f(inp["gm_norm_w"])[0]), "onw": rep(f(inp["o_norm_w"])[0]),
        "w_tok": w_tok, "w_qkv": w_qkv, "w_ab": w_ab, "wsT": wsT, "bs_bc": bs_bc, "convw": convw, "gpar": gpar,
        "w_out": f(inp["w_out"])[0], "wq": f(inp["peer_wq"])[0], "keysT": keysT, "UT": UT, "V": Vv,
        "consts": make_consts(),
    }
    maps = []
    for ci in range(n_cores):
        m = dict(shared)
        s0 = ci * NT * 128
        m["x_own"] = np.ascontiguousarray(x[s0:s0 + NT * 128])
        xp = np.zeros((max(NPREV, 1) * 128, 1024), np.float32)
        pm = np.zeros((128, max(NPREV, 1)), np.float32)
        if NPREV > 0:
            lo = s0 - NPREV * 128
            for t in range(NPREV):
                g0 = lo + t * 128
                if g0 >= 0:
                    xp[t * 128:(t + 1) * 128] = x[g0:g0 + 128]
                    pm[:, t] = 1.0
        m["x_prev"] = xp
        m["pmask"] = pm
        maps.append(m)
    return maps


_CACHE = {}


def kernel(**inputs):
    n_cores, NT = 8, 16
    maps = prep_inputs(inputs, n_cores, NT)
    key = (n_cores, NT)
    if key not in _CACHE:
        _CACHE[key] = build(n_cores, NT)
    nc = _CACHE[key]
    res = run_bass_kernel_spmd(nc, maps, core_ids=list(range(n_cores)))
    out = np.concatenate([np.asarray(r["y"], dtype=np.float32) for r in res.results], axis=0)
    return out[None, :, :].astype(np.float32)
```

```python
import numpy as np
from contextlib import ExitStack
import concourse.bass as bass
import concourse.mybir as mybir
from concourse.bass_utils import run_bass_kernel_spmd

F32 = mybir.dt.float32
BF16 = mybir.dt.bfloat16
ALU = mybir.AluOpType
AF = mybir.ActivationFunctionType

ENGS = ["sync", "scalar", "vector", "gpsimd", "tensor"]
SEM_CHUNK = 4000
EPS = 1e-6


class Buf:
    __slots__ = ("name", "w", "r", "dsem", "dcnt", "excl")

    def __init__(self, name, excl=False):
        self.excl = excl
        self.name = name
        self.w = None
        self.r = {}
        self.dsem = None
        self.dcnt = 0


class Op:
    __slots__ = ("eng", "fn", "deps", "dma", "sig", "need", "dbuf", "dval", "inc")

    def __init__(self, eng, fn, dma):
        self.eng = eng
        self.fn = fn
        self.deps = []
        self.dma = dma
        self.sig = None
        self.need = False
        self.dbuf = None
        self.dval = 0
        self.inc = 16


class Prog:
    def __init__(self, nc):
        self.nc = nc
        self.q = {e: [] for e in ENGS}
        self.dma_bufs = []
        self.last = {e: None for e in ENGS}

    def op(self, eng, fn, r=(), w=(), dma_buf=None, extra=()):
        deps = list(extra)
        for b in r:
            if b.w is not None:
                deps.append(b.w)
            if b.excl:
                for o in b.r.values():
                    if o.eng != eng:
                        deps.append(o)
        for b in w:
            if b.w is not None and not (dma_buf is not None and b.w.dma and b.w.dbuf is dma_buf):
                deps.append(b.w)
            for o in b.r.values():
                deps.append(o)
        dma = dma_buf is not None
        rec = Op(eng, fn, dma)
        seen = set()
        for d in deps:
            if id(d) in seen:
                continue
            seen.add(id(d))
            if (not d.dma) and d.eng == eng:
                if eng == "tensor":
                    continue
                if not any(b.w is d for b in r) and d not in extra:
                    continue
            d.need = True
            rec.deps.append(d)
        if dma:
            if dma_buf.dsem is None:
                dma_buf.dsem = len(self.dma_bufs)
                self.dma_bufs.append(dma_buf)
            dma_buf.dcnt += 16
            rec.dbuf = dma_buf
            rec.dval = dma_buf.dcnt
        self.q[eng].append(rec)
        if not dma:
            self.last[eng] = rec
        for b in r:
            b.r[eng if not dma else ("dma", id(rec))] = rec
        for b in w:
            b.w = rec
            b.r = {}
        return rec

    def barrier(self, bufs):
        deps = [o for o in self.last.values() if o is not None]
        for b in bufs:
            if b.w is not None:
                deps.append(b.w)
            deps.extend(b.r.values())
        for e in ENGS:
            self.op(e, lambda en: en.nop(), extra=[d for d in deps if not (d.eng == e and not d.dma)])

    def emit(self):
        nc = self.nc
        nsig = {}
        for e in ENGS:
            c = 0
            for rec in self.q[e]:
                if rec.dma:
                    continue
                if rec.need:
                    c += 1
                    rec.sig = c
            nsig[e] = c
        with ExitStack() as es:
            esems = {}
            for e in ENGS:
                n = (nsig[e] + SEM_CHUNK - 1) // SEM_CHUNK
                esems[e] = [es.enter_context(nc.semaphore(f"s_{e}_{i}")) for i in range(n)]
            dsems = [es.enter_context(nc.semaphore(f"d_{i}")) for i in range(len(self.dma_bufs))]
            block = es.enter_context(nc.Block())
            prog = self

            def make(e):
                def body(eng):
                    known = {}
                    maxchunk = {}
                    for rec in prog.q[e]:
                        for d in rec.deps:
                            if d.dma:
                                key = ("d", d.dbuf.dsem)
                                val = d.dval
                                sem = dsems[d.dbuf.dsem]
                            else:
                                ci = (d.sig - 1) // SEM_CHUNK
                                if maxchunk.get(d.eng, -1) > ci:
                                    continue
                                key = (d.eng, ci)
                                val = d.sig - ci * SEM_CHUNK
                                sem = esems[d.eng][ci]
                            if known.get(key, 0) >= val:
                                continue
                            eng.wait_ge(sem, val)
                            known[key] = val
                            if not d.dma:
                                maxchunk[d.eng] = max(maxchunk.get(d.eng, -1), ci)
                        ins = rec.fn(eng)
                        if rec.dma:
                            ins.then_inc(dsems[rec.dbuf.dsem], 16)
                        elif rec.need:
                            ci = (rec.sig - 1) // SEM_CHUNK
                            ins.then_inc(esems[e][ci], 1)
                return body

            block.sync(make("sync"))
            block.scalar(make("scalar"))
            block.vector(make("vector"))
            block.gpsimd(make("gpsimd"))
            block.tensor(make("tensor"))


def run_interleaved(gens):
    active = list(gens)
    while active:
        for g in list(active):
            try:
                next(g)
            except StopIteration:
                active.remove(g)


C_ID, C_TRIU, C_BLK, C_NEGU, C_SU, C_CAUS, C_SEL, C_ONES, C_N = 0, 128, 256, 384, 512, 640, 768, 1280, 1408


def make_consts():
    c = np.zeros((128, C_N), np.float32)
    idx = np.arange(128)
    j = idx[:, None]
    i = idx[None, :]
    same = (j // 64) == (i // 64)
    c[:, C_ID:C_ID + 128] = np.eye(128)
    c[:, C_TRIU:C_TRIU + 128] = ((i >= j) & same)
    c[:, C_BLK:C_BLK + 128] = same
    c[:, C_NEGU:C_NEGU + 128] = np.where((i >= j) & same, 0.0, -30000.0)
    c[:, C_SU:C_SU + 128] = ((i > j) & same)
    c[:, C_CAUS:C_CAUS + 128] = (j <= i)
    for h in range(4):
        c[32 + h, C_SEL + h * 128:C_SEL + (h + 1) * 128] = 1.0
    c[:, C_ONES:C_ONES + 128] = 1.0
    return c


def build(n_cores, NT, dbg=False, phase=9):
    NPREV = (n_cores - 1) * NT
    NTOK = NT * 128
    GT = min(4, NT)
    NG = NT // GT
    NB = 16
    nc = bass.Bass("TRN2", target_bir_lowering=False)

    def din(name, shape, dt=F32):
        return nc.dram_tensor(name, shape, dt, kind="ExternalInput").ap()

    x_own = din("x_own", [NTOK, 1024])
    x_prev = din("x_prev", [max(NPREV, 1) * 128, 1024])
    pmask = din("pmask", [128, max(NPREV, 1)])
    c_col = din("c_col", [128, 8])
    w_ada = din("w_ada", [1024, 6144])
    b_ada = din("b_ada", [1, 6144])
    n1w = din("n1w", [128, 1024])
    n2w = din("n2w", [128, 1024])
    fnw = din("fnw", [128, 1024])
    gmnw = din("gmnw", [128, 512])
    onw = din("onw", [128, 128])
    w_tok = din("w_tok", [1024, 1536])
    w_qkv = din("w_qkv", [1024, 1536])
    w_ab = din("w_ab", [1024, 64])
    wsT = din("wsT", [128, 8 * 128])
    bs_bc = din("bs_bc", [128, 512])
    convw = din("convw", [128, 48])
    gpar = din("gpar", [128, 8])
    w_out = din("w_out", [1024, 1024])
    wq = din("wq", [1024, 2048])
    keysT = din("keysT", [128, 16 * 128])
    UT = din("UT", [1024, 16384])
    V = din("V", [16384, 1024])
    consts = din("consts", [128, C_N])
    y = nc.dram_tensor("y", [NTOK, 1024], F32, kind="ExternalOutput").ap()
    x1s = nc.dram_tensor("x1s", [NTOK, 1024], F32).ap()
    dbg_t = nc.dram_tensor("dbg", [NTOK, 2048], F32, kind="ExternalOutput").ap() if dbg else None

    P = Prog(nc)
    top = ExitStack()

    _names = {}

    def sbt(es, name, shape, dt=F32):
        n = _names.get(name, 0)
        _names[name] = n + 1
        if n:
            name = f"{name}_{n}"
        return es.enter_context(nc.sbuf_tensor(name, shape, dt))

    ps = [top.enter_context(nc.psum_tensor(f"ps{i}", [128, 512], F32)) for i in range(8)]
    bps = [Buf(f"ps{i}", excl=True) for i in range(8)]

    cst = sbt(top, "cst", [128, C_N]); bcst = Buf("cst")
    idb = sbt(top, "idb", [128, 128], BF16); bidb = Buf("idb")
    modscr = nc.dram_tensor("modscr", [3 * 128, 1024], F32).ap()
    bmodscr = Buf("modscr")
    epsc = sbt(top, "epsc", [128, 1]); bepsc = Buf("epsc")
    by = Buf("y"); bx1s = Buf("x1s"); bdbg = Buf("dbg")

    ident = cst[:, C_ID:C_ID + 128]
    triU = cst[:, C_TRIU:C_TRIU + 128]
    blk = cst[:, C_BLK:C_BLK + 128]
    negU = cst[:, C_NEGU:C_NEGU + 128]
    sU = cst[:, C_SU:C_SU + 128]
    caus = cst[:, C_CAUS:C_CAUS + 128]
    ones = cst[:, C_ONES:C_ONES + 128]

    def V_(fn, r=(), w=()):
        return P.op("vector", fn, r, w)

    def A_(fn, r=(), w=()):
        return P.op("scalar", fn, r, w)

    def G_(fn, r=(), w=()):
        return P.op("gpsimd", fn, r, w)

    def T_(fn, r=(), w=()):
        return P.op("tensor", fn, r, w)

    def D_(fn, r=(), w=(), buf=None, eng="sync"):
        return P.op(eng, fn, r, w, dma_buf=buf)

    def bc3(ap2d, n, axis):
        k = ap2d.shape[1]
        if axis == 2:
            return ap2d.unsqueeze(2).to_broadcast([ap2d.shape[0], k, n])
        return ap2d.unsqueeze(1).to_broadcast([ap2d.shape[0], n, k])

    D_(lambda e: e.dma_start(out=cst[:], in_=consts[:, :]), w=[bcst], buf=bcst)
    V_(lambda e: e.tensor_copy(out=idb[:], in_=ident), r=[bcst], w=[bidb])
    V_(lambda e: e.memset(epsc[:], EPS), w=[bepsc])

    mx = ExitStack()
    A1 = sbt(mx, "A1", [128, 1024]); B1 = sbt(mx, "B1", [128, 1024]); GT1 = sbt(mx, "GT1", [128, 1024])
    bA1, bB1, bGT1 = Buf("A1"), Buf("B1"), Buf("GT1")
    wtok = sbt(mx, "wtok", [128, 8, 1536], BF16); bwtok = Buf("wtok")
    wout = sbt(mx, "wout", [128, 8, 1024], BF16); bwout = Buf("wout")
    wqkv = sbt(mx, "wqkv", [128, 8, 1536], BF16); bwqkv = Buf("wqkv")
    wab = sbt(mx, "wab", [128, 8, 64], BF16); bwab = Buf("wab")
    wsm = sbt(mx, "wsm", [128, 8, 128]); bwsm = Buf("wsm")
    bsb = sbt(mx, "bsb", [128, 512]); bbsb = Buf("bsb")
    gmn = sbt(mx, "gmn", [128, 512]); bgmn = Buf("gmn")
    onb = sbt(mx, "onb", [128, 128]); bonb = Buf("onb")
    cw = sbt(mx, "cw", [128, 12, 4]); bcw = Buf("cw")
    gp = sbt(mx, "gp", [128, 12]); bgp = Buf("gp")

    with ExitStack() as su:
        ccl = sbt(su, "ccl", [128, 8]); bccl = Buf("ccl")
        scl = sbt(su, "scl", [128, 8]); bscl = Buf("scl")
        wad = [sbt(su, f"wad{i}", [128, 8, 512]) for i in range(2)]
        bwad = [Buf(f"wad{i}") for i in range(2)]
        bad = sbt(su, "bad", [1, 6144]); bbad = Buf("bad")
        tn1 = sbt(su, "tn1", [128, 1024]); btn1 = Buf("tn1")
        tn2 = sbt(su, "tn2", [128, 1024]); btn2 = Buf("tn2")
        D_(lambda e: e.dma_start(out=ccl[:], in_=c_col[:, :]), w=[bccl], buf=bccl)
        D_(lambda e: e.dma_start(out=bad[:], in_=b_ada[:, :]), w=[bbad], buf=bbad)
        A_(lambda e: e.activation(out=scl[:], in_=ccl[:], func=AF.Silu), r=[bccl], w=[bscl])
        D_(lambda e: e.dma_start(out=wqkv[:], in_=w_qkv.rearrange("(kc p) n -> p kc n", p=128)), w=[bwqkv], buf=bwqkv, eng="gpsimd")
        D_(lambda e: e.dma_start(out=wab[:], in_=w_ab.rearrange("(kc p) n -> p kc n", p=128)), w=[bwab], buf=bwab, eng="gpsimd")
        D_(lambda e: e.dma_start(out=wtok[:], in_=w_tok.rearrange("(kc p) n -> p kc n", p=128)), w=[bwtok], buf=bwtok, eng="gpsimd")
        D_(lambda e: e.dma_start(out=wout[:], in_=w_out.rearrange("(kc p) n -> p kc n", p=128)), w=[bwout], buf=bwout, eng="gpsimd")
        D_(lambda e: e.dma_start(out=wsm[:], in_=wsT.rearrange("p (h t) -> p h t", h=8)), w=[bwsm], buf=bwsm)
        D_(lambda e: e.dma_start(out=bsb[:], in_=bs_bc[:, :]), w=[bbsb], buf=bbsb)
        D_(lambda e: e.dma_start(out=gmn[:], in_=gmnw[:, :]), w=[bgmn], buf=bgmn)
        D_(lambda e: e.dma_start(out=onb[:], in_=onw[:, :]), w=[bonb], buf=bonb)
        D_(lambda e: e.dma_start(out=cw[:], in_=convw.rearrange("p (c k) -> p c k", k=4)), w=[bcw], buf=bcw)
        D_(lambda e: e.dma_start(out=gp[:, 0:8], in_=gpar[:, :]), w=[bgp], buf=bgp)
        V_(lambda e: e.tensor_tensor(out=wsm[:], in0=wsm[:], in1=bc3(caus, 8, 1), op=ALU.mult), r=[bwsm, bcst], w=[bwsm])
        A_(lambda e: e.activation(out=gp[:, 8:12], in_=gp[:, 0:4], func=AF.Exp), r=[bgp], w=[bgp])
        V_(lambda e: e.tensor_scalar(out=gp[:, 8:12], in0=gp[:, 8:12], scalar1=-1.0, scalar2=None, op0=ALU.mult), r=[bgp], w=[bgp])
        dests = [(B1, bB1), (None, None), (GT1, bGT1), (tn2, btn2), (None, None), (tn2, btn2)]
        scr_row = {3: 1, 4: 0, 5: 2}
        for j in range(12):
            sl = j % 2
            D_(lambda e, j=j, sl=sl: e.dma_start(out=wad[sl][:], in_=w_ada[:, j * 512:(j + 1) * 512].rearrange("(kc p) n -> p kc n", p=128)),
               w=[bwad[sl]], buf=bwad[sl])
            pb = j % 2
            for kc in range(8):
                T_(lambda e, kc=kc, sl=sl, pb=pb: e.matmul(ps[pb][:, :], lhsT=scl[:, kc:kc + 1].to_broadcast([128, 128]), rhs=wad[sl][:, kc, :],
                                                           start=(kc == 0), stop=False), r=[bscl, bwad[sl]], w=[bps[pb]])
            T_(lambda e, j=j, pb=pb: e.matmul(ps[pb][:, :], lhsT=ones[0:1, :], rhs=bad[0:1, j * 512:(j + 1) * 512], start=False, stop=True),
               r=[bcst, bbad], w=[bps[pb]])
            vec, half = j // 2, j % 2
            cs = slice(half * 512, (half + 1) * 512)
            if vec in (1, 4):
                dst, bdst, nw = (A1, bA1, n1w) if vec == 1 else (tn2, btn2, n2w)
                if half == 0:
                    D_(lambda e, nw=nw: e.dma_start(out=tn1[:], in_=nw[:, :]), w=[btn1], buf=btn1)
                V_(lambda e, dst=dst, cs=cs, pb=pb: e.scalar_tensor_tensor(out=dst[:, cs], in0=ps[pb][:, :], scalar=1.0, in1=tn1[:, cs],
                                                                         op0=ALU.add, op1=ALU.mult), r=[bps[pb], btn1], w=[bdst])
            else:
                dst, bdst = dests[vec]
                A_(lambda e, dst=dst, cs=cs, pb=pb: e.activation(out=dst[:, cs], in_=ps[pb][:, :], func=AF.Copy), r=[bps[pb]], w=[bdst])
            if vec >= 3 and half == 1:
                rr0 = scr_row[vec] * 128
                D_(lambda e, rr0=rr0: e.dma_start(out=modscr[rr0:rr0 + 128, :], in_=tn2[:]), r=[btn2], w=[bmodscr], buf=bmodscr)
        P.barrier([bwad[0], bwad[1], btn1, btn2, bbad, bscl, bccl, bmodscr])

    if phase == 0:
        P.op("sync", lambda e: e.nop())
        P.emit(); mx.close(); top.close()
        return nc
    NBUF = 2
    xt = [sbt(mx, f"xt{i}", [128, 1024]) for i in range(NBUF)]; bxt = [Buf(f"xt{i}") for i in range(NBUF)]
    pmk = sbt(mx, "pmk", [128, max(NPREV, 1)]); bpmk = Buf("pmk")
    junk = sbt(mx, "junk", [128, 1024]); bjunk = Buf("junk")
    junk2 = sbt(mx, "junk2", [128, 512]); bjunk2 = Buf("junk2")
    st = [sbt(mx, f"st{i}", [128, 8]) for i in range(NBUF)]; bst = [Buf(f"st{i}") for i in range(NBUF)]
    hb = [sbt(mx, f"hb{i}", [128, 1024], BF16) for i in range(NBUF)]; bhb = [Buf(f"hb{i}") for i in range(NBUF)]
    hT = [sbt(mx, f"hT{i}", [128, 8, 128], BF16) for i in range(NBUF)]; bhT = [Buf(f"hT{i}") for i in range(NBUF)]
    cin = sbt(mx, "cin", [128, 12, 131]); bcin = Buf("cin")
    cacc = sbt(mx, "cacc", [128, 12, 128]); bcacc = Buf("cacc")
    bcaccs = [Buf(f"cacc_c{i}") for i in range(12)]
    qkv = cacc; bqkv = bcacc
    sq = sbt(mx, "sq", [128, 8, 128]); bsq = Buf("sq")
    rs = sbt(mx, "rs", [128, 8, 128]); brs = Buf("rs")
    abr = sbt(mx, "abr", [128, 16]); babr = Buf("abr")
    gbt = [sbt(mx, f"gbt{i}", [128, 24]) for i in range(NBUF)]; bgbt = [Buf(f"gbt{i}") for i in range(NBUF)]
    kT = [sbt(mx, f"kT{i}", [128, 4, 128]) for i in range(NBUF)]; bkT = [Buf(f"kT{i}") for i in range(NBUF)]
    kdec = [sbt(mx, f"kdec{i}", [128, 4, 128], BF16) for i in range(NBUF)]; bkdec = [Buf(f"kdec{i}") for i in range(NBUF)]
    ck = [sbt(mx, f"ck{i}", [128, 4, 128], BF16) for i in range(NBUF)]; bck = [Buf(f"ck{i}") for i in range(NBUF)]
    vb = [sbt(mx, f"vb{i}", [128, 4, 128], BF16) for i in range(NBUF)]; bvb = [Buf(f"vb{i}") for i in range(NBUF)]
    qdT = [sbt(mx, f"qdT{i}", [128, 4, 128]) for i in range(NBUF)]; bqdT = [Buf(f"qdT{i}") for i in range(NBUF)]
    LT = [sbt(mx, f"LT{i}", [128, 4, 128], BF16) for i in range(NBUF)]; bLT = [Buf(f"LT{i}") for i in range(NBUF)]
    Erow = [sbt(mx, f"Erow{i}", [128, 4, 128]) for i in range(NBUF)]; bErow = [Buf(f"Erow{i}") for i in range(NBUF)]
    DTi = sbt(mx, "DTi", [128, 4, 128]); bDTi = Buf("DTi")
    BU = sbt(mx, "BU", [128, 4, 128]); bBU = Buf("BU")
    atT = [sbt(mx, f"atT{i}", [128, 4, 128]) for i in range(NBUF)]; batT = [Buf(f"atT{i}") for i in range(NBUF)]
    Pm = [sbt(mx, f"Pm{i}", [128, 4, 128], BF16) for i in range(2)]; bPm = [Buf(f"Pm{i}") for i in range(2)]
    PTm = [sbt(mx, f"PTm{i}", [128, 4, 128], BF16) for i in range(2)]; bPTm = [Buf(f"PTm{i}") for i in range(2)]
    Xm = [sbt(mx, f"Xm{i}", [128, 4, 128], BF16) for i in range(2)]; bXm = [Buf(f"Xm{i}") for i in range(2)]
    A1m = sbt(mx, "A1m", [128, 4, 128], BF16); bA1m = Buf("A1m")
    U1m = sbt(mx, "U1m", [128, 4, 128], BF16); bU1m = Buf("U1m")
    MT = [sbt(mx, f"MT{i}", [128, 4, 128]) for i in range(2)]; bMT = [Buf(f"MT{i}") for i in range(2)]
    Cm = [sbt(mx, f"Cm{i}", [128, 4, 128]) for i in range(2)]; bCm = [Buf(f"Cm{i}") for i in range(2)]
    eI = sbt(mx, "eI", [128, 4, 128]); beI = Buf("eI")
    Sst = [sbt(mx, f"Sst{i}", [128, 4, 128]) for i in range(2)]; bSst = [Buf(f"Sst{i}") for i in range(2)]
    rr = sbt(mx, "rr", [128, 4, 128], BF16); brr = Buf("rr")
    vn = sbt(mx, "vn", [128, 4, 128]); bvn = Buf("vn")
    osb = sbt(mx, "osb", [128, 4, 128]); bosb = Buf("osb")
    uu = sbt(mx, "uu", [128, 512]); buu = Buf("uu")
    vv = sbt(mx, "vv", [128, 512]); bvv = Buf("vv")
    zz = sbt(mx, "zz", [128, 512]); bzz = Buf("zz")
    mix = sbt(mx, "mix", [128, 1024], BF16); bmix = Buf("mix")
    mixT = sbt(mx, "mixT", [128, 8, 128], BF16); bmixT = Buf("mixT")
    x1t = sbt(mx, "x1t", [128, 1024]); bx1t = Buf("x1t")

    if NPREV > 0:
        D_(lambda e: e.dma_start(out=pmk[:], in_=pmask[:, :]), w=[bpmk], buf=bpmk)
    G_(lambda e: e.memset(cin[:], 0.0), w=[bcin])
    G_(lambda e: e.memset(Sst[0][:], 0.0), w=[bSst[0]])
    scur = [0]
    mtc = [0]

    def stage1(ti, own, g):
        b = g % NBUF
        need_q = own or (ti == NPREV - 1)
        c0 = 0 if need_q else 4
        src = x_own[ti * 128:(ti + 1) * 128, :] if own else x_prev[ti * 128:(ti + 1) * 128, :]
        D_(lambda e: e.dma_start(out=xt[b][:], in_=src), w=[bxt[b]], buf=bxt[b])
        yield
        A_(lambda e: e.activation(out=junk[:], in_=xt[b][:], func=AF.Square, accum_out=st[b][:, 0:1]), r=[bxt[b]], w=[bjunk, bst[b]])
        A_(lambda e: e.activation(out=st[b][:, 1:2], in_=st[b][:, 0:1], func=AF.Ln, bias=epsc[:, 0:1], scale=1.0 / 1024), r=[bst[b], bepsc], w=[bst[b]])
        A_(lambda e: e.activation(out=st[b][:, 2:3], in_=st[b][:, 1:2], func=AF.Exp, scale=-0.5), r=[bst[b]], w=[bst[b]])
        yield
        V_(lambda e: e.scalar_tensor_tensor(out=junk[:], in0=xt[b][:], scalar=st[b][:, 2:3], in1=A1[:], op0=ALU.mult, op1=ALU.mult),
           r=[bxt[b], bst[b], bA1], w=[bjunk])
        if own:
            V_(lambda e: e.tensor_tensor(out=hb[b][:], in0=junk[:], in1=B1[:], op=ALU.add), r=[bjunk, bB1], w=[bhb[b]])
        else:
            V_(lambda e: e.scalar_tensor_tensor(out=hb[b][:], in0=B1[:], scalar=pmk[:, ti:ti + 1], in1=junk[:], op0=ALU.mult, op1=ALU.add),
               r=[bjunk, bB1, bpmk], w=[bhb[b]])
        yield
        for kc in range(8):
            T_(lambda e, kc=kc: e.matmul(ps[kc // 4][:, (kc % 4) * 128:(kc % 4 + 1) * 128],
                                         lhsT=hb[b][:, kc * 128:(kc + 1) * 128], rhs=idb[:], start=True, stop=True), r=[bhb[b], bidb], w=[bps[kc // 4]])
        A_(lambda e: e.activation(out=hT[b][:, 0:4, :], in_=ps[0][:, :].rearrange("p (a c) -> p a c", a=4), func=AF.Copy), r=[bps[0]], w=[bhT[b]])
        V_(lambda e: e.tensor_copy(out=hT[b][:, 4:8, :], in_=ps[1][:, :].rearrange("p (a c) -> p a c", a=4)), r=[bps[1]], w=[bhT[b]])
        yield
        for ch in range(c0, 12):
            bank = ch // 4
            for kc in range(8):
                T_(lambda e, ch=ch, kc=kc, bank=bank: e.matmul(ps[bank][:, (ch % 4) * 128:(ch % 4 + 1) * 128], lhsT=wqkv[:, kc, ch * 128:(ch + 1) * 128],
                                                              rhs=hT[b][:, kc, :], start=(kc == 0), stop=(kc == 7)), r=[bwqkv, bhT[b]], w=[bps[bank]])
            if ch % 4 == 3:
                eng = A_ if bank != 1 else V_
                if bank != 1:
                    A_(lambda e, bank=bank: e.activation(out=cin[:, bank * 4:(bank + 1) * 4, 3:131], in_=ps[bank][:, :].rearrange("p (a c) -> p a c", a=4), func=AF.Copy),
                       r=[bps[bank]], w=[bcin])
                else:
                    V_(lambda e, bank=bank: e.tensor_copy(out=cin[:, bank * 4:(bank + 1) * 4, 3:131], in_=ps[bank][:, :].rearrange("p (a c) -> p a c", a=4)),
                       r=[bps[bank]], w=[bcin])
                yield
        for kc in range(8):
            T_(lambda e, kc=kc: e.matmul(ps[3][:, 0:64], lhsT=hT[b][:, kc, :], rhs=wab[:, kc, :], start=(kc == 0), stop=(kc == 7)), r=[bwab, bhT[b]], w=[bps[3]])
        V_(lambda e: e.tensor_tensor(out=abr[:, 0:4], in0=ps[3][:, 0:4], in1=gp[:, 4:8], op=ALU.add), r=[bps[3], bgp], w=[babr])
        A_(lambda e: e.activation(out=abr[:, 0:4], in_=abr[:, 0:4], func=AF.Exp), r=[babr], w=[babr])
        A_(lambda e: e.activation(out=abr[:, 4:8], in_=ps[3][:, 32:36], func=AF.Exp, scale=-1.0), r=[bps[3]], w=[babr])
        A_(lambda e: e.activation(out=abr[:, 8:12], in_=abr[:, 0:4], func=AF.Ln, bias=1.0, scale=1.0), r=[babr], w=[babr])
        V_(lambda e: e.tensor_tensor(out=gbt[b][:, 0:4], in0=abr[:, 8:12], in1=gp[:, 8:12], op=ALU.mult), r=[babr, bgp], w=[bgbt[b]])
        V_(lambda e: e.tensor_scalar(out=abr[:, 12:16], in0=abr[:, 4:8], scalar1=1.0, scalar2=None, op0=ALU.add), r=[babr], w=[babr])
        V_(lambda e: e.reciprocal(out=gbt[b][:, 4:8], in_=abr[:, 12:16]), r=[babr], w=[bgbt[b]])
        yield
        for k in range(4):
            for ch in range(c0, 12):
                if k == 0:
                    V_(lambda e, ch=ch: e.tensor_scalar(out=cacc[:, ch, :], in0=cin[:, ch, 0:128], scalar1=cw[:, ch, 0:1], scalar2=None, op0=ALU.mult),
                       r=[bcin, bcw], w=[bcaccs[ch], bcacc])
                else:
                    V_(lambda e, ch=ch, k=k: e.scalar_tensor_tensor(out=cacc[:, ch, :], in0=cin[:, ch, k:k + 128], scalar=cw[:, ch, k:k + 1], in1=cacc[:, ch, :],
                                                                    op0=ALU.mult, op1=ALU.add), r=[bcin, bcw, bcaccs[ch]], w=[bcaccs[ch]])
            yield
        G_(lambda e: e.tensor_copy(out=cin[:, :, 0:3], in_=cin[:, :, 128:131]), r=[bcin], w=[bcin])
        A_(lambda e: e.activation(out=qkv[:, c0:12, :], in_=cacc[:, c0:12, :], func=AF.Silu), r=[bcacc] + bcaccs[c0:12], w=[bcacc])
        yield
        A_(lambda e: e.activation(out=sq[:, c0:8, :], in_=qkv[:, c0:8, :], func=AF.Square), r=[bqkv], w=[bsq])
        for hf in range(c0 // 4, 2):
            T_(lambda e, hf=hf: e.matmul(ps[hf][:, :], lhsT=ones, rhs=sq[:, hf * 4:(hf + 1) * 4, :].rearrange("p a c -> p (a c)"), start=True, stop=True),
               r=[bcst, bsq], w=[bps[hf]])
            A_(lambda e, hf=hf: e.activation(out=rs[:, hf * 4:(hf + 1) * 4, :].rearrange("p a c -> p (a c)"), in_=ps[hf][:, :], func=AF.Ln, bias=epsc[:, 0:1], scale=1.0),
               r=[bps[hf], bepsc], w=[brs])
        A_(lambda e: e.activation(out=rs[:, c0:8, :], in_=rs[:, c0:8, :], func=AF.Exp, scale=-0.5), r=[brs], w=[brs])
        yield
        T_(lambda e: e.matmul(ps[2][:, 8:12], lhsT=triU, rhs=gbt[b][:, 0:4], start=True, stop=True), r=[bcst, bgbt[b]], w=[bps[2]])
        T_(lambda e: e.matmul(ps[2][:, 12:16], lhsT=blk, rhs=gbt[b][:, 0:4], start=True, stop=True), r=[bcst, bgbt[b]], w=[bps[2]])
        V_(lambda e: e.tensor_copy(out=gbt[b][:, 8:16], in_=ps[2][:, 8:16]), r=[bps[2]], w=[bgbt[b]])
        for h in range(4):
            T_(lambda e, h=h: e.matmul(ps[3][:, h * 128:(h + 1) * 128], lhsT=gbt[b][:, h:h + 1].to_broadcast([128, 128]), rhs=triU, start=True, stop=True),
               r=[bgbt[b], bcst], w=[bps[3]])
        yield
        import os as _os; _sk = _os.environ.get('KSKIP', '')
        if 'cols' not in _sk:
            A_(lambda e: e.activation(out=gbt[b][:, 16:20], in_=gbt[b][:, 8:12], func=AF.Exp), r=[bgbt[b]], w=[bgbt[b]])
            V_(lambda e: e.scalar_tensor_tensor(out=gbt[b][:, 16:20], in0=gbt[b][:, 16:20], scalar=-1.0, in1=gbt[b][:, 4:8], op0=ALU.mult, op1=ALU.mult),
               r=[bgbt[b]], w=[bgbt[b]])
            V_(lambda e: e.tensor_tensor(out=gbt[b][:, 20:24], in0=gbt[b][:, 12:16], in1=gbt[b][:, 8:12], op=ALU.subtract), r=[bgbt[b]], w=[bgbt[b]])
            A_(lambda e: e.activation(out=gbt[b][:, 20:24], in_=gbt[b][:, 20:24], func=AF.Exp), r=[bgbt[b]], w=[bgbt[b]])
        if 'erow' not in _sk:
            A_(lambda e: e.activation(out=Erow[b][:].rearrange("p a c -> p (a c)"), in_=ps[3][:, :], func=AF.Exp), r=[bps[3]], w=[bErow[b]])
        if 'dti' not in _sk:
            V_(lambda e: e.tensor_tensor(out=DTi[:], in0=ps[3][:, :].rearrange("p (a c) -> p a c", a=4), in1=bc3(negU, 4, 1), op=ALU.add), r=[bps[3], bcst], w=[bDTi])
            V_(lambda e: e.tensor_tensor(out=DTi[:], in0=DTi[:], in1=bc3(gbt[b][:, 8:12], 128, 2), op=ALU.subtract), r=[bDTi, bgbt[b]], w=[bDTi])
            A_(lambda e: e.activation(out=DTi[:], in_=DTi[:], func=AF.Exp), r=[bDTi], w=[bDTi])
            yield
        for h in range(4):
            T_(lambda e, h=h: e.matmul(ps[2][:, h * 128:(h + 1) * 128], lhsT=gbt[b][:, 4 + h:5 + h].to_broadcast([128, 128]), rhs=ident, start=True, stop=True),
               r=[bcst, bgbt[b]], w=[bps[2]])
        V_(lambda e: e.tensor_tensor(out=BU[:], in0=ps[2][:, :].rearrange("p (a c) -> p a c", a=4), in1=bc3(sU, 4, 1), op=ALU.mult), r=[bps[2], bcst], w=[bBU])
        V_(lambda e: e.tensor_tensor(out=kT[b][:], in0=qkv[:, 4:8, :], in1=rs[:, 4:8, :], op=ALU.mult), r=[bqkv, brs], w=[bkT[b]])
        if own:
            V_(lambda e: e.scalar_tensor_tensor(out=qdT[b][:], in0=qkv[:, 0:4, :], scalar=128.0 ** -0.5, in1=rs[:, 0:4, :], op0=ALU.mult, op1=ALU.mult),
               r=[bqkv, brs], w=[bqdT[b]])
        yield
        for h in range(4):
            T_(lambda e, h=h: e.matmul(ps[0][:, h * 128:(h + 1) * 128], lhsT=kT[b][:, h, :], rhs=kT[b][:, h, :], start=True, stop=True), r=[bkT[b]], w=[bps[0]])
        if own:
            for h in range(4):
                T_(lambda e, h=h: e.matmul(ps[1][:, h * 128:(h + 1) * 128], lhsT=kT[b][:, h, :], rhs=qdT[b][:, h, :], start=True, stop=True), r=[bkT[b], bqdT[b]], w=[bps[1]])
        V_(lambda e: e.tensor_tensor(out=LT[b][:], in0=ps[0][:, :].rearrange("p (a c) -> p a c", a=4), in1=DTi[:], op=ALU.mult), r=[bps[0], bDTi], w=[bLT[b]])
        V_(lambda e: e.tensor_tensor(out=LT[b][:], in0=LT[b][:], in1=BU[:], op=ALU.mult), r=[bLT[b], bBU], w=[bLT[b]])
        if own:
            V_(lambda e: e.tensor_tensor(out=atT[b][:], in0=ps[1][:, :].rearrange("p (a c) -> p a c", a=4), in1=DTi[:], op=ALU.mult), r=[bps[1], bDTi], w=[batT[b]])
            G_(lambda e: e.tensor_tensor(out=qdT[b][:], in0=qdT[b][:], in1=Erow[b][:], op=ALU.mult), r=[bqdT[b], bErow[b]], w=[bqdT[b]])
        yield
        for h in range(4):
            T_(lambda e, h=h: e.matmul(ps[2][:, h * 128:(h + 1) * 128], lhsT=kT[b][:, h, :], rhs=ident, start=True, stop=True), r=[bkT[b], bcst], w=[bps[2]])
        for h in range(4):
            T_(lambda e, h=h: e.matmul(ps[3][:, h * 128:(h + 1) * 128], lhsT=qkv[:, 8 + h, :], rhs=ident, start=True, stop=True), r=[bqkv, bcst], w=[bps[3]])
        V_(lambda e: e.tensor_tensor(out=kdec[b][:], in0=ps[2][:, :].rearrange("p (a c) -> p a c", a=4), in1=bc3(gbt[b][:, 20:24], 128, 2), op=ALU.mult),
           r=[bps[2], bgbt[b]], w=[bkdec[b]])
        V_(lambda e: e.tensor_tensor(out=ck[b][:], in0=ps[2][:, :].rearrange("p (a c) -> p a c", a=4), in1=bc3(gbt[b][:, 16:20], 128, 2), op=ALU.mult),
           r=[bps[2], bgbt[b]], w=[bck[b]])
        V_(lambda e: e.tensor_tensor(out=vb[b][:], in0=ps[3][:, :].rearrange("p (a c) -> p a c", a=4), in1=bc3(gbt[b][:, 4:8], 128, 2), op=ALU.mult),
           r=[bps[3], bgbt[b]], w=[bvb[b]])
        yield

    def stage2(ti, own, g):
        b = g % NBUF
        for h in range(4):
            T_(lambda e, h=h: e.matmul(ps[4][:, h * 128:(h + 1) * 128], lhsT=LT[b][:, h, :], rhs=idb[:], start=True, stop=True), r=[bLT[b], bidb], w=[bps[4]])
        A_(lambda e: e.activation(out=Pm[0][:].rearrange("p a c -> p (a c)"), in_=ps[4][:, :], func=AF.Copy), r=[bps[4]], w=[bPm[0]])
        G_(lambda e: e.tensor_tensor(out=Xm[0][:], in0=bc3(idb[:], 4, 1), in1=LT[b][:], op=ALU.subtract), r=[bidb, bLT[b]], w=[bXm[0]])
        yield
        cur = 0
        PTl = [LT[b], PTm[1]]; bPTl = [bLT[b], bPTm[1]]
        for lvl in range(5):
            nxt = 1 - cur
            if lvl == 1:
                PTl[0] = PTm[0]; bPTl[0] = bPTm[0]
            for h in range(4):
                T_(lambda e, h=h, cur=cur, pt=PTl[cur]: e.matmul(ps[4][:, h * 128:(h + 1) * 128], lhsT=pt[:, h, :], rhs=Pm[cur][:, h, :], start=True, stop=True),
                   r=[bPTl[cur], bPm[cur]], w=[bps[4]])
            if lvl < 4:
                for h in range(4):
                    T_(lambda e, h=h, cur=cur, pt=PTl[cur]: e.matmul(ps[5][:, h * 128:(h + 1) * 128], lhsT=Pm[cur][:, h, :], rhs=pt[:, h, :], start=True, stop=True),
                       r=[bPTl[cur], bPm[cur]], w=[bps[5]])
            A_(lambda e, nxt=nxt: e.activation(out=Pm[nxt][:].rearrange("p a c -> p (a c)"), in_=ps[4][:, :], func=AF.Copy), r=[bps[4]], w=[bPm[nxt]])
            if lvl < 4:
                V_(lambda e, nxt=nxt: e.tensor_copy(out=PTm[nxt][:].rearrange("p a c -> p (a c)"), in_=ps[5][:, :]), r=[bps[5]], w=[bPTm[nxt]])
                PTl[nxt] = PTm[nxt]; bPTl[nxt] = bPTm[nxt]
            yield
            for h in range(4):
                T_(lambda e, h=h, cur=cur: e.matmul(ps[4][:, h * 128:(h + 1) * 128], lhsT=idb[:], rhs=Xm[cur][:, h, :], start=True, stop=False),
                   r=[bidb, bXm[cur]], w=[bps[4]])
                T_(lambda e, h=h, cur=cur, nxt=nxt: e.matmul(ps[4][:, h * 128:(h + 1) * 128], lhsT=Pm[nxt][:, h, :], rhs=Xm[cur][:, h, :], start=False, stop=True),
                   r=[bPm[nxt], bXm[cur]], w=[bps[4]])
            V_(lambda e, nxt=nxt: e.tensor_copy(out=Xm[nxt][:].rearrange("p a c -> p (a c)"), in_=ps[4][:, :]), r=[bps[4]], w=[bXm[nxt]])
            yield
            cur = nxt
        TT = Xm[cur]; bTT = bXm[cur]
        for h in range(4):
            T_(lambda e, h=h: e.matmul(ps[4][:, h * 128:(h + 1) * 128], lhsT=TT[:, h, :], rhs=ck[b][:, h, :], start=True, stop=True), r=[bTT, bck[b]], w=[bps[4]])
        for h in range(4):
            T_(lambda e, h=h: e.matmul(ps[5][:, h * 128:(h + 1) * 128], lhsT=TT[:, h, :], rhs=vb[b][:, h, :], start=True, stop=True), r=[bTT, bvb[b]], w=[bps[5]])
        A_(lambda e: e.activation(out=A1m[:].rearrange("p a c -> p (a c)"), in_=ps[4][:, :], func=AF.Copy), r=[bps[4]], w=[bA1m])
        V_(lambda e: e.tensor_copy(out=U1m[:].rearrange("p a c -> p (a c)"), in_=ps[5][:, :]), r=[bps[5]], w=[bU1m])
        yield
        res = []
        for c in range(2):
            m = mtc[0] % 2
            mtc[0] += 1
            rows = slice(64 * c, 64 * c + 64)
            for h in range(4):
                T_(lambda e, h=h, rows=rows: e.matmul(ps[4][:, h * 128:(h + 1) * 128], lhsT=A1m[rows, h, :], rhs=kdec[b][rows, h, :], start=True, stop=True),
                   r=[bA1m, bkdec[b]], w=[bps[4]])
            for h in range(4):
                T_(lambda e, h=h, rows=rows: e.matmul(ps[5][:, h * 128:(h + 1) * 128], lhsT=kdec[b][rows, h, :], rhs=U1m[rows, h, :], start=True, stop=True),
                   r=[bU1m, bkdec[b]], w=[bps[5]])
            col = 64 * c + 63
            G_(lambda e, col=col: e.tensor_tensor(out=eI[:], in0=bc3(ident, 4, 1), in1=Erow[b][:, :, col:col + 1].to_broadcast([128, 4, 128]), op=ALU.mult),
               r=[bcst, bErow[b]], w=[beI])
            V_(lambda e, m=m: e.tensor_tensor(out=MT[m][:], in0=ps[4][:, :].rearrange("p (a c) -> p a c", a=4), in1=eI[:], op=ALU.add), r=[bps[4], beI], w=[bMT[m]])
            A_(lambda e, m=m: e.activation(out=Cm[m][:].rearrange("p a c -> p (a c)"), in_=ps[5][:, :], func=AF.Copy), r=[bps[5]], w=[bCm[m]])
            res.append(m)
            yield
        stage2.out[g] = (res, TT, bTT)
    stage2.out = {}

    def stage3(ti, own, g):
        b = g % NBUF
        res, TT, bTT = stage2.out[g]
        for c in range(2):
            m = res[c]
            rows = slice(64 * c, 64 * c + 64)
            s0 = scur[0]
            s1 = 1 - s0
            if own:
                for h in range(4):
                    T_(lambda e, h=h, s0=s0: e.matmul(ps[6][:, h * 128:(h + 1) * 128], lhsT=kT[b][:, h, :], rhs=Sst[s0][:, h, :], start=True, stop=True),
                       r=[bkT[b], bSst[s0]], w=[bps[6]])
                V_(lambda e, rows=rows: e.tensor_tensor(out=rr[rows], in0=ps[6][rows, :].rearrange("p (a c) -> p a c", a=4), in1=bc3(gbt[b][rows, 16:20], 128, 2), op=ALU.mult),
                   r=[bps[6], bgbt[b]], w=[brr])
                G_(lambda e, rows=rows: e.tensor_tensor(out=rr[rows], in0=rr[rows], in1=vb[b][rows], op=ALU.add), r=[brr, bvb[b]], w=[brr])
                yield
                for h in range(4):
                    T_(lambda e, h=h, rows=rows: e.matmul(ps[6][:, h * 128:(h + 1) * 128], lhsT=TT[rows, h, :], rhs=rr[rows, h, :], start=True, stop=True),
                       r=[bTT, brr], w=[bps[6]])
                A_(lambda e, rows=rows: e.activation(out=vn[rows].rearrange("p a c -> p (a c)"), in_=ps[6][rows, :], func=AF.Copy), r=[bps[6]], w=[bvn])
                yield
                for h in range(4):
                    T_(lambda e, h=h, s0=s0: e.matmul(ps[6][:, h * 128:(h + 1) * 128], lhsT=qdT[b][:, h, :], rhs=Sst[s0][:, h, :], start=True, stop=False),
                       r=[bqdT[b], bSst[s0]], w=[bps[6]])
                    T_(lambda e, h=h, rows=rows: e.matmul(ps[6][:, h * 128:(h + 1) * 128], lhsT=atT[b][rows, h, :], rhs=vn[rows, h, :], start=False, stop=True),
                       r=[batT[b], bvn], w=[bps[6]])
                A_(lambda e, rows=rows: e.activation(out=osb[rows].rearrange("p a c -> p (a c)"), in_=ps[6][rows, :], func=AF.Copy), r=[bps[6]], w=[bosb])
                yield
            for h in range(4):
                T_(lambda e, h=h, m=m: e.matmul(ps[7][:, h * 128:(h + 1) * 128], lhsT=ident, rhs=Cm[m][:, h, :], start=True, stop=False), r=[bcst, bCm[m]], w=[bps[7]])
                T_(lambda e, h=h, m=m, s0=s0: e.matmul(ps[7][:, h * 128:(h + 1) * 128], lhsT=MT[m][:, h, :], rhs=Sst[s0][:, h, :], start=False, stop=True),
                   r=[bMT[m], bSst[s0]], w=[bps[7]])
            V_(lambda e, s1=s1: e.tensor_copy(out=Sst[s1][:].rearrange("p a c -> p (a c)"), in_=ps[7][:, :]), r=[bps[7]], w=[bSst[s1]])
            scur[0] = s1
            yield

    def stage_tok(ti, g):
        b = g % NBUF
        for gi in range(3):
            for kc in range(8):
                T_(lambda e, gi=gi, kc=kc: e.matmul(ps[gi][:, :], lhsT=hT[b][:, kc, :], rhs=wtok[:, kc, gi * 512:(gi + 1) * 512], start=(kc == 0), stop=(kc == 7)),
                   r=[bhT[b], bwtok], w=[bps[gi]])
        A_(lambda e: e.activation(out=uu[:], in_=ps[0][:, :], func=AF.Gelu_apprx_tanh), r=[bps[0]], w=[buu])
        A_(lambda e: e.activation(out=vv[:], in_=ps[1][:, :], func=AF.Gelu_apprx_tanh), r=[bps[1]], w=[bvv])
        A_(lambda e: e.activation(out=zz[:], in_=ps[2][:, :], func=AF.Silu), r=[bps[2]], w=[bzz])
        yield
        A_(lambda e: e.activation(out=junk2[:], in_=vv[:], func=AF.Square, accum_out=st[b][:, 4:5]), r=[bvv], w=[bjunk2, bst[b]])
        A_(lambda e: e.activation(out=st[b][:, 5:6], in_=st[b][:, 4:5], func=AF.Sqrt, bias=epsc[:, 0:1], scale=1.0 / 512), r=[bst[b], bepsc], w=[bst[b]])
        V_(lambda e: e.reciprocal(out=st[b][:, 6:7], in_=st[b][:, 5:6]), r=[bst[b]], w=[bst[b]])
        V_(lambda e: e.scalar_tensor_tensor(out=vv[:], in0=vv[:], scalar=st[b][:, 6:7], in1=gmn[:], op0=ALU.mult, op1=ALU.mult), r=[bvv, bst[b], bgmn], w=[bvv])
        yield
        for h in range(8):
            T_(lambda e, h=h: e.matmul(ps[0][:, h * 64:(h + 1) * 64], lhsT=wsm[:, h, :], rhs=vv[:, h * 64:(h + 1) * 64], start=True, stop=True), r=[bwsm, bvv], w=[bps[0]])
        V_(lambda e: e.tensor_tensor(out=junk2[:], in0=ps[0][:, :], in1=bsb[:], op=ALU.add), r=[bps[0], bbsb], w=[bjunk2])
        V_(lambda e: e.tensor_tensor(out=mix[:, 0:512], in0=junk2[:], in1=uu[:], op=ALU.mult), r=[bjunk2, buu], w=[bmix])
        yield

    def stage_out(ti, g):
        b = g % NBUF
        for h in range(4):
            A_(lambda e, h=h: e.activation(out=junk2[:, h * 128:(h + 1) * 128], in_=osb[:, h, :], func=AF.Square, accum_out=st[b][:, 4 + h:5 + h]), r=[bosb], w=[bjunk2, bst[b]])
        A_(lambda e: e.activation(out=st[b][:, 4:8], in_=st[b][:, 4:8], func=AF.Sqrt, bias=epsc[:, 0:1], scale=1.0 / 128), r=[bst[b], bepsc], w=[bst[b]])
        V_(lambda e: e.reciprocal(out=st[b][:, 4:8], in_=st[b][:, 4:8]), r=[bst[b]], w=[bst[b]])
        V_(lambda e: e.tensor_tensor(out=osb[:], in0=osb[:], in1=bc3(st[b][:, 4:8], 128, 2), op=ALU.mult), r=[bosb, bst[b]], w=[bosb])
        G_(lambda e: e.tensor_tensor(out=osb[:], in0=osb[:], in1=bc3(onb[:], 4, 1), op=ALU.mult), r=[bosb, bonb], w=[bosb])
        V_(lambda e: e.tensor_tensor(out=mix[:, 512:1024], in0=osb[:].rearrange("p a c -> p (a c)"), in1=zz[:], op=ALU.mult), r=[bosb, bzz], w=[bmix])
        yield
        for kc in range(8):
            T_(lambda e, kc=kc: e.matmul(ps[kc // 4][:, (kc % 4) * 128:(kc % 4 + 1) * 128], lhsT=mix[:, kc * 128:(kc + 1) * 128], rhs=idb[:], start=True, stop=True),
               r=[bmix, bidb], w=[bps[kc // 4]])
        A_(lambda e: e.activation(out=mixT[:, 0:4, :], in_=ps[0][:, :].rearrange("p (a c) -> p a c", a=4), func=AF.Copy), r=[bps[0]], w=[bmixT])
        V_(lambda e: e.tensor_copy(out=mixT[:, 4:8, :], in_=ps[1][:, :].rearrange("p (a c) -> p a c", a=4)), r=[bps[1]], w=[bmixT])
        yield
        for hf in range(2):
            for kc in range(8):
                T_(lambda e, hf=hf, kc=kc: e.matmul(ps[2 + hf][:, :], lhsT=mixT[:, kc, :], rhs=wout[:, kc, hf * 512:(hf + 1) * 512], start=(kc == 0), stop=(kc == 7)),
                   r=[bmixT, bwout], w=[bps[2 + hf]])
            cs = slice(hf * 512, (hf + 1) * 512)
            V_(lambda e, hf=hf, cs=cs: e.tensor_tensor(out=x1t[:, cs], in0=ps[2 + hf][:, :], in1=GT1[:, cs], op=ALU.mult), r=[bps[2 + hf], bGT1], w=[bx1t])
        G_(lambda e: e.tensor_tensor(out=x1t[:], in0=x1t[:], in1=xt[b][:], op=ALU.add), r=[bx1t, bxt[b]], w=[bx1t])
        D_(lambda e: e.dma_start(out=x1s[ti * 128:(ti + 1) * 128, :], in_=x1t[:]), r=[bx1t], w=[bx1s], buf=bx1s)
        yield

    tiles = [(t, False) for t in range(NPREV)] + [(t, True) for t in range(NT)]

    def tile_rest(ti, own, g):
        if phase == 11:
            return
        yield from stage2(ti, own, g)
        if own and phase not in (12, 13):
            yield from stage_tok(ti, g)
        if phase == 12:
            return
        yield from stage3(ti, own, g)
        if own and phase not in (13, 14):
            yield from stage_out(ti, g)

    import os, itertools
    CUT = int(os.environ.get("KCUT", "1000"))
    _stage1 = stage1
    stage1 = lambda a, b_, c_: itertools.islice(_stage1(a, b_, c_), CUT)
    run_interleaved([stage1(tiles[0][0], tiles[0][1], 0)])
    for g, (ti, own) in enumerate(tiles):
        gens = [tile_rest(ti, own, g)]
        if g + 1 < len(tiles):
            gens.append(stage1(tiles[g + 1][0], tiles[g + 1][1], g + 1))
        run_interleaved(gens)

    P.barrier([bx1s, bcin, bSst[0], bSst[1], bpmk])
    if phase in (1, 11, 12, 13, 14):
        P.op("sync", lambda e: e.nop(), r=[bx1s])
        P.emit(); mx.close(); top.close()
        return nc
    mx.close()

    pe = ExitStack()
    import os as _os3
    KDBG = _os3.environ.get("KDBG", "")
    s2a = sbt(pe, "s2a", [128, GT, 8, 128]); bs2a = [Buf(f"s2a{i}") for i in range(GT)]
    aa = sbt(pe, "aa", [128, GT, 8, 128]); baa = [Buf(f"aa{i}") for i in range(GT)]
    kap = sbt(pe, "kap", [128, GT, 8]); bkap = [Buf(f"kap{i}") for i in range(GT)]
    tau = sbt(pe, "tau", [128, GT, 8])
    h2T = sbt(pe, "h2T", [128, 8, GT * 128], BF16); bh2T = Buf("h2T")
    acc = sbt(pe, "acc", [128, GT, 1024]); bacc = [Buf(f"acc{i}") for i in range(GT)]
    x1g = sbt(pe, "x1g", [128, GT, 1024]); bx1g = [Buf(f"x1g{i}") for i in range(GT)]
    pst = sbt(pe, "pst", [128, 8]); bpst = Buf("pst")
    Ub = [sbt(pe, f"Ub{i}", [128, 8, 1024], BF16) for i in range(2)]; bUb = [Buf(f"Ub{i}") for i in range(2)]

    def load_U(bi):
        sl = bi % 2
        D_(lambda e: e.dma_start(out=Ub[sl][:], in_=UT[:, bi * 1024:(bi + 1) * 1024].rearrange("(kc p) n -> p kc n", p=128)), w=[bUb[sl]], buf=bUb[sl], eng="gpsimd")
    A2 = sbt(pe, "A2", [128, 1024]); B2 = sbt(pe, "B2", [128, 1024])
    GT2 = sbt(pe, "GT2", [128, 1024]); FNW = sbt(pe, "FNW", [128, 1024])
    bA2, bB2, bGT2, bFNW = Buf("A2"), Buf("B2"), Buf("GT2"), Buf("FNW")
    D_(lambda e: e.dma_start(out=A2[:], in_=modscr[0:128, :]), r=[bmodscr], w=[bA2], buf=bA2)
    D_(lambda e: e.dma_start(out=B2[:], in_=modscr[128:256, :]), r=[bmodscr], w=[bB2], buf=bB2)
    D_(lambda e: e.dma_start(out=GT2[:], in_=modscr[256:384, :]), r=[bmodscr], w=[bGT2], buf=bGT2)
    D_(lambda e: e.dma_start(out=FNW[:], in_=fnw[:, :]), w=[bFNW], buf=bFNW)

    for grp in range(NG):
        with ExitStack() as rt:
            wqs = [sbt(rt, f"wqs{i}", [128, 8, 256]) for i in range(2)]; bwqs = [Buf(f"wqs{i}") for i in range(2)]
            h2f = sbt(rt, "h2f", [128, 8, GT * 128]); bh2f = Buf("h2f")
            kys = sbt(rt, "kys", [128, 16, 128]); bkys = Buf("kys")
            h2 = sbt(rt, "h2", [128, 1024]); bh2 = Buf("h2")
            jk = sbt(rt, "jk", [128, 1024]); bjk = Buf("jk")
            e16 = sbt(rt, "e16", [128, 8, 16]); be16 = Buf("e16")
            zz8 = sbt(rt, "zz8", [128, 16]); bzz8 = Buf("zz8")
            D_(lambda e: e.dma_start(out=kys[:], in_=keysT.rearrange("p (c k) -> p c k", c=16)), w=[bkys], buf=bkys)
            for tt in range(GT):
                ti = grp * GT + tt
                D_(lambda e, tt=tt, ti=ti: e.dma_start(out=x1g[:, tt, :], in_=x1s[ti * 128:(ti + 1) * 128, :]), r=[bx1s], w=[bx1g[tt]], buf=bx1g[tt])
                A_(lambda e, tt=tt: e.activation(out=jk[:], in_=x1g[:, tt, :], func=AF.Square, accum_out=pst[:, 0:1]), r=[bx1g[tt]], w=[bjk, bpst])
                A_(lambda e: e.activation(out=pst[:, 1:2], in_=pst[:, 0:1], func=AF.Sqrt, bias=epsc[:, 0:1], scale=1.0 / 1024), r=[bpst, bepsc], w=[bpst])
                V_(lambda e: e.reciprocal(out=pst[:, 2:3], in_=pst[:, 1:2]), r=[bpst], w=[bpst])
                V_(lambda e, tt=tt: e.scalar_tensor_tensor(out=jk[:], in0=x1g[:, tt, :], scalar=pst[:, 2:3], in1=A2[:], op0=ALU.mult, op1=ALU.mult),
                   r=[bx1g[tt], bpst, bA2], w=[bjk])
                V_(lambda e: e.tensor_tensor(out=h2[:], in0=jk[:], in1=B2[:], op=ALU.add), r=[bjk, bB2], w=[bh2])
                for kc in range(8):
                    T_(lambda e, kc=kc: e.matmul(ps[kc // 4][:, (kc % 4) * 128:(kc % 4 + 1) * 128], lhsT=h2[:, kc * 128:(kc + 1) * 128], rhs=ident, start=True, stop=True),
                       r=[bh2, bcst], w=[bps[kc // 4]])
                for hf in range(2):
                    A_(lambda e, hf=hf, tt=tt: e.activation(out=h2f[:, hf * 4:(hf + 1) * 4, tt * 128:(tt + 1) * 128], in_=ps[hf][:, :].rearrange("p (a c) -> p a c", a=4), func=AF.Copy),
                       r=[bps[hf]], w=[bh2f])
                    V_(lambda e, hf=hf, tt=tt: e.tensor_copy(out=h2T[:, hf * 4:(hf + 1) * 4, tt * 128:(tt + 1) * 128], in_=ps[hf][:, :].rearrange("p (a c) -> p a c", a=4)),
                       r=[bps[hf]], w=[bh2T])
            NW = GT * 128
            qTh = [sbt(rt, f"qTh{i}", [128, 2, NW]) for i in range(2)]; bqTh = [Buf(f"qTh{i}") for i in range(2)]
            c16g = sbt(rt, "c16g", [128, GT, 8, 16]); bc16g = [Buf(f"c16g{i}") for i in range(GT)]
            v12h = [sbt(rt, f"v12h{i}", [128, 32]) for i in range(2)]; bv12h = [Buf(f"v12h{i}") for i in range(2)]
            cdh = [sbt(rt, f"cdh{i}", [128, 256]) for i in range(2)]; bcdh = [Buf(f"cdh{i}") for i in range(2)]
            tkh = [sbt(rt, f"tkh{i}", [128, 128]) for i in range(2)]; btkh = [Buf(f"tkh{i}") for i in range(2)]
            bsc = [[Buf(f"sc{t_}_{h_}") for h_ in range(8)] for t_ in range(GT)]
            rc = 0
            for h in range(8):
                sl = h % 2
                D_(lambda e, h=h, sl=sl: e.dma_start(out=wqs[sl][:], in_=wq[:, h * 256:(h + 1) * 256].rearrange("(kc p) n -> p kc n", p=128)), w=[bwqs[sl]], buf=bwqs[sl])
                if h == 7 and KDBG != 'rt':
                    load_U(0)
                    load_U(1)
                for cc in range(2):
                    pb = 2 + cc
                    for kc in range(8):
                        T_(lambda e, cc=cc, kc=kc, sl=sl, pb=pb: e.matmul(ps[pb][:, 0:NW], lhsT=wqs[sl][:, kc, cc * 128:(cc + 1) * 128], rhs=h2f[:, kc, :],
                                                                          start=(kc == 0), stop=(kc == 7)), r=[bwqs[sl], bh2f], w=[bps[pb]])
                    A_(lambda e, cc=cc, sl=sl, pb=pb: e.activation(out=qTh[sl][:, cc, :], in_=ps[pb][:, 0:NW], func=AF.Copy), r=[bps[pb]], w=[bqTh[sl]])
                for tt in range(GT):
                    tsl = slice(tt * 128, (tt + 1) * 128)
                    w_ = rc % 2
                    rc += 1
                    bk = 4 + w_
                    for half in range(2):
                        T_(lambda e, half=half, bk=bk, h=h, tsl=tsl, sl=sl: e.matmul(ps[bk][:, half * 128:(half + 1) * 128], lhsT=qTh[sl][:, half, tsl], rhs=kys[:, 2 * h + half, :],
                                                                                 start=True, stop=True), r=[bqTh[sl], bkys], w=[bps[bk]])
                    A_(lambda e, tt=tt, h=h, bk=bk: e.activation(out=aa[:, tt, h, :], in_=ps[bk][:, 0:128], func=AF.Copy), r=[bps[bk]], w=[bsc[tt][h]])
                    A_(lambda e, tt=tt, h=h, bk=bk: e.activation(out=s2a[:, tt, h, :], in_=ps[bk][:, 128:256], func=AF.Copy), r=[bps[bk]], w=[bsc[tt][h]])
                    for half, src in enumerate((aa[:, tt, h, :], s2a[:, tt, h, :])):
                        o0 = half * 16
                        V_(lambda e, src=src, o0=o0, w_=w_: e.max(out=v12h[w_][:, o0:o0 + 8], in_=src), r=[bsc[tt][h]], w=[bv12h[w_]])
                        V_(lambda e, src=src, o0=o0, w_=w_: e.match_replace(out=tkh[w_][:], in_to_replace=v12h[w_][:, o0:o0 + 8], in_values=src, imm_value=-1e30),
                           r=[bsc[tt][h], bv12h[w_]], w=[btkh[w_]])
                        V_(lambda e, o0=o0, w_=w_: e.max(out=v12h[w_][:, o0 + 8:o0 + 16], in_=tkh[w_][:]), r=[btkh[w_]], w=[bv12h[w_]])
                    V_(lambda e, w_=w_: e.tensor_tensor(out=cdh[w_][:].rearrange("p (a b) -> p a b", a=16), in0=v12h[w_][:, 0:16].unsqueeze(2).to_broadcast([128, 16, 16]),
                                                        in1=v12h[w_][:, 16:32].unsqueeze(1).to_broadcast([128, 16, 16]), op=ALU.add), r=[bv12h[w_]], w=[bcdh[w_]])
                    V_(lambda e, tt=tt, h=h, w_=w_: e.max(out=c16g[:, tt, h, 0:8], in_=cdh[w_][:]), r=[bcdh[w_]], w=[bc16g[tt]])
                    V_(lambda e, tt=tt, h=h, w_=w_: e.match_replace(out=cdh[w_][:], in_to_replace=c16g[:, tt, h, 0:8], in_values=cdh[w_][:], imm_value=-1e30),
                       r=[bcdh[w_], bc16g[tt]], w=[bcdh[w_]])
                    V_(lambda e, tt=tt, h=h, w_=w_: e.max(out=c16g[:, tt, h, 8:16], in_=cdh[w_][:]), r=[bcdh[w_]], w=[bc16g[tt]])
            for tt in range(GT):
                V_(lambda e, tt=tt: e.tensor_copy(out=tau[:, tt, :], in_=c16g[:, tt, :, 15]), r=[bc16g[tt]], w=[bkap[tt]])
                V_(lambda e, tt=tt: e.tensor_tensor(out=e16[:], in0=c16g[:, tt], in1=c16g[:, tt, :, 0:1].to_broadcast([128, 8, 16]), op=ALU.subtract), r=[bc16g[tt]], w=[be16])
                A_(lambda e: e.activation(out=e16[:], in_=e16[:], func=AF.Exp), r=[be16], w=[be16])
                V_(lambda e: e.tensor_reduce(out=zz8[:, 0:8], in_=e16[:], axis=mybir.AxisListType.X, op=ALU.add), r=[be16], w=[bzz8])
                A_(lambda e: e.activation(out=zz8[:, 8:16], in_=zz8[:, 0:8], func=AF.Ln), r=[bzz8], w=[bzz8])
                V_(lambda e, tt=tt: e.tensor_tensor(out=kap[:, tt, :], in0=c16g[:, tt, :, 0], in1=zz8[:, 8:16], op=ALU.add), r=[bc16g[tt], bzz8], w=[bkap[tt]])
                V_(lambda e, tt=tt: e.tensor_scalar(out=kap[:, tt, :], in0=kap[:, tt, :], scalar1=-1.0, scalar2=None, op0=ALU.mult), r=[bkap[tt]], w=[bkap[tt]])
                G_(lambda e, tt=tt: e.memset(acc[:, tt, :], 0.0), w=[bacc[tt]])
            P.barrier([bh2f, bkys, bwqs[0], bwqs[1], be16, bzz8, bh2, bjk] + bqTh + bv12h + bcdh + btkh + bc16g)

        with ExitStack() as ex:
            Vb = [sbt(ex, f"Vb{i}", [128, 8, 1024], BF16) for i in range(2)]; bVb = [Buf(f"Vb{i}") for i in range(2)]
            Ag = [sbt(ex, f"Ag{i}", [128, 8, GT * 128], BF16) for i in range(2)]; bAg = [Buf(f"Ag{i}") for i in range(2)]
            NSP, NEB = 4, 3
            Sp = [sbt(ex, f"Sp{i}", [128, 8, 128]) for i in range(NSP)]; bSp = [Buf(f"Sp{i}") for i in range(NSP)]
            Eb = [sbt(ex, f"Eb{i}", [128, 8, 128], BF16) for i in range(NEB)]; bEb = [Buf(f"Eb{i}") for i in range(NEB)]
            Mb = [sbt(ex, f"Mb{i}", [128, 8, 128], BF16) for i in range(NEB)]; bMb = [Buf(f"Mb{i}") for i in range(NEB)]
            Wt = [sbt(ex, f"Wt{i}", [128, 8, 128], BF16) for i in range(2)]; bWt = [Buf(f"Wt{i}") for i in range(2)]
            NW = GT * 128
            NDVE = 8
            NBX = NB if KDBG != 'rt' else 0

            def load_V(bi):
                sl = bi % 2
                D_(lambda e: e.dma_start(out=Vb[sl][:], in_=V[bi * 1024:(bi + 1) * 1024, :].rearrange("(c p) n -> p c n", p=128)), w=[bVb[sl]], buf=bVb[sl], eng="gpsimd")

            def act_mm(bi, ch):
                sl = bi % 2
                pb = ch % 2
                for kc in range(8):
                    T_(lambda e, kc=kc: e.matmul(ps[pb][:, 0:NW], lhsT=Ub[sl][:, kc, ch * 128:(ch + 1) * 128], rhs=h2T[:, kc, :], start=(kc == 0), stop=(kc == 7)),
                       r=[bUb[sl], bh2T], w=[bps[pb]])

            def act_gelu(bi, ch):
                sl = bi % 2
                pb = ch % 2
                A_(lambda e: e.activation(out=Ag[sl][:, ch, :], in_=ps[pb][:, 0:NW], func=AF.Gelu_apprx_tanh), r=[bps[pb]], w=[bAg[sl]])

            def act_chunk(bi, ch):
                act_mm(bi, ch)
                act_gelu(bi, ch)

            spc = [0]
            ebc = [0]
            deferred = {}

            def unit(bi, tt, uidx):
                sl = bi % 2
                gpair = 2 + 2 * (uidx % 2)
                w3 = uidx % 2
                slots = {}

                def emit_sp(h):
                    s_ = spc[0] % NSP
                    spc[0] += 1
                    slots[h] = s_
                    eng = V_ if h < NDVE else G_
                    eng(lambda e: e.tensor_tensor(out=Sp[s_][:], in0=aa[:, tt, h, bi * 8:(bi + 1) * 8].unsqueeze(2).to_broadcast([128, 8, 128]),
                                                  in1=s2a[:, tt, h, :].unsqueeze(1).to_broadcast([128, 8, 128]), op=ALU.add),
                        r=[baa[tt], bs2a[tt]], w=[bSp[s_]])

                emit_sp(0)
                emit_sp(1)
                if 'wt' in deferred:
                    deferred.pop('wt')()
                emit_sp(2)
                emit_sp(3)
                cpu = 8 // GT
                for h in range(8):
                    s_ = slots[h]
                    w2 = ebc[0] % NEB
                    ebc[0] += 1
                    A_(lambda e, h=h, s_=s_, w2=w2: e.activation(out=Eb[w2][:], in_=Sp[s_][:], func=AF.Exp, bias=kap[:, tt, h:h + 1], scale=1.0),
                       r=[bSp[s_], bkap[tt]], w=[bEb[w2]])
                    V_(lambda e, h=h, s_=s_, w2=w2: e.scalar_tensor_tensor(out=Mb[w2][:], in0=Sp[s_][:], scalar=tau[:, tt, h:h + 1], in1=Eb[w2][:], op0=ALU.is_ge, op1=ALU.mult),
                       r=[bSp[s_], bEb[w2], bkap[tt]], w=[bMb[w2]])
                    for ch in range(8):
                        bank = gpair + ch // 4
                        T_(lambda e, ch=ch, bank=bank, h=h, w2=w2: e.matmul(ps[bank][:, (ch % 4) * 128:(ch % 4 + 1) * 128], lhsT=Mb[w2][:, ch, :], rhs=idb[:],
                                                                           start=(h == 0 and ch % 4 == 0), stop=(h == 7), skip_group_check=True), r=[bMb[w2], bidb], w=[bps[bank]])
                    if h + 4 < 8:
                        emit_sp(h + 4)
                    if h == 2 and 'acc' in deferred:
                        deferred.pop('acc')()
                    if bi + 1 < NBX:
                        pairs = max(1, cpu // 2)
                        step = 8 // pairs
                        for p_ in range(pairs):
                            h_mm = 1 if pairs == 1 else step * p_
                            h_ge = 6 if pairs == 1 else step * p_ + step - 1
                            c0 = tt * cpu + 2 * p_
                            if h == h_mm:
                                act_mm(bi + 1, c0)
                                act_mm(bi + 1, c0 + 1)
                            if h == h_ge:
                                act_gelu(bi + 1, c0)
                                act_gelu(bi + 1, c0 + 1)

                def do_wt():
                    for q in range(2):
                        V_(lambda e, q=q: e.tensor_tensor(out=Wt[w3][:, q * 4:(q + 1) * 4, :], in0=ps[gpair + q][:, :].rearrange("p (a c) -> p a c", a=4),
                                                          in1=Ag[sl][:, q * 4:(q + 1) * 4, tt * 128:(tt + 1) * 128], op=ALU.mult), r=[bps[gpair + q], bAg[sl]], w=[bWt[w3]])
                    for hf in range(2):
                        for ch in range(8):
                            T_(lambda e, hf=hf, ch=ch: e.matmul(ps[6 + hf][:, :], lhsT=Wt[w3][:, ch, :], rhs=Vb[sl][:, ch, hf * 512:(hf + 1) * 512], start=(ch == 0), stop=(ch == 7)),
                               r=[bWt[w3], bVb[sl]], w=[bps[6 + hf]])

                def do_acc():
                    for hf in range(2):
                        V_(lambda e, hf=hf: e.tensor_tensor(out=acc[:, tt, hf * 512:(hf + 1) * 512], in0=acc[:, tt, hf * 512:(hf + 1) * 512], in1=ps[6 + hf][:, :], op=ALU.add),
                           r=[bps[6 + hf], bacc[tt]], w=[bacc[tt]])

                deferred['wt'] = do_wt
                deferred['acc'] = do_acc

            if NBX > 0:
                load_V(0)
                for ch in range(8):
                    act_chunk(0, ch)
            uidx = 0
            for bi in range(NBX):
                for tt in range(GT):
                    unit(bi, tt, uidx)
                    uidx += 1
                    if tt == min(1, GT - 1) and bi + 1 < NBX:
                        load_V(bi + 1)
                    if tt == min(2, GT - 1) and bi + 2 < NBX:
                        load_U(bi + 2)
            if 'wt' in deferred:
                deferred.pop('wt')()
            if 'acc' in deferred:
                deferred.pop('acc')()
            for tt in range(GT):
                ti = grp * GT + tt
                import os as _os2
                if _os2.environ.get("KDBG", "") in ("ff", "rt"):
                    pass
                elif _os2.environ.get("KDBG", "") == "x1":
                    V_(lambda e, tt=tt: e.tensor_copy(out=acc[:, tt, :], in_=x1g[:, tt, :]), r=[bacc[tt], bx1g[tt]], w=[bacc[tt]])
                else:
                    V_(lambda e, tt=tt: e.tensor_tensor(out=acc[:, tt, :], in0=acc[:, tt, :], in1=GT2[:], op=ALU.mult), r=[bacc[tt], bGT2], w=[bacc[tt]])
                    V_(lambda e, tt=tt: e.tensor_tensor(out=acc[:, tt, :], in0=acc[:, tt, :], in1=x1g[:, tt, :], op=ALU.add), r=[bacc[tt], bx1g[tt]], w=[bacc[tt]])
                if _os2.environ.get('KDBG', '') not in ('ff', 'rt'):
                    A_(lambda e, tt=tt: e.activation(out=x1g[:, tt, :], in_=acc[:, tt, :], func=AF.Square, accum_out=pst[:, 4:5]), r=[bacc[tt]], w=[bx1g[tt], bpst])
                    A_(lambda e: e.activation(out=pst[:, 5:6], in_=pst[:, 4:5], func=AF.Sqrt, bias=epsc[:, 0:1], scale=1.0 / 1024), r=[bpst, bepsc], w=[bpst])
                    V_(lambda e: e.reciprocal(out=pst[:, 6:7], in_=pst[:, 5:6]), r=[bpst], w=[bpst])
                    V_(lambda e, tt=tt: e.scalar_tensor_tensor(out=acc[:, tt, :], in0=acc[:, tt, :], scalar=pst[:, 6:7], in1=FNW[:], op0=ALU.mult, op1=ALU.mult),
                       r=[bacc[tt], bpst, bFNW], w=[bacc[tt]])
                D_(lambda e, tt=tt, ti=ti: e.dma_start(out=y[ti * 128:(ti + 1) * 128, :], in_=acc[:, tt, :]), r=[bacc[tt]], w=[by], buf=by)
            P.barrier([bUb[0], bUb[1], bVb[0], bVb[1], bWt[0], bWt[1], by] + bAg + bSp + bEb + bMb + bacc + bx1g)
    P.op("sync", lambda e: e.nop(), r=[by])
    P.emit()
    pe.close()
    top.close()
    return nc


def prep_inputs(inp, n_cores, NT):
    f = lambda a: np.ascontiguousarray(np.asarray(a, dtype=np.float32))
    x = f(inp["x"])[0]
    S = x.shape[0]
    assert S == n_cores * NT * 128
    NPREV = (n_cores - 1) * NT
    w_in = f(inp["w_in"])[0]
    w_tok = np.ascontiguousarray(np.concatenate([w_in[:, 0:1024], w_in[:, 2560:3072]], axis=1))
    w_qkv = np.ascontiguousarray(w_in[:, 1024:2560])
    w_ab = np.zeros((1024, 64), np.float32)
    w_ab[:, 0:4] = w_in[:, 3072:3076]
    w_ab[:, 32:36] = w_in[:, 3076:3080]
    rep = lambda v, n=128: np.ascontiguousarray(np.broadcast_to(v[None, :], (n, v.shape[0])))
    gm_ws = f(inp["gm_ws"])[0]
    wsT = np.ascontiguousarray(gm_ws.transpose(2, 0, 1).reshape(128, 8 * 128))
    gm_bs = f(inp["gm_bs"])[0]
    bs_bc = np.ascontiguousarray(np.repeat(gm_bs.T[:, :, None], 64, axis=2).reshape(128, 512))
    conv_w = f(inp["conv_w"])[0]
    convw = np.ascontiguousarray(conv_w.reshape(4, 12, 128).transpose(2, 1, 0).reshape(128, 48))
    gpar = np.zeros((128, 8), np.float32)
    gpar[:, 0:4] = f(inp["a_log"])[0][None, :]
    gpar[:, 4:8] = f(inp["dt_bias"])[0][None, :]
    k1 = f(inp["peer_keys1"])[0]
    k2 = f(inp["peer_keys2"])[0]
    keysT = np.zeros((128, 16, 128), np.float32)
    for h in range(8):
        keysT[:, 2 * h, :] = k1[h].T
        keysT[:, 2 * h + 1, :] = k2[h].T
    keysT = keysT.reshape(128, 16 * 128)
    UT = np.ascontiguousarray(f(inp["peer_u"])[0].T)
    Vv = f(inp["peer_v"])[0]
    c = f(inp["c"])[0]
    shared = {
        "c_col": np.ascontiguousarray(c.reshape(8, 128).T),
        "w_ada": f(inp["w_ada"])[0], "b_ada": f(inp["b_ada"])[0][None, :],
        "n1w": rep(f(inp["norm1_w"])[0]), "n2w": rep(f(inp["norm2_w"])[0]), "fnw": rep(f(inp["final_norm_w"])),
        "gmnw": rep(f(inp["gm_norm_w"])[0]), "onw": rep(f(inp["o_norm_w"])[0]),
        "w_tok": w_tok, "w_qkv": w_qkv, "w_ab": w_ab, "wsT": wsT, "bs_bc": bs_bc, "convw": convw, "gpar": gpar,
        "w_out": f(inp["w_out"])[0], "wq": f(inp["peer_wq"])[0], "keysT": keysT, "UT": UT, "V": Vv,
        "consts": make_consts(),
    }
    maps = []
    for ci in range(n_cores):
        m = dict(shared)
        s0 = ci * NT * 128
        m["x_own"] = np.ascontiguousarray(x[s0:s0 + NT * 128])
        xp = np.zeros((max(NPREV, 1) * 128, 1024), np.float32)
        pm = np.zeros((128, max(NPREV, 1)), np.float32)
        if NPREV > 0:
            lo = s0 - NPREV * 128
            for t in range(NPREV):
                g0 = lo + t * 128
                if g0 >= 0:
                    xp[t * 128:(t + 1) * 128] = x[g0:g0 + 128]
                    pm[:, t] = 1.0
        m["x_prev"] = xp
        m["pmask"] = pm
        maps.append(m)
    return maps


_CACHE = {}


def kernel(**inputs):
    n_cores, NT = 8, 16
    maps = prep_inputs(inputs, n_cores, NT)
    key = (n_cores, NT)
    if key not in _CACHE:
        _CACHE[key] = build(n_cores, NT)
    nc = _CACHE[key]
    res = run_bass_kernel_spmd(nc, maps, core_ids=list(range(n_cores)))
    out = np.concatenate([np.asarray(r["y"], dtype=np.float32) for r in res.results], axis=0)
    return out[None, :, :].astype(np.float32)
```

```python
import numpy as np
from contextlib import ExitStack
import concourse.bass as bass
import concourse.mybir as mybir
from concourse.bass_utils import run_bass_kernel_spmd

F32 = mybir.dt.float32
BF16 = mybir.dt.bfloat16
ALU = mybir.AluOpType
AF = mybir.ActivationFunctionType

ENGS = ["sync", "scalar", "vector", "gpsimd", "tensor"]
SEM_CHUNK = 4000
EPS = 1e-6


class Buf:
    __slots__ = ("name", "w", "r", "dsem", "dcnt", "excl")

    def __init__(self, name, excl=False):
        self.excl = excl
        self.name = name
        self.w = None
        self.r = {}
        self.dsem = None
        self.dcnt = 0


class Op:
    __slots__ = ("eng", "fn", "deps", "dma", "sig", "need", "dbuf", "dval", "inc")

    def __init__(self, eng, fn, dma):
        self.eng = eng
        self.fn = fn
        self.deps = []
        self.dma = dma
        self.sig = None
        self.need = False
        self.dbuf = None
        self.dval = 0
        self.inc = 16


class Prog:
    def __init__(self, nc):
        self.nc = nc
        self.q = {e: [] for e in ENGS}
        self.dma_bufs = []
        self.last = {e: None for e in ENGS}

    def op(self, eng, fn, r=(), w=(), dma_buf=None, extra=()):
        deps = list(extra)
        for b in r:
            if b.w is not None:
                deps.append(b.w)
            if b.excl:
                for o in b.r.values():
                    if o.eng != eng:
                        deps.append(o)
        for b in w:
            if b.w is not None and not (dma_buf is not None and b.w.dma and b.w.dbuf is dma_buf):
                deps.append(b.w)
            for o in b.r.values():
                deps.append(o)
        dma = dma_buf is not None
        rec = Op(eng, fn, dma)
        seen = set()
        for d in deps:
            if id(d) in seen:
                continue
            seen.add(id(d))
            if (not d.dma) and d.eng == eng:
                if eng == "tensor":
                    continue
                if not any(b.w is d for b in r) and d not in extra:
                    continue
            d.need = True
            rec.deps.append(d)
        if dma:
            if dma_buf.dsem is None:
                dma_buf.dsem = len(self.dma_bufs)
                self.dma_bufs.append(dma_buf)
            dma_buf.dcnt += 16
            rec.dbuf = dma_buf
            rec.dval = dma_buf.dcnt
        self.q[eng].append(rec)
        if not dma:
            self.last[eng] = rec
        for b in r:
            b.r[eng if not dma else ("dma", id(rec))] = rec
        for b in w:
            b.w = rec
            b.r = {}
        return rec

    def barrier(self, bufs):
        deps = [o for o in self.last.values() if o is not None]
        for b in bufs:
            if b.w is not None:
                deps.append(b.w)
            deps.extend(b.r.values())
        for e in ENGS:
            self.op(e, lambda en: en.nop(), extra=[d for d in deps if not (d.eng == e and not d.dma)])

    def emit(self):
        nc = self.nc
        nsig = {}
        for e in ENGS:
            c = 0
            for rec in self.q[e]:
                if rec.dma:
                    continue
                if rec.need:
                    c += 1
                    rec.sig = c
            nsig[e] = c
        with ExitStack() as es:
            esems = {}
            for e in ENGS:
                n = (nsig[e] + SEM_CHUNK - 1) // SEM_CHUNK
                esems[e] = [es.enter_context(nc.semaphore(f"s_{e}_{i}")) for i in range(n)]
            dsems = [es.enter_context(nc.semaphore(f"d_{i}")) for i in range(len(self.dma_bufs))]
            block = es.enter_context(nc.Block())
            prog = self

            def make(e):
                def body(eng):
                    known = {}
                    maxchunk = {}
                    for rec in prog.q[e]:
                        for d in rec.deps:
                            if d.dma:
                                key = ("d", d.dbuf.dsem)
                                val = d.dval
                                sem = dsems[d.dbuf.dsem]
                            else:
                                ci = (d.sig - 1) // SEM_CHUNK
                                if maxchunk.get(d.eng, -1) > ci:
                                    continue
                                key = (d.eng, ci)
                                val = d.sig - ci * SEM_CHUNK
                                sem = esems[d.eng][ci]
                            if known.get(key, 0) >= val:
                                continue
                            eng.wait_ge(sem, val)
                            known[key] = val
                            if not d.dma:
                                maxchunk[d.eng] = max(maxchunk.get(d.eng, -1), ci)
                        ins = rec.fn(eng)
                        if rec.dma:
                            ins.then_inc(dsems[rec.dbuf.dsem], 16)
                        elif rec.need:
                            ci = (rec.sig - 1) // SEM_CHUNK
                            ins.then_inc(esems[e][ci], 1)
                return body

            block.sync(make("sync"))
            block.scalar(make("scalar"))
            block.vector(make("vector"))
            block.gpsimd(make("gpsimd"))
            block.tensor(make("tensor"))


def run_interleaved(gens):
    active = list(gens)
    while active:
        for g in list(active):
            try:
                next(g)
            except StopIteration:
                active.remove(g)


C_ID, C_TRIU, C_BLK, C_NEGU, C_SU, C_CAUS, C_SEL, C_ONES, C_N = 0, 128, 256, 384, 512, 640, 768, 1280, 1408


def make_consts():
    c = np.zeros((128, C_N), np.float32)
    idx = np.arange(128)
    j = idx[:, None]
    i = idx[None, :]
    same = (j // 64) == (i // 64)
    c[:, C_ID:C_ID + 128] = np.eye(128)
    c[:, C_TRIU:C_TRIU + 128] = ((i >= j) & same)
    c[:, C_BLK:C_BLK + 128] = same
    c[:, C_NEGU:C_NEGU + 128] = np.where((i >= j) & same, 0.0, -30000.0)
    c[:, C_SU:C_SU + 128] = ((i > j) & same)
    c[:, C_CAUS:C_CAUS + 128] = (j <= i)
    for h in range(4):
        c[32 + h, C_SEL + h * 128:C_SEL + (h + 1) * 128] = 1.0
    c[:, C_ONES:C_ONES + 128] = 1.0
    return c


def build(n_cores, NT, dbg=False, phase=9):
    NPREV = (n_cores - 1) * NT
    NTOK = NT * 128
    GT = min(4, NT)
    NG = NT // GT
    NB = 16
    nc = bass.Bass("TRN2", target_bir_lowering=False)

    def din(name, shape, dt=F32):
        return nc.dram_tensor(name, shape, dt, kind="ExternalInput").ap()

    x_own = din("x_own", [NTOK, 1024])
    x_prev = din("x_prev", [max(NPREV, 1) * 128, 1024])
    pmask = din("pmask", [128, max(NPREV, 1)])
    c_col = din("c_col", [128, 8])
    w_ada = din("w_ada", [1024, 6144])
    b_ada = din("b_ada", [1, 6144])
    n1w = din("n1w", [128, 1024])
    n2w = din("n2w", [128, 1024])
    fnw = din("fnw", [128, 1024])
    gmnw = din("gmnw", [128, 512])
    onw = din("onw", [128, 128])
    w_tok = din("w_tok", [1024, 1536])
    w_qkv = din("w_qkv", [1024, 1536])
    w_ab = din("w_ab", [1024, 64])
    wsT = din("wsT", [128, 8 * 128])
    bs_bc = din("bs_bc", [128, 512])
    convw = din("convw", [128, 48])
    gpar = din("gpar", [128, 8])
    w_out = din("w_out", [1024, 1024])
    wq = din("wq", [1024, 2048])
    keysT = din("keysT", [128, 16 * 128])
    UT = din("UT", [1024, 16384])
    V = din("V", [16384, 1024])
    consts = din("consts", [128, C_N])
    y = nc.dram_tensor("y", [NTOK, 1024], F32, kind="ExternalOutput").ap()
    x1s = nc.dram_tensor("x1s", [NTOK, 1024], F32).ap()
    dbg_t = nc.dram_tensor("dbg", [NTOK, 2048], F32, kind="ExternalOutput").ap() if dbg else None

    P = Prog(nc)
    top = ExitStack()

    _names = {}

    def sbt(es, name, shape, dt=F32):
        n = _names.get(name, 0)
        _names[name] = n + 1
        if n:
            name = f"{name}_{n}"
        return es.enter_context(nc.sbuf_tensor(name, shape, dt))

    ps = [top.enter_context(nc.psum_tensor(f"ps{i}", [128, 512], F32)) for i in range(8)]
    bps = [Buf(f"ps{i}", excl=True) for i in range(8)]

    cst = sbt(top, "cst", [128, C_N]); bcst = Buf("cst")
    idb = sbt(top, "idb", [128, 128], BF16); bidb = Buf("idb")
    modscr = nc.dram_tensor("modscr", [3 * 128, 1024], F32).ap()
    bmodscr = Buf("modscr")
    epsc = sbt(top, "epsc", [128, 1]); bepsc = Buf("epsc")
    by = Buf("y"); bx1s = Buf("x1s"); bdbg = Buf("dbg")

    ident = cst[:, C_ID:C_ID + 128]
    triU = cst[:, C_TRIU:C_TRIU + 128]
    blk = cst[:, C_BLK:C_BLK + 128]
    negU = cst[:, C_NEGU:C_NEGU + 128]
    sU = cst[:, C_SU:C_SU + 128]
    caus = cst[:, C_CAUS:C_CAUS + 128]
    ones = cst[:, C_ONES:C_ONES + 128]

    def V_(fn, r=(), w=()):
        return P.op("vector", fn, r, w)

    def A_(fn, r=(), w=()):
        return P.op("scalar", fn, r, w)

    def G_(fn, r=(), w=()):
        return P.op("gpsimd", fn, r, w)

    def T_(fn, r=(), w=()):
        return P.op("tensor", fn, r, w)

    def D_(fn, r=(), w=(), buf=None, eng="sync"):
        return P.op(eng, fn, r, w, dma_buf=buf)

    def bc3(ap2d, n, axis):
        k = ap2d.shape[1]
        if axis == 2:
            return ap2d.unsqueeze(2).to_broadcast([ap2d.shape[0], k, n])
        return ap2d.unsqueeze(1).to_broadcast([ap2d.shape[0], n, k])

    D_(lambda e: e.dma_start(out=cst[:], in_=consts[:, :]), w=[bcst], buf=bcst)
    V_(lambda e: e.tensor_copy(out=idb[:], in_=ident), r=[bcst], w=[bidb])
    V_(lambda e: e.memset(epsc[:], EPS), w=[bepsc])

    mx = ExitStack()
    A1 = sbt(mx, "A1", [128, 1024]); B1 = sbt(mx, "B1", [128, 1024]); GT1 = sbt(mx, "GT1", [128, 1024])
    bA1, bB1, bGT1 = Buf("A1"), Buf("B1"), Buf("GT1")
    wtok = sbt(mx, "wtok", [128, 8, 1536], BF16); bwtok = Buf("wtok")
    wout = sbt(mx, "wout", [128, 8, 1024], BF16); bwout = Buf("wout")
    wqkv = sbt(mx, "wqkv", [128, 8, 1536], BF16); bwqkv = Buf("wqkv")
    wab = sbt(mx, "wab", [128, 8, 64], BF16); bwab = Buf("wab")
    wsm = sbt(mx, "wsm", [128, 8, 128]); bwsm = Buf("wsm")
    bsb = sbt(mx, "bsb", [128, 512]); bbsb = Buf("bsb")
    gmn = sbt(mx, "gmn", [128, 512]); bgmn = Buf("gmn")
    onb = sbt(mx, "onb", [128, 128]); bonb = Buf("onb")
    cw = sbt(mx, "cw", [128, 12, 4]); bcw = Buf("cw")
    gp = sbt(mx, "gp", [128, 12]); bgp = Buf("gp")

    with ExitStack() as su:
        ccl = sbt(su, "ccl", [128, 8]); bccl = Buf("ccl")
        scl = sbt(su, "scl", [128, 8]); bscl = Buf("scl")
        wad = [sbt(su, f"wad{i}", [128, 8, 512]) for i in range(2)]
        bwad = [Buf(f"wad{i}") for i in range(2)]
        bad = sbt(su, "bad", [1, 6144]); bbad = Buf("bad")
        tn1 = sbt(su, "tn1", [128, 1024]); btn1 = Buf("tn1")
        tn2 = sbt(su, "tn2", [128, 1024]); btn2 = Buf("tn2")
        D_(lambda e: e.dma_start(out=ccl[:], in_=c_col[:, :]), w=[bccl], buf=bccl)
        D_(lambda e: e.dma_start(out=bad[:], in_=b_ada[:, :]), w=[bbad], buf=bbad)
        A_(lambda e: e.activation(out=scl[:], in_=ccl[:], func=AF.Silu), r=[bccl], w=[bscl])
        D_(lambda e: e.dma_start(out=wqkv[:], in_=w_qkv.rearrange("(kc p) n -> p kc n", p=128)), w=[bwqkv], buf=bwqkv, eng="gpsimd")
        D_(lambda e: e.dma_start(out=wab[:], in_=w_ab.rearrange("(kc p) n -> p kc n", p=128)), w=[bwab], buf=bwab, eng="gpsimd")
        D_(lambda e: e.dma_start(out=wtok[:], in_=w_tok.rearrange("(kc p) n -> p kc n", p=128)), w=[bwtok], buf=bwtok, eng="gpsimd")
        D_(lambda e: e.dma_start(out=wout[:], in_=w_out.rearrange("(kc p) n -> p kc n", p=128)), w=[bwout], buf=bwout, eng="gpsimd")
        D_(lambda e: e.dma_start(out=wsm[:], in_=wsT.rearrange("p (h t) -> p h t", h=8)), w=[bwsm], buf=bwsm)
        D_(lambda e: e.dma_start(out=bsb[:], in_=bs_bc[:, :]), w=[bbsb], buf=bbsb)
        D_(lambda e: e.dma_start(out=gmn[:], in_=gmnw[:, :]), w=[bgmn], buf=bgmn)
        D_(lambda e: e.dma_start(out=onb[:], in_=onw[:, :]), w=[bonb], buf=bonb)
        D_(lambda e: e.dma_start(out=cw[:], in_=convw.rearrange("p (c k) -> p c k", k=4)), w=[bcw], buf=bcw)
        D_(lambda e: e.dma_start(out=gp[:, 0:8], in_=gpar[:, :]), w=[bgp], buf=bgp)
        V_(lambda e: e.tensor_tensor(out=wsm[:], in0=wsm[:], in1=bc3(caus, 8, 1), op=ALU.mult), r=[bwsm, bcst], w=[bwsm])
        A_(lambda e: e.activation(out=gp[:, 8:12], in_=gp[:, 0:4], func=AF.Exp), r=[bgp], w=[bgp])
        V_(lambda e: e.tensor_scalar(out=gp[:, 8:12], in0=gp[:, 8:12], scalar1=-1.0, scalar2=None, op0=ALU.mult), r=[bgp], w=[bgp])
        dests = [(B1, bB1), (None, None), (GT1, bGT1), (tn2, btn2), (None, None), (tn2, btn2)]
        scr_row = {3: 1, 4: 0, 5: 2}
        for j in range(12):
            sl = j % 2
            D_(lambda e, j=j, sl=sl: e.dma_start(out=wad[sl][:], in_=w_ada[:, j * 512:(j + 1) * 512].rearrange("(kc p) n -> p kc n", p=128)),
               w=[bwad[sl]], buf=bwad[sl])
            pb = j % 2
            for kc in range(8):
                T_(lambda e, kc=kc, sl=sl, pb=pb: e.matmul(ps[pb][:, :], lhsT=scl[:, kc:kc + 1].to_broadcast([128, 128]), rhs=wad[sl][:, kc, :],
                                                           start=(kc == 0), stop=False), r=[bscl, bwad[sl]], w=[bps[pb]])
            T_(lambda e, j=j, pb=pb: e.matmul(ps[pb][:, :], lhsT=ones[0:1, :], rhs=bad[0:1, j * 512:(j + 1) * 512], start=False, stop=True),
               r=[bcst, bbad], w=[bps[pb]])
            vec, half = j // 2, j % 2
            cs = slice(half * 512, (half + 1) * 512)
            if vec in (1, 4):
                dst, bdst, nw = (A1, bA1, n1w) if vec == 1 else (tn2, btn2, n2w)
                if half == 0:
                    D_(lambda e, nw=nw: e.dma_start(out=tn1[:], in_=nw[:, :]), w=[btn1], buf=btn1)
                V_(lambda e, dst=dst, cs=cs, pb=pb: e.scalar_tensor_tensor(out=dst[:, cs], in0=ps[pb][:, :], scalar=1.0, in1=tn1[:, cs],
                                                                         op0=ALU.add, op1=ALU.mult), r=[bps[pb], btn1], w=[bdst])
            else:
                dst, bdst = dests[vec]
                A_(lambda e, dst=dst, cs=cs, pb=pb: e.activation(out=dst[:, cs], in_=ps[pb][:, :], func=AF.Copy), r=[bps[pb]], w=[bdst])
            if vec >= 3 and half == 1:
                rr0 = scr_row[vec] * 128
                D_(lambda e, rr0=rr0: e.dma_start(out=modscr[rr0:rr0 + 128, :], in_=tn2[:]), r=[btn2], w=[bmodscr], buf=bmodscr)
        P.barrier([bwad[0], bwad[1], btn1, btn2, bbad, bscl, bccl, bmodscr])

    if phase == 0:
        P.op("sync", lambda e: e.nop())
        P.emit(); mx.close(); top.close()
        return nc
    NBUF = 2
    xt = [sbt(mx, f"xt{i}", [128, 1024]) for i in range(NBUF)]; bxt = [Buf(f"xt{i}") for i in range(NBUF)]
    pmk = sbt(mx, "pmk", [128, max(NPREV, 1)]); bpmk = Buf("pmk")
    junk = sbt(mx, "junk", [128, 1024]); bjunk = Buf("junk")
    junk2 = sbt(mx, "junk2", [128, 512]); bjunk2 = Buf("junk2")
    st = [sbt(mx, f"st{i}", [128, 8]) for i in range(NBUF)]; bst = [Buf(f"st{i}") for i in range(NBUF)]
    hb = [sbt(mx, f"hb{i}", [128, 1024], BF16) for i in range(NBUF)]; bhb = [Buf(f"hb{i}") for i in range(NBUF)]
    hT = [sbt(mx, f"hT{i}", [128, 8, 128], BF16) for i in range(NBUF)]; bhT = [Buf(f"hT{i}") for i in range(NBUF)]
    cin = sbt(mx, "cin", [128, 12, 131]); bcin = Buf("cin")
    cacc = sbt(mx, "cacc", [128, 12, 128]); bcacc = Buf("cacc")
    bcaccs = [Buf(f"cacc_c{i}") for i in range(12)]
    qkv = cacc; bqkv = bcacc
    sq = sbt(mx, "sq", [128, 8, 128]); bsq = Buf("sq")
    rs = sbt(mx, "rs", [128, 8, 128]); brs = Buf("rs")
    abr = sbt(mx, "abr", [128, 16]); babr = Buf("abr")
    gbt = [sbt(mx, f"gbt{i}", [128, 24]) for i in range(NBUF)]; bgbt = [Buf(f"gbt{i}") for i in range(NBUF)]
    kT = [sbt(mx, f"kT{i}", [128, 4, 128]) for i in range(NBUF)]; bkT = [Buf(f"kT{i}") for i in range(NBUF)]
    kdec = [sbt(mx, f"kdec{i}", [128, 4, 128], BF16) for i in range(NBUF)]; bkdec = [Buf(f"kdec{i}") for i in range(NBUF)]
    ck = [sbt(mx, f"ck{i}", [128, 4, 128], BF16) for i in range(NBUF)]; bck = [Buf(f"ck{i}") for i in range(NBUF)]
    vb = [sbt(mx, f"vb{i}", [128, 4, 128], BF16) for i in range(NBUF)]; bvb = [Buf(f"vb{i}") for i in range(NBUF)]
    qdT = [sbt(mx, f"qdT{i}", [128, 4, 128]) for i in range(NBUF)]; bqdT = [Buf(f"qdT{i}") for i in range(NBUF)]
    LT = [sbt(mx, f"LT{i}", [128, 4, 128], BF16) for i in range(NBUF)]; bLT = [Buf(f"LT{i}") for i in range(NBUF)]
    Erow = [sbt(mx, f"Erow{i}", [128, 4, 128]) for i in range(NBUF)]; bErow = [Buf(f"Erow{i}") for i in range(NBUF)]
    DTi = sbt(mx, "DTi", [128, 4, 128]); bDTi = Buf("DTi")
    BU = sbt(mx, "BU", [128, 4, 128]); bBU = Buf("BU")
    atT = [sbt(mx, f"atT{i}", [128, 4, 128]) for i in range(NBUF)]; batT = [Buf(f"atT{i}") for i in range(NBUF)]
    Pm = [sbt(mx, f"Pm{i}", [128, 4, 128], BF16) for i in range(2)]; bPm = [Buf(f"Pm{i}") for i in range(2)]
    PTm = [sbt(mx, f"PTm{i}", [128, 4, 128], BF16) for i in range(2)]; bPTm = [Buf(f"PTm{i}") for i in range(2)]
    Xm = [sbt(mx, f"Xm{i}", [128, 4, 128], BF16) for i in range(2)]; bXm = [Buf(f"Xm{i}") for i in range(2)]
    A1m = sbt(mx, "A1m", [128, 4, 128], BF16); bA1m = Buf("A1m")
    U1m = sbt(mx, "U1m", [128, 4, 128], BF16); bU1m = Buf("U1m")
    MT = [sbt(mx, f"MT{i}", [128, 4, 128]) for i in range(2)]; bMT = [Buf(f"MT{i}") for i in range(2)]
    Cm = [sbt(mx, f"Cm{i}", [128, 4, 128]) for i in range(2)]; bCm = [Buf(f"Cm{i}") for i in range(2)]
    eI = sbt(mx, "eI", [128, 4, 128]); beI = Buf("eI")
    Sst = [sbt(mx, f"Sst{i}", [128, 4, 128]) for i in range(2)]; bSst = [Buf(f"Sst{i}") for i in range(2)]
    rr = sbt(mx, "rr", [128, 4, 128], BF16); brr = Buf("rr")
    vn = sbt(mx, "vn", [128, 4, 128]); bvn = Buf("vn")
    osb = sbt(mx, "osb", [128, 4, 128]); bosb = Buf("osb")
    uu = sbt(mx, "uu", [128, 512]); buu = Buf("uu")
    vv = sbt(mx, "vv", [128, 512]); bvv = Buf("vv")
    zz = sbt(mx, "zz", [128, 512]); bzz = Buf("zz")
    mix = sbt(mx, "mix", [128, 1024], BF16); bmix = Buf("mix")
    mixT = sbt(mx, "mixT", [128, 8, 128], BF16); bmixT = Buf("mixT")
    x1t = sbt(mx, "x1t", [128, 1024]); bx1t = Buf("x1t")

    if NPREV > 0:
        D_(lambda e: e.dma_start(out=pmk[:], in_=pmask[:, :]), w=[bpmk], buf=bpmk)
    G_(lambda e: e.memset(cin[:], 0.0), w=[bcin])
    G_(lambda e: e.memset(Sst[0][:], 0.0), w=[bSst[0]])
    scur = [0]
    mtc = [0]

    def stage1(ti, own, g):
        b = g % NBUF
        need_q = own or (ti == NPREV - 1)
        c0 = 0 if need_q else 4
        src = x_own[ti * 128:(ti + 1) * 128, :] if own else x_prev[ti * 128:(ti + 1) * 128, :]
        D_(lambda e: e.dma_start(out=xt[b][:], in_=src), w=[bxt[b]], buf=bxt[b])
        yield
        A_(lambda e: e.activation(out=junk[:], in_=xt[b][:], func=AF.Square, accum_out=st[b][:, 0:1]), r=[bxt[b]], w=[bjunk, bst[b]])
        A_(lambda e: e.activation(out=st[b][:, 1:2], in_=st[b][:, 0:1], func=AF.Ln, bias=epsc[:, 0:1], scale=1.0 / 1024), r=[bst[b], bepsc], w=[bst[b]])
        A_(lambda e: e.activation(out=st[b][:, 2:3], in_=st[b][:, 1:2], func=AF.Exp, scale=-0.5), r=[bst[b]], w=[bst[b]])
        yield
        V_(lambda e: e.scalar_tensor_tensor(out=junk[:], in0=xt[b][:], scalar=st[b][:, 2:3], in1=A1[:], op0=ALU.mult, op1=ALU.mult),
           r=[bxt[b], bst[b], bA1], w=[bjunk])
        if own:
            V_(lambda e: e.tensor_tensor(out=hb[b][:], in0=junk[:], in1=B1[:], op=ALU.add), r=[bjunk, bB1], w=[bhb[b]])
        else:
            V_(lambda e: e.scalar_tensor_tensor(out=hb[b][:], in0=B1[:], scalar=pmk[:, ti:ti + 1], in1=junk[:], op0=ALU.mult, op1=ALU.add),
               r=[bjunk, bB1, bpmk], w=[bhb[b]])
        yield
        for kc in range(8):
            T_(lambda e, kc=kc: e.matmul(ps[kc // 4][:, (kc % 4) * 128:(kc % 4 + 1) * 128],
                                         lhsT=hb[b][:, kc * 128:(kc + 1) * 128], rhs=idb[:], start=True, stop=True), r=[bhb[b], bidb], w=[bps[kc // 4]])
        A_(lambda e: e.activation(out=hT[b][:, 0:4, :], in_=ps[0][:, :].rearrange("p (a c) -> p a c", a=4), func=AF.Copy), r=[bps[0]], w=[bhT[b]])
        V_(lambda e: e.tensor_copy(out=hT[b][:, 4:8, :], in_=ps[1][:, :].rearrange("p (a c) -> p a c", a=4)), r=[bps[1]], w=[bhT[b]])
        yield
        for ch in range(c0, 12):
            bank = ch // 4
            for kc in range(8):
                T_(lambda e, ch=ch, kc=kc, bank=bank: e.matmul(ps[bank][:, (ch % 4) * 128:(ch % 4 + 1) * 128], lhsT=wqkv[:, kc, ch * 128:(ch + 1) * 128],
                                                              rhs=hT[b][:, kc, :], start=(kc == 0), stop=(kc == 7)), r=[bwqkv, bhT[b]], w=[bps[bank]])
            if ch % 4 == 3:
                eng = A_ if bank != 1 else V_
                if bank != 1:
                    A_(lambda e, bank=bank: e.activation(out=cin[:, bank * 4:(bank + 1) * 4, 3:131], in_=ps[bank][:, :].rearrange("p (a c) -> p a c", a=4), func=AF.Copy),
                       r=[bps[bank]], w=[bcin])
                else:
                    V_(lambda e, bank=bank: e.tensor_copy(out=cin[:, bank * 4:(bank + 1) * 4, 3:131], in_=ps[bank][:, :].rearrange("p (a c) -> p a c", a=4)),
                       r=[bps[bank]], w=[bcin])
                yield
        for kc in range(8):
            T_(lambda e, kc=kc: e.matmul(ps[3][:, 0:64], lhsT=hT[b][:, kc, :], rhs=wab[:, kc, :], start=(kc == 0), stop=(kc == 7)), r=[bwab, bhT[b]], w=[bps[3]])
        V_(lambda e: e.tensor_tensor(out=abr[:, 0:4], in0=ps[3][:, 0:4], in1=gp[:, 4:8], op=ALU.add), r=[bps[3], bgp], w=[babr])
        A_(lambda e: e.activation(out=abr[:, 0:4], in_=abr[:, 0:4], func=AF.Exp), r=[babr], w=[babr])
        A_(lambda e: e.activation(out=abr[:, 4:8], in_=ps[3][:, 32:36], func=AF.Exp, scale=-1.0), r=[bps[3]], w=[babr])
        A_(lambda e: e.activation(out=abr[:, 8:12], in_=abr[:, 0:4], func=AF.Ln, bias=1.0, scale=1.0), r=[babr], w=[babr])
        V_(lambda e: e.tensor_tensor(out=gbt[b][:, 0:4], in0=abr[:, 8:12], in1=gp[:, 8:12], op=ALU.mult), r=[babr, bgp], w=[bgbt[b]])
        V_(lambda e: e.tensor_scalar(out=abr[:, 12:16], in0=abr[:, 4:8], scalar1=1.0, scalar2=None, op0=ALU.add), r=[babr], w=[babr])
        V_(lambda e: e.reciprocal(out=gbt[b][:, 4:8], in_=abr[:, 12:16]), r=[babr], w=[bgbt[b]])
        yield
        for k in range(4):
            for ch in range(c0, 12):
                if k == 0:
                    V_(lambda e, ch=ch: e.tensor_scalar(out=cacc[:, ch, :], in0=cin[:, ch, 0:128], scalar1=cw[:, ch, 0:1], scalar2=None, op0=ALU.mult),
                       r=[bcin, bcw], w=[bcaccs[ch], bcacc])
                else:
                    V_(lambda e, ch=ch, k=k: e.scalar_tensor_tensor(out=cacc[:, ch, :], in0=cin[:, ch, k:k + 128], scalar=cw[:, ch, k:k + 1], in1=cacc[:, ch, :],
                                                                    op0=ALU.mult, op1=ALU.add), r=[bcin, bcw, bcaccs[ch]], w=[bcaccs[ch]])
            yield
        G_(lambda e: e.tensor_copy(out=cin[:, :, 0:3], in_=cin[:, :, 128:131]), r=[bcin], w=[bcin])
        A_(lambda e: e.activation(out=qkv[:, c0:12, :], in_=cacc[:, c0:12, :], func=AF.Silu), r=[bcacc] + bcaccs[c0:12], w=[bcacc])
        yield
        A_(lambda e: e.activation(out=sq[:, c0:8, :], in_=qkv[:, c0:8, :], func=AF.Square), r=[bqkv], w=[bsq])
        for hf in range(c0 // 4, 2):
            T_(lambda e, hf=hf: e.matmul(ps[hf][:, :], lhsT=ones, rhs=sq[:, hf * 4:(hf + 1) * 4, :].rearrange("p a c -> p (a c)"), start=True, stop=True),
               r=[bcst, bsq], w=[bps[hf]])
            A_(lambda e, hf=hf: e.activation(out=rs[:, hf * 4:(hf + 1) * 4, :].rearrange("p a c -> p (a c)"), in_=ps[hf][:, :], func=AF.Ln, bias=epsc[:, 0:1], scale=1.0),
               r=[bps[hf], bepsc], w=[brs])
        A_(lambda e: e.activation(out=rs[:, c0:8, :], in_=rs[:, c0:8, :], func=AF.Exp, scale=-0.5), r=[brs], w=[brs])
        yield
        T_(lambda e: e.matmul(ps[2][:, 8:12], lhsT=triU, rhs=gbt[b][:, 0:4], start=True, stop=True), r=[bcst, bgbt[b]], w=[bps[2]])
        T_(lambda e: e.matmul(ps[2][:, 12:16], lhsT=blk, rhs=gbt[b][:, 0:4], start=True, stop=True), r=[bcst, bgbt[b]], w=[bps[2]])
        V_(lambda e: e.tensor_copy(out=gbt[b][:, 8:16], in_=ps[2][:, 8:16]), r=[bps[2]], w=[bgbt[b]])
        for h in range(4):
            T_(lambda e, h=h: e.matmul(ps[3][:, h * 128:(h + 1) * 128], lhsT=gbt[b][:, h:h + 1].to_broadcast([128, 128]), rhs=triU, start=True, stop=True),
               r=[bgbt[b], bcst], w=[bps[3]])
        yield
        import os as _os; _sk = _os.environ.get('KSKIP', '')
        if 'cols' not in _sk:
            A_(lambda e: e.activation(out=gbt[b][:, 16:20], in_=gbt[b][:, 8:12], func=AF.Exp), r=[bgbt[b]], w=[bgbt[b]])
            V_(lambda e: e.scalar_tensor_tensor(out=gbt[b][:, 16:20], in0=gbt[b][:, 16:20], scalar=-1.0, in1=gbt[b][:, 4:8], op0=ALU.mult, op1=ALU.mult),
               r=[bgbt[b]], w=[bgbt[b]])
            V_(lambda e: e.tensor_tensor(out=gbt[b][:, 20:24], in0=gbt[b][:, 12:16], in1=gbt[b][:, 8:12], op=ALU.subtract), r=[bgbt[b]], w=[bgbt[b]])
            A_(lambda e: e.activation(out=gbt[b][:, 20:24], in_=gbt[b][:, 20:24], func=AF.Exp), r=[bgbt[b]], w=[bgbt[b]])
        if 'erow' not in _sk:
            A_(lambda e: e.activation(out=Erow[b][:].rearrange("p a c -> p (a c)"), in_=ps[3][:, :], func=AF.Exp), r=[bps[3]], w=[bErow[b]])
        if 'dti' not in _sk:
            V_(lambda e: e.tensor_tensor(out=DTi[:], in0=ps[3][:, :].rearrange("p (a c) -> p a c", a=4), in1=bc3(negU, 4, 1), op=ALU.add), r=[bps[3], bcst], w=[bDTi])
            V_(lambda e: e.tensor_tensor(out=DTi[:], in0=DTi[:], in1=bc3(gbt[b][:, 8:12], 128, 2), op=ALU.subtract), r=[bDTi, bgbt[b]], w=[bDTi])
            A_(lambda e: e.activation(out=DTi[:], in_=DTi[:], func=AF.Exp), r=[bDTi], w=[bDTi])
            yield
        for h in range(4):
            T_(lambda e, h=h: e.matmul(ps[2][:, h * 128:(h + 1) * 128], lhsT=gbt[b][:, 4 + h:5 + h].to_broadcast([128, 128]), rhs=ident, start=True, stop=True),
               r=[bcst, bgbt[b]], w=[bps[2]])
        V_(lambda e: e.tensor_tensor(out=BU[:], in0=ps[2][:, :].rearrange("p (a c) -> p a c", a=4), in1=bc3(sU, 4, 1), op=ALU.mult), r=[bps[2], bcst], w=[bBU])
        V_(lambda e: e.tensor_tensor(out=kT[b][:], in0=qkv[:, 4:8, :], in1=rs[:, 4:8, :], op=ALU.mult), r=[bqkv, brs], w=[bkT[b]])
        if own:
            V_(lambda e: e.scalar_tensor_tensor(out=qdT[b][:], in0=qkv[:, 0:4, :], scalar=128.0 ** -0.5, in1=rs[:, 0:4, :], op0=ALU.mult, op1=ALU.mult),
               r=[bqkv, brs], w=[bqdT[b]])
        yield
        for h in range(4):
            T_(lambda e, h=h: e.matmul(ps[0][:, h * 128:(h + 1) * 128], lhsT=kT[b][:, h, :], rhs=kT[b][:, h, :], start=True, stop=True), r=[bkT[b]], w=[bps[0]])
        if own:
            for h in range(4):
                T_(lambda e, h=h: e.matmul(ps[1][:, h * 128:(h + 1) * 128], lhsT=kT[b][:, h, :], rhs=qdT[b][:, h, :], start=True, stop=True), r=[bkT[b], bqdT[b]], w=[bps[1]])
        V_(lambda e: e.tensor_tensor(out=LT[b][:], in0=ps[0][:, :].rearrange("p (a c) -> p a c", a=4), in1=DTi[:], op=ALU.mult), r=[bps[0], bDTi], w=[bLT[b]])
        V_(lambda e: e.tensor_tensor(out=LT[b][:], in0=LT[b][:], in1=BU[:], op=ALU.mult), r=[bLT[b], bBU], w=[bLT[b]])
        if own:
            V_(lambda e: e.tensor_tensor(out=atT[b][:], in0=ps[1][:, :].rearrange("p (a c) -> p a c", a=4), in1=DTi[:], op=ALU.mult), r=[bps[1], bDTi], w=[batT[b]])
            G_(lambda e: e.tensor_tensor(out=qdT[b][:], in0=qdT[b][:], in1=Erow[b][:], op=ALU.mult), r=[bqdT[b], bErow[b]], w=[bqdT[b]])
        yield
        for h in range(4):
            T_(lambda e, h=h: e.matmul(ps[2][:, h * 128:(h + 1) * 128], lhsT=kT[b][:, h, :], rhs=ident, start=True, stop=True), r=[bkT[b], bcst], w=[bps[2]])
        for h in range(4):
            T_(lambda e, h=h: e.matmul(ps[3][:, h * 128:(h + 1) * 128], lhsT=qkv[:, 8 + h, :], rhs=ident, start=True, stop=True), r=[bqkv, bcst], w=[bps[3]])
        V_(lambda e: e.tensor_tensor(out=kdec[b][:], in0=ps[2][:, :].rearrange("p (a c) -> p a c", a=4), in1=bc3(gbt[b][:, 20:24], 128, 2), op=ALU.mult),
           r=[bps[2], bgbt[b]], w=[bkdec[b]])
        V_(lambda e: e.tensor_tensor(out=ck[b][:], in0=ps[2][:, :].rearrange("p (a c) -> p a c", a=4), in1=bc3(gbt[b][:, 16:20], 128, 2), op=ALU.mult),
           r=[bps[2], bgbt[b]], w=[bck[b]])
        V_(lambda e: e.tensor_tensor(out=vb[b][:], in0=ps[3][:, :].rearrange("p (a c) -> p a c", a=4), in1=bc3(gbt[b][:, 4:8], 128, 2), op=ALU.mult),
           r=[bps[3], bgbt[b]], w=[bvb[b]])
        yield

    def stage2(ti, own, g):
        b = g % NBUF
        for h in range(4):
            T_(lambda e, h=h: e.matmul(ps[4][:, h * 128:(h + 1) * 128], lhsT=LT[b][:, h, :], rhs=idb[:], start=True, stop=True), r=[bLT[b], bidb], w=[bps[4]])
        A_(lambda e: e.activation(out=Pm[0][:].rearrange("p a c -> p (a c)"), in_=ps[4][:, :], func=AF.Copy), r=[bps[4]], w=[bPm[0]])
        G_(lambda e: e.tensor_tensor(out=Xm[0][:], in0=bc3(idb[:], 4, 1), in1=LT[b][:], op=ALU.subtract), r=[bidb, bLT[b]], w=[bXm[0]])
        yield
        cur = 0
        PTl = [LT[b], PTm[1]]; bPTl = [bLT[b], bPTm[1]]
        for lvl in range(5):
            nxt = 1 - cur
            if lvl == 1:
                PTl[0] = PTm[0]; bPTl[0] = bPTm[0]
            for h in range(4):
                T_(lambda e, h=h, cur=cur, pt=PTl[cur]: e.matmul(ps[4][:, h * 128:(h + 1) * 128], lhsT=pt[:, h, :], rhs=Pm[cur][:, h, :], start=True, stop=True),
                   r=[bPTl[cur], bPm[cur]], w=[bps[4]])
            if lvl < 4:
                for h in range(4):
                    T_(lambda e, h=h, cur=cur, pt=PTl[cur]: e.matmul(ps[5][:, h * 128:(h + 1) * 128], lhsT=Pm[cur][:, h, :], rhs=pt[:, h, :], start=True, stop=True),
                       r=[bPTl[cur], bPm[cur]], w=[bps[5]])
            A_(lambda e, nxt=nxt: e.activation(out=Pm[nxt][:].rearrange("p a c -> p (a c)"), in_=ps[4][:, :], func=AF.Copy), r=[bps[4]], w=[bPm[nxt]])
            if lvl < 4:
                V_(lambda e, nxt=nxt: e.tensor_copy(out=PTm[nxt][:].rearrange("p a c -> p (a c)"), in_=ps[5][:, :]), r=[bps[5]], w=[bPTm[nxt]])
                PTl[nxt] = PTm[nxt]; bPTl[nxt] = bPTm[nxt]
            yield
            for h in range(4):
                T_(lambda e, h=h, cur=cur: e.matmul(ps[4][:, h * 128:(h + 1) * 128], lhsT=idb[:], rhs=Xm[cur][:, h, :], start=True, stop=False),
                   r=[bidb, bXm[cur]], w=[bps[4]])
                T_(lambda e, h=h, cur=cur, nxt=nxt: e.matmul(ps[4][:, h * 128:(h + 1) * 128], lhsT=Pm[nxt][:, h, :], rhs=Xm[cur][:, h, :], start=False, stop=True),
                   r=[bPm[nxt], bXm[cur]], w=[bps[4]])
            V_(lambda e, nxt=nxt: e.tensor_copy(out=Xm[nxt][:].rearrange("p a c -> p (a c)"), in_=ps[4][:, :]), r=[bps[4]], w=[bXm[nxt]])
            yield
            cur = nxt
        TT = Xm[cur]; bTT = bXm[cur]
        for h in range(4):
            T_(lambda e, h=h: e.matmul(ps[4][:, h * 128:(h + 1) * 128], lhsT=TT[:, h, :], rhs=ck[b][:, h, :], start=True, stop=True), r=[bTT, bck[b]], w=[bps[4]])
        for h in range(4):
            T_(lambda e, h=h: e.matmul(ps[5][:, h * 128:(h + 1) * 128], lhsT=TT[:, h, :], rhs=vb[b][:, h, :], start=True, stop=True), r=[bTT, bvb[b]], w=[bps[5]])
        A_(lambda e: e.activation(out=A1m[:].rearrange("p a c -> p (a c)"), in_=ps[4][:, :], func=AF.Copy), r=[bps[4]], w=[bA1m])
        V_(lambda e: e.tensor_copy(out=U1m[:].rearrange("p a c -> p (a c)"), in_=ps[5][:, :]), r=[bps[5]], w=[bU1m])
        yield
        res = []
        for c in range(2):
            m = mtc[0] % 2
            mtc[0] += 1
            rows = slice(64 * c, 64 * c + 64)
            for h in range(4):
                T_(lambda e, h=h, rows=rows: e.matmul(ps[4][:, h * 128:(h + 1) * 128], lhsT=A1m[rows, h, :], rhs=kdec[b][rows, h, :], start=True, stop=True),
                   r=[bA1m, bkdec[b]], w=[bps[4]])
            for h in range(4):
                T_(lambda e, h=h, rows=rows: e.matmul(ps[5][:, h * 128:(h + 1) * 128], lhsT=kdec[b][rows, h, :], rhs=U1m[rows, h, :], start=True, stop=True),
                   r=[bU1m, bkdec[b]], w=[bps[5]])
            col = 64 * c + 63
            G_(lambda e, col=col: e.tensor_tensor(out=eI[:], in0=bc3(ident, 4, 1), in1=Erow[b][:, :, col:col + 1].to_broadcast([128, 4, 128]), op=ALU.mult),
               r=[bcst, bErow[b]], w=[beI])
            V_(lambda e, m=m: e.tensor_tensor(out=MT[m][:], in0=ps[4][:, :].rearrange("p (a c) -> p a c", a=4), in1=eI[:], op=ALU.add), r=[bps[4], beI], w=[bMT[m]])
            A_(lambda e, m=m: e.activation(out=Cm[m][:].rearrange("p a c -> p (a c)"), in_=ps[5][:, :], func=AF.Copy), r=[bps[5]], w=[bCm[m]])
            res.append(m)
            yield
        stage2.out[g] = (res, TT, bTT)
    stage2.out = {}

    def stage3(ti, own, g):
        b = g % NBUF
        res, TT, bTT = stage2.out[g]
        for c in range(2):
            m = res[c]
            rows = slice(64 * c, 64 * c + 64)
            s0 = scur[0]
            s1 = 1 - s0
            if own:
                for h in range(4):
                    T_(lambda e, h=h, s0=s0: e.matmul(ps[6][:, h * 128:(h + 1) * 128], lhsT=kT[b][:, h, :], rhs=Sst[s0][:, h, :], start=True, stop=True),
                       r=[bkT[b], bSst[s0]], w=[bps[6]])
                V_(lambda e, rows=rows: e.tensor_tensor(out=rr[rows], in0=ps[6][rows, :].rearrange("p (a c) -> p a c", a=4), in1=bc3(gbt[b][rows, 16:20], 128, 2), op=ALU.mult),
                   r=[bps[6], bgbt[b]], w=[brr])
                V_(lambda e, rows=rows: e.tensor_tensor(out=rr[rows], in0=rr[rows], in1=vb[b][rows], op=ALU.add), r=[brr, bvb[b]], w=[brr])
                yield
                for h in range(4):
                    T_(lambda e, h=h, rows=rows: e.matmul(ps[6][:, h * 128:(h + 1) * 128], lhsT=TT[rows, h, :], rhs=rr[rows, h, :], start=True, stop=True),
                       r=[bTT, brr], w=[bps[6]])
                A_(lambda e, rows=rows: e.activation(out=vn[rows].rearrange("p a c -> p (a c)"), in_=ps[6][rows, :], func=AF.Copy), r=[bps[6]], w=[bvn])
                yield
                for h in range(4):
                    T_(lambda e, h=h, s0=s0: e.matmul(ps[6][:, h * 128:(h + 1) * 128], lhsT=qdT[b][:, h, :], rhs=Sst[s0][:, h, :], start=True, stop=False),
                       r=[bqdT[b], bSst[s0]], w=[bps[6]])
                    T_(lambda e, h=h, rows=rows: e.matmul(ps[6][:, h * 128:(h + 1) * 128], lhsT=atT[b][rows, h, :], rhs=vn[rows, h, :], start=False, stop=True),
                       r=[batT[b], bvn], w=[bps[6]])
                A_(lambda e, rows=rows: e.activation(out=osb[rows].rearrange("p a c -> p (a c)"), in_=ps[6][rows, :], func=AF.Copy), r=[bps[6]], w=[bosb])
                yield
            for h in range(4):
                T_(lambda e, h=h, m=m: e.matmul(ps[7][:, h * 128:(h + 1) * 128], lhsT=ident, rhs=Cm[m][:, h, :], start=True, stop=False), r=[bcst, bCm[m]], w=[bps[7]])
                T_(lambda e, h=h, m=m, s0=s0: e.matmul(ps[7][:, h * 128:(h + 1) * 128], lhsT=MT[m][:, h, :], rhs=Sst[s0][:, h, :], start=False, stop=True),
                   r=[bMT[m], bSst[s0]], w=[bps[7]])
            V_(lambda e, s1=s1: e.tensor_copy(out=Sst[s1][:].rearrange("p a c -> p (a c)"), in_=ps[7][:, :]), r=[bps[7]], w=[bSst[s1]])
            scur[0] = s1
            yield

    def stage_tok(ti, g):
        b = g % NBUF
        for gi in range(3):
            for kc in range(8):
                T_(lambda e, gi=gi, kc=kc: e.matmul(ps[gi][:, :], lhsT=hT[b][:, kc, :], rhs=wtok[:, kc, gi * 512:(gi + 1) * 512], start=(kc == 0), stop=(kc == 7)),
                   r=[bhT[b], bwtok], w=[bps[gi]])
        A_(lambda e: e.activation(out=uu[:], in_=ps[0][:, :], func=AF.Gelu_apprx_tanh), r=[bps[0]], w=[buu])
        A_(lambda e: e.activation(out=vv[:], in_=ps[1][:, :], func=AF.Gelu_apprx_tanh), r=[bps[1]], w=[bvv])
        A_(lambda e: e.activation(out=zz[:], in_=ps[2][:, :], func=AF.Silu), r=[bps[2]], w=[bzz])
        yield
        A_(lambda e: e.activation(out=junk2[:], in_=vv[:], func=AF.Square, accum_out=st[b][:, 4:5]), r=[bvv], w=[bjunk2, bst[b]])
        A_(lambda e: e.activation(out=st[b][:, 5:6], in_=st[b][:, 4:5], func=AF.Sqrt, bias=epsc[:, 0:1], scale=1.0 / 512), r=[bst[b], bepsc], w=[bst[b]])
        V_(lambda e: e.reciprocal(out=st[b][:, 6:7], in_=st[b][:, 5:6]), r=[bst[b]], w=[bst[b]])
        V_(lambda e: e.scalar_tensor_tensor(out=vv[:], in0=vv[:], scalar=st[b][:, 6:7], in1=gmn[:], op0=ALU.mult, op1=ALU.mult), r=[bvv, bst[b], bgmn], w=[bvv])
        yield
        for h in range(8):
            T_(lambda e, h=h: e.matmul(ps[0][:, h * 64:(h + 1) * 64], lhsT=wsm[:, h, :], rhs=vv[:, h * 64:(h + 1) * 64], start=True, stop=True), r=[bwsm, bvv], w=[bps[0]])
        V_(lambda e: e.tensor_tensor(out=junk2[:], in0=ps[0][:, :], in1=bsb[:], op=ALU.add), r=[bps[0], bbsb], w=[bjunk2])
        V_(lambda e: e.tensor_tensor(out=mix[:, 0:512], in0=junk2[:], in1=uu[:], op=ALU.mult), r=[bjunk2, buu], w=[bmix])
        yield

    def stage_out(ti, g):
        b = g % NBUF
        for h in range(4):
            A_(lambda e, h=h: e.activation(out=junk2[:, h * 128:(h + 1) * 128], in_=osb[:, h, :], func=AF.Square, accum_out=st[b][:, 4 + h:5 + h]), r=[bosb], w=[bjunk2, bst[b]])
        A_(lambda e: e.activation(out=st[b][:, 4:8], in_=st[b][:, 4:8], func=AF.Sqrt, bias=epsc[:, 0:1], scale=1.0 / 128), r=[bst[b], bepsc], w=[bst[b]])
        V_(lambda e: e.reciprocal(out=st[b][:, 4:8], in_=st[b][:, 4:8]), r=[bst[b]], w=[bst[b]])
        V_(lambda e: e.tensor_tensor(out=osb[:], in0=osb[:], in1=bc3(st[b][:, 4:8], 128, 2), op=ALU.mult), r=[bosb, bst[b]], w=[bosb])
        V_(lambda e: e.tensor_tensor(out=osb[:], in0=osb[:], in1=bc3(onb[:], 4, 1), op=ALU.mult), r=[bosb, bonb], w=[bosb])
        V_(lambda e: e.tensor_tensor(out=mix[:, 512:1024], in0=osb[:].rearrange("p a c -> p (a c)"), in1=zz[:], op=ALU.mult), r=[bosb, bzz], w=[bmix])
        yield
        for kc in range(8):
            T_(lambda e, kc=kc: e.matmul(ps[kc // 4][:, (kc % 4) * 128:(kc % 4 + 1) * 128], lhsT=mix[:, kc * 128:(kc + 1) * 128], rhs=idb[:], start=True, stop=True),
               r=[bmix, bidb], w=[bps[kc // 4]])
        A_(lambda e: e.activation(out=mixT[:, 0:4, :], in_=ps[0][:, :].rearrange("p (a c) -> p a c", a=4), func=AF.Copy), r=[bps[0]], w=[bmixT])
        V_(lambda e: e.tensor_copy(out=mixT[:, 4:8, :], in_=ps[1][:, :].rearrange("p (a c) -> p a c", a=4)), r=[bps[1]], w=[bmixT])
        yield
        for hf in range(2):
            for kc in range(8):
                T_(lambda e, hf=hf, kc=kc: e.matmul(ps[2 + hf][:, :], lhsT=mixT[:, kc, :], rhs=wout[:, kc, hf * 512:(hf + 1) * 512], start=(kc == 0), stop=(kc == 7)),
                   r=[bmixT, bwout], w=[bps[2 + hf]])
            cs = slice(hf * 512, (hf + 1) * 512)
            V_(lambda e, hf=hf, cs=cs: e.tensor_tensor(out=x1t[:, cs], in0=ps[2 + hf][:, :], in1=GT1[:, cs], op=ALU.mult), r=[bps[2 + hf], bGT1], w=[bx1t])
        V_(lambda e: e.tensor_tensor(out=x1t[:], in0=x1t[:], in1=xt[b][:], op=ALU.add), r=[bx1t, bxt[b]], w=[bx1t])
        D_(lambda e: e.dma_start(out=x1s[ti * 128:(ti + 1) * 128, :], in_=x1t[:]), r=[bx1t], w=[bx1s], buf=bx1s)
        yield

    tiles = [(t, False) for t in range(NPREV)] + [(t, True) for t in range(NT)]

    def tile_rest(ti, own, g):
        if phase == 11:
            return
        yield from stage2(ti, own, g)
        if own and phase not in (12, 13):
            yield from stage_tok(ti, g)
        if phase == 12:
            return
        yield from stage3(ti, own, g)
        if own and phase not in (13, 14):
            yield from stage_out(ti, g)

    import os, itertools
    CUT = int(os.environ.get("KCUT", "1000"))
    _stage1 = stage1
    stage1 = lambda a, b_, c_: itertools.islice(_stage1(a, b_, c_), CUT)
    run_interleaved([stage1(tiles[0][0], tiles[0][1], 0)])
    for g, (ti, own) in enumerate(tiles):
        gens = [tile_rest(ti, own, g)]
        if g + 1 < len(tiles):
            gens.append(stage1(tiles[g + 1][0], tiles[g + 1][1], g + 1))
        run_interleaved(gens)

    P.barrier([bx1s, bcin, bSst[0], bSst[1], bpmk])
    if phase in (1, 11, 12, 13, 14):
        P.op("sync", lambda e: e.nop(), r=[bx1s])
        P.emit(); mx.close(); top.close()
        return nc
    mx.close()

    pe = ExitStack()
    import os as _os3
    KDBG = _os3.environ.get("KDBG", "")
    s2a = sbt(pe, "s2a", [128, GT, 8, 128]); bs2a = [Buf(f"s2a{i}") for i in range(GT)]
    aa = sbt(pe, "aa", [128, GT, 8, 128]); baa = [Buf(f"aa{i}") for i in range(GT)]
    kap = sbt(pe, "kap", [128, GT, 8]); bkap = [Buf(f"kap{i}") for i in range(GT)]
    tau = sbt(pe, "tau", [128, GT, 8])
    h2T = sbt(pe, "h2T", [128, 8, GT * 128], BF16); bh2T = Buf("h2T")
    acc = sbt(pe, "acc", [128, GT, 1024]); bacc = [Buf(f"acc{i}") for i in range(GT)]
    x1g = sbt(pe, "x1g", [128, GT, 1024]); bx1g = [Buf(f"x1g{i}") for i in range(GT)]
    pst = sbt(pe, "pst", [128, 8]); bpst = Buf("pst")
    Ub = [sbt(pe, f"Ub{i}", [128, 8, 1024], BF16) for i in range(2)]; bUb = [Buf(f"Ub{i}") for i in range(2)]

    def load_U(bi):
        sl = bi % 2
        D_(lambda e: e.dma_start(out=Ub[sl][:], in_=UT[:, bi * 1024:(bi + 1) * 1024].rearrange("(kc p) n -> p kc n", p=128)), w=[bUb[sl]], buf=bUb[sl], eng="gpsimd")
    A2 = sbt(pe, "A2", [128, 1024]); B2 = sbt(pe, "B2", [128, 1024])
    GT2 = sbt(pe, "GT2", [128, 1024]); FNW = sbt(pe, "FNW", [128, 1024])
    bA2, bB2, bGT2, bFNW = Buf("A2"), Buf("B2"), Buf("GT2"), Buf("FNW")
    D_(lambda e: e.dma_start(out=A2[:], in_=modscr[0:128, :]), r=[bmodscr], w=[bA2], buf=bA2)
    D_(lambda e: e.dma_start(out=B2[:], in_=modscr[128:256, :]), r=[bmodscr], w=[bB2], buf=bB2)
    D_(lambda e: e.dma_start(out=GT2[:], in_=modscr[256:384, :]), r=[bmodscr], w=[bGT2], buf=bGT2)
    D_(lambda e: e.dma_start(out=FNW[:], in_=fnw[:, :]), w=[bFNW], buf=bFNW)

    for grp in range(NG):
        with ExitStack() as rt:
            wqs = [sbt(rt, f"wqs{i}", [128, 8, 256]) for i in range(2)]; bwqs = [Buf(f"wqs{i}") for i in range(2)]
            h2f = sbt(rt, "h2f", [128, 8, GT * 128]); bh2f = Buf("h2f")
            kys = sbt(rt, "kys", [128, 16, 128]); bkys = Buf("kys")
            h2 = sbt(rt, "h2", [128, 1024]); bh2 = Buf("h2")
            jk = sbt(rt, "jk", [128, 1024]); bjk = Buf("jk")
            e16 = sbt(rt, "e16", [128, 8, 16]); be16 = Buf("e16")
            zz8 = sbt(rt, "zz8", [128, 16]); bzz8 = Buf("zz8")
            D_(lambda e: e.dma_start(out=kys[:], in_=keysT.rearrange("p (c k) -> p c k", c=16)), w=[bkys], buf=bkys)
            for tt in range(GT):
                ti = grp * GT + tt
                D_(lambda e, tt=tt, ti=ti: e.dma_start(out=x1g[:, tt, :], in_=x1s[ti * 128:(ti + 1) * 128, :]), r=[bx1s], w=[bx1g[tt]], buf=bx1g[tt])
                A_(lambda e, tt=tt: e.activation(out=jk[:], in_=x1g[:, tt, :], func=AF.Square, accum_out=pst[:, 0:1]), r=[bx1g[tt]], w=[bjk, bpst])
                A_(lambda e: e.activation(out=pst[:, 1:2], in_=pst[:, 0:1], func=AF.Sqrt, bias=epsc[:, 0:1], scale=1.0 / 1024), r=[bpst, bepsc], w=[bpst])
                V_(lambda e: e.reciprocal(out=pst[:, 2:3], in_=pst[:, 1:2]), r=[bpst], w=[bpst])
                V_(lambda e, tt=tt: e.scalar_tensor_tensor(out=jk[:], in0=x1g[:, tt, :], scalar=pst[:, 2:3], in1=A2[:], op0=ALU.mult, op1=ALU.mult),
                   r=[bx1g[tt], bpst, bA2], w=[bjk])
                V_(lambda e: e.tensor_tensor(out=h2[:], in0=jk[:], in1=B2[:], op=ALU.add), r=[bjk, bB2], w=[bh2])
                for kc in range(8):
                    T_(lambda e, kc=kc: e.matmul(ps[kc // 4][:, (kc % 4) * 128:(kc % 4 + 1) * 128], lhsT=h2[:, kc * 128:(kc + 1) * 128], rhs=ident, start=True, stop=True),
                       r=[bh2, bcst], w=[bps[kc // 4]])
                for hf in range(2):
                    A_(lambda e, hf=hf, tt=tt: e.activation(out=h2f[:, hf * 4:(hf + 1) * 4, tt * 128:(tt + 1) * 128], in_=ps[hf][:, :].rearrange("p (a c) -> p a c", a=4), func=AF.Copy),
                       r=[bps[hf]], w=[bh2f])
                    V_(lambda e, hf=hf, tt=tt: e.tensor_copy(out=h2T[:, hf * 4:(hf + 1) * 4, tt * 128:(tt + 1) * 128], in_=ps[hf][:, :].rearrange("p (a c) -> p a c", a=4)),
                       r=[bps[hf]], w=[bh2T])
            NW = GT * 128
            qTh = [sbt(rt, f"qTh{i}", [128, 2, NW]) for i in range(2)]; bqTh = [Buf(f"qTh{i}") for i in range(2)]
            c16g = sbt(rt, "c16g", [128, GT, 8, 16]); bc16g = [Buf(f"c16g{i}") for i in range(GT)]
            v12h = [sbt(rt, f"v12h{i}", [128, 32]) for i in range(2)]; bv12h = [Buf(f"v12h{i}") for i in range(2)]
            cdh = [sbt(rt, f"cdh{i}", [128, 256]) for i in range(2)]; bcdh = [Buf(f"cdh{i}") for i in range(2)]
            tkh = [sbt(rt, f"tkh{i}", [128, 128]) for i in range(2)]; btkh = [Buf(f"tkh{i}") for i in range(2)]
            bsc = [[Buf(f"sc{t_}_{h_}") for h_ in range(8)] for t_ in range(GT)]
            rc = 0
            for h in range(8):
                sl = h % 2
                D_(lambda e, h=h, sl=sl: e.dma_start(out=wqs[sl][:], in_=wq[:, h * 256:(h + 1) * 256].rearrange("(kc p) n -> p kc n", p=128)), w=[bwqs[sl]], buf=bwqs[sl])
                if h == 7 and KDBG != 'rt':
                    load_U(0)
                    load_U(1)
                for cc in range(2):
                    pb = 2 + cc
                    for kc in range(8):
                        T_(lambda e, cc=cc, kc=kc, sl=sl, pb=pb: e.matmul(ps[pb][:, 0:NW], lhsT=wqs[sl][:, kc, cc * 128:(cc + 1) * 128], rhs=h2f[:, kc, :],
                                                                          start=(kc == 0), stop=(kc == 7)), r=[bwqs[sl], bh2f], w=[bps[pb]])
                    A_(lambda e, cc=cc, sl=sl, pb=pb: e.activation(out=qTh[sl][:, cc, :], in_=ps[pb][:, 0:NW], func=AF.Copy), r=[bps[pb]], w=[bqTh[sl]])
                for tt in range(GT):
                    tsl = slice(tt * 128, (tt + 1) * 128)
                    w_ = rc % 2
                    rc += 1
                    bk = 4 + w_
                    for half in range(2):
                        T_(lambda e, half=half, bk=bk, h=h, tsl=tsl, sl=sl: e.matmul(ps[bk][:, half * 128:(half + 1) * 128], lhsT=qTh[sl][:, half, tsl], rhs=kys[:, 2 * h + half, :],
                                                                                 start=True, stop=True), r=[bqTh[sl], bkys], w=[bps[bk]])
                    A_(lambda e, tt=tt, h=h, bk=bk: e.activation(out=aa[:, tt, h, :], in_=ps[bk][:, 0:128], func=AF.Copy), r=[bps[bk]], w=[bsc[tt][h]])
                    A_(lambda e, tt=tt, h=h, bk=bk: e.activation(out=s2a[:, tt, h, :], in_=ps[bk][:, 128:256], func=AF.Copy), r=[bps[bk]], w=[bsc[tt][h]])
                    for half, src in enumerate((aa[:, tt, h, :], s2a[:, tt, h, :])):
                        o0 = half * 16
                        V_(lambda e, src=src, o0=o0, w_=w_: e.max(out=v12h[w_][:, o0:o0 + 8], in_=src), r=[bsc[tt][h]], w=[bv12h[w_]])
                        V_(lambda e, src=src, o0=o0, w_=w_: e.match_replace(out=tkh[w_][:], in_to_replace=v12h[w_][:, o0:o0 + 8], in_values=src, imm_value=-1e30),
                           r=[bsc[tt][h], bv12h[w_]], w=[btkh[w_]])
                        V_(lambda e, o0=o0, w_=w_: e.max(out=v12h[w_][:, o0 + 8:o0 + 16], in_=tkh[w_][:]), r=[btkh[w_]], w=[bv12h[w_]])
                    V_(lambda e, w_=w_: e.tensor_tensor(out=cdh[w_][:].rearrange("p (a b) -> p a b", a=16), in0=v12h[w_][:, 0:16].unsqueeze(2).to_broadcast([128, 16, 16]),
                                                        in1=v12h[w_][:, 16:32].unsqueeze(1).to_broadcast([128, 16, 16]), op=ALU.add), r=[bv12h[w_]], w=[bcdh[w_]])
                    V_(lambda e, tt=tt, h=h, w_=w_: e.max(out=c16g[:, tt, h, 0:8], in_=cdh[w_][:]), r=[bcdh[w_]], w=[bc16g[tt]])
                    V_(lambda e, tt=tt, h=h, w_=w_: e.match_replace(out=cdh[w_][:], in_to_replace=c16g[:, tt, h, 0:8], in_values=cdh[w_][:], imm_value=-1e30),
                       r=[bcdh[w_], bc16g[tt]], w=[bcdh[w_]])
                    V_(lambda e, tt=tt, h=h, w_=w_: e.max(out=c16g[:, tt, h, 8:16], in_=cdh[w_][:]), r=[bcdh[w_]], w=[bc16g[tt]])
            for tt in range(GT):
                V_(lambda e, tt=tt: e.tensor_copy(out=tau[:, tt, :], in_=c16g[:, tt, :, 15]), r=[bc16g[tt]], w=[bkap[tt]])
                V_(lambda e, tt=tt: e.tensor_tensor(out=e16[:], in0=c16g[:, tt], in1=c16g[:, tt, :, 0:1].to_broadcast([128, 8, 16]), op=ALU.subtract), r=[bc16g[tt]], w=[be16])
                A_(lambda e: e.activation(out=e16[:], in_=e16[:], func=AF.Exp), r=[be16], w=[be16])
                V_(lambda e: e.tensor_reduce(out=zz8[:, 0:8], in_=e16[:], axis=mybir.AxisListType.X, op=ALU.add), r=[be16], w=[bzz8])
                A_(lambda e: e.activation(out=zz8[:, 8:16], in_=zz8[:, 0:8], func=AF.Ln), r=[bzz8], w=[bzz8])
                V_(lambda e, tt=tt: e.tensor_tensor(out=kap[:, tt, :], in0=c16g[:, tt, :, 0], in1=zz8[:, 8:16], op=ALU.add), r=[bc16g[tt], bzz8], w=[bkap[tt]])
                V_(lambda e, tt=tt: e.tensor_scalar(out=kap[:, tt, :], in0=kap[:, tt, :], scalar1=-1.0, scalar2=None, op0=ALU.mult), r=[bkap[tt]], w=[bkap[tt]])
                G_(lambda e, tt=tt: e.memset(acc[:, tt, :], 0.0), w=[bacc[tt]])
            P.barrier([bh2f, bkys, bwqs[0], bwqs[1], be16, bzz8, bh2, bjk] + bqTh + bv12h + bcdh + btkh + bc16g)

        with ExitStack() as ex:
            Vb = [sbt(ex, f"Vb{i}", [128, 8, 1024], BF16) for i in range(2)]; bVb = [Buf(f"Vb{i}") for i in range(2)]
            Ag = [sbt(ex, f"Ag{i}", [128, 8, GT * 128], BF16) for i in range(2)]; bAg = [Buf(f"Ag{i}") for i in range(2)]
            NSP, NEB = 4, 3
            Sp = [sbt(ex, f"Sp{i}", [128, 8, 128]) for i in range(NSP)]; bSp = [Buf(f"Sp{i}") for i in range(NSP)]
            Eb = [sbt(ex, f"Eb{i}", [128, 8, 128], BF16) for i in range(NEB)]; bEb = [Buf(f"Eb{i}") for i in range(NEB)]
            Mb = [sbt(ex, f"Mb{i}", [128, 8, 128], BF16) for i in range(NEB)]; bMb = [Buf(f"Mb{i}") for i in range(NEB)]
            Wt = [sbt(ex, f"Wt{i}", [128, 8, 128], BF16) for i in range(2)]; bWt = [Buf(f"Wt{i}") for i in range(2)]
            NW = GT * 128
            NDVE = 8
            NBX = NB if KDBG != 'rt' else 0

            def load_V(bi):
                sl = bi % 2
                D_(lambda e: e.dma_start(out=Vb[sl][:], in_=V[bi * 1024:(bi + 1) * 1024, :].rearrange("(c p) n -> p c n", p=128)), w=[bVb[sl]], buf=bVb[sl], eng="gpsimd")

            def act_mm(bi, ch):
                sl = bi % 2
                pb = ch % 2
                for kc in range(8):
                    T_(lambda e, kc=kc: e.matmul(ps[pb][:, 0:NW], lhsT=Ub[sl][:, kc, ch * 128:(ch + 1) * 128], rhs=h2T[:, kc, :], start=(kc == 0), stop=(kc == 7)),
                       r=[bUb[sl], bh2T], w=[bps[pb]])

            def act_gelu(bi, ch):
                sl = bi % 2
                pb = ch % 2
                A_(lambda e: e.activation(out=Ag[sl][:, ch, :], in_=ps[pb][:, 0:NW], func=AF.Gelu_apprx_tanh), r=[bps[pb]], w=[bAg[sl]])

            def act_chunk(bi, ch):
                act_mm(bi, ch)
                act_gelu(bi, ch)

            spc = [0]
            ebc = [0]
            deferred = {}

            def unit(bi, tt, uidx):
                sl = bi % 2
                gpair = 2 + 2 * (uidx % 2)
                w3 = uidx % 2
                slots = {}

                def emit_sp(h):
                    s_ = spc[0] % NSP
                    spc[0] += 1
                    slots[h] = s_
                    eng = V_ if h < NDVE else G_
                    eng(lambda e: e.tensor_tensor(out=Sp[s_][:], in0=aa[:, tt, h, bi * 8:(bi + 1) * 8].unsqueeze(2).to_broadcast([128, 8, 128]),
                                                  in1=s2a[:, tt, h, :].unsqueeze(1).to_broadcast([128, 8, 128]), op=ALU.add),
                        r=[baa[tt], bs2a[tt]], w=[bSp[s_]])

                emit_sp(0)
                emit_sp(1)
                if 'wt' in deferred:
                    deferred.pop('wt')()
                emit_sp(2)
                emit_sp(3)
                cpu = 8 // GT
                for h in range(8):
                    s_ = slots[h]
                    w2 = ebc[0] % NEB
                    ebc[0] += 1
                    A_(lambda e, h=h, s_=s_, w2=w2: e.activation(out=Eb[w2][:], in_=Sp[s_][:], func=AF.Exp, bias=kap[:, tt, h:h + 1], scale=1.0),
                       r=[bSp[s_], bkap[tt]], w=[bEb[w2]])
                    V_(lambda e, h=h, s_=s_, w2=w2: e.scalar_tensor_tensor(out=Mb[w2][:], in0=Sp[s_][:], scalar=tau[:, tt, h:h + 1], in1=Eb[w2][:], op0=ALU.is_ge, op1=ALU.mult),
                       r=[bSp[s_], bEb[w2], bkap[tt]], w=[bMb[w2]])
                    for ch in range(8):
                        bank = gpair + ch // 4
                        T_(lambda e, ch=ch, bank=bank, h=h, w2=w2: e.matmul(ps[bank][:, (ch % 4) * 128:(ch % 4 + 1) * 128], lhsT=Mb[w2][:, ch, :], rhs=idb[:],
                                                                           start=(h == 0 and ch % 4 == 0), stop=(h == 7), skip_group_check=True), r=[bMb[w2], bidb], w=[bps[bank]])
                    if h + 4 < 8:
                        emit_sp(h + 4)
                    if h == 2 and 'acc' in deferred:
                        deferred.pop('acc')()
                    if bi + 1 < NBX:
                        pairs = max(1, cpu // 2)
                        step = 8 // pairs
                        for p_ in range(pairs):
                            h_mm = 1 if pairs == 1 else step * p_
                            h_ge = 6 if pairs == 1 else step * p_ + step - 1
                            c0 = tt * cpu + 2 * p_
                            if h == h_mm:
                                act_mm(bi + 1, c0)
                                act_mm(bi + 1, c0 + 1)
                            if h == h_ge:
                                act_gelu(bi + 1, c0)
                                act_gelu(bi + 1, c0 + 1)

                def do_wt():
                    for q in range(2):
                        V_(lambda e, q=q: e.tensor_tensor(out=Wt[w3][:, q * 4:(q + 1) * 4, :], in0=ps[gpair + q][:, :].rearrange("p (a c) -> p a c", a=4),
                                                          in1=Ag[sl][:, q * 4:(q + 1) * 4, tt * 128:(tt + 1) * 128], op=ALU.mult), r=[bps[gpair + q], bAg[sl]], w=[bWt[w3]])
                    for hf in range(2):
                        for ch in range(8):
                            T_(lambda e, hf=hf, ch=ch: e.matmul(ps[6 + hf][:, :], lhsT=Wt[w3][:, ch, :], rhs=Vb[sl][:, ch, hf * 512:(hf + 1) * 512], start=(ch == 0), stop=(ch == 7)),
                               r=[bWt[w3], bVb[sl]], w=[bps[6 + hf]])

                def do_acc():
                    for hf in range(2):
                        V_(lambda e, hf=hf: e.tensor_tensor(out=acc[:, tt, hf * 512:(hf + 1) * 512], in0=acc[:, tt, hf * 512:(hf + 1) * 512], in1=ps[6 + hf][:, :], op=ALU.add),
                           r=[bps[6 + hf], bacc[tt]], w=[bacc[tt]])

                deferred['wt'] = do_wt
                deferred['acc'] = do_acc

            if NBX > 0:
                load_V(0)
                for ch in range(8):
                    act_chunk(0, ch)
            uidx = 0
            for bi in range(NBX):
                for tt in range(GT):
                    unit(bi, tt, uidx)
                    uidx += 1
                    if tt == min(1, GT - 1) and bi + 1 < NBX:
                        load_V(bi + 1)
                    if tt == min(2, GT - 1) and bi + 2 < NBX:
                        load_U(bi + 2)
            if 'wt' in deferred:
                deferred.pop('wt')()
            if 'acc' in deferred:
                deferred.pop('acc')()
            for tt in range(GT):
                ti = grp * GT + tt
                import os as _os2
                if _os2.environ.get("KDBG", "") in ("ff", "rt"):
                    pass
                elif _os2.environ.get("KDBG", "") == "x1":
                    V_(lambda e, tt=tt: e.tensor_copy(out=acc[:, tt, :], in_=x1g[:, tt, :]), r=[bacc[tt], bx1g[tt]], w=[bacc[tt]])
                else:
                    V_(lambda e, tt=tt: e.tensor_tensor(out=acc[:, tt, :], in0=acc[:, tt, :], in1=GT2[:], op=ALU.mult), r=[bacc[tt], bGT2], w=[bacc[tt]])
                    V_(lambda e, tt=tt: e.tensor_tensor(out=acc[:, tt, :], in0=acc[:, tt, :], in1=x1g[:, tt, :], op=ALU.add), r=[bacc[tt], bx1g[tt]], w=[bacc[tt]])
                if _os2.environ.get('KDBG', '') not in ('ff', 'rt'):
                    A_(lambda e, tt=tt: e.activation(out=x1g[:, tt, :], in_=acc[:, tt, :], func=AF.Square, accum_out=pst[:, 4:5]), r=[bacc[tt]], w=[bx1g[tt], bpst])
                    A_(lambda e: e.activation(out=pst[:, 5:6], in_=pst[:, 4:5], func=AF.Sqrt, bias=epsc[:, 0:1], scale=1.0 / 1024), r=[bpst, bepsc], w=[bpst])
                    V_(lambda e: e.reciprocal(out=pst[:, 6:7], in_=pst[:, 5:6]), r=[bpst], w=[bpst])
                    V_(lambda e, tt=tt: e.scalar_tensor_tensor(out=acc[:, tt, :], in0=acc[:, tt, :], scalar=pst[:, 6:7], in1=FNW[:], op0=ALU.mult, op1=ALU.mult),
                       r=[bacc[tt], bpst, bFNW], w=[bacc[tt]])
                D_(lambda e, tt=tt, ti=ti: e.dma_start(out=y[ti * 128:(ti + 1) * 128, :], in_=acc[:, tt, :]), r=[bacc[tt]], w=[by], buf=by)
            P.barrier([bUb[0], bUb[1], bVb[0], bVb[1], bWt[0], bWt[1], by] + bAg + bSp + bEb + bMb + bacc + bx1g)
    P.op("sync", lambda e: e.nop(), r=[by])
    P.emit()
    pe.close()
    top.close()
    return nc


def prep_inputs(inp, n_cores, NT):
    f = lambda a: np.ascontiguousarray(np.asarray(a, dtype=np.float32))
    x = f(inp["x"])[0]
    S = x.shape[0]
    assert S == n_cores * NT * 128
    NPREV = (n_cores - 1) * NT
    w_in = f(inp["w_in"])[0]
    w_tok = np.ascontiguousarray(np.concatenate([w_in[:, 0:1024], w_in[:, 2560:3072]], axis=1))
    w_qkv = np.ascontiguousarray(w_in[:, 1024:2560])
    w_ab = np.zeros((1024, 64), np.float32)
    w_ab[:, 0:4] = w_in[:, 3072:3076]
    w_ab[:, 32:36] = w_in[:, 3076:3080]
    rep = lambda v, n=128: np.ascontiguousarray(np.broadcast_to(v[None, :], (n, v.shape[0])))
    gm_ws = f(inp["gm_ws"])[0]
    wsT = np.ascontiguousarray(gm_ws.transpose(2, 0, 1).reshape(128, 8 * 128))
    gm_bs = f(inp["gm_bs"])[0]
    bs_bc = np.ascontiguousarray(np.repeat(gm_bs.T[:, :, None], 64, axis=2).reshape(128, 512))
    conv_w = f(inp["conv_w"])[0]
    convw = np.ascontiguousarray(conv_w.reshape(4, 12, 128).transpose(2, 1, 0).reshape(128, 48))
    gpar = np.zeros((128, 8), np.float32)
    gpar[:, 0:4] = f(inp["a_log"])[0][None, :]
    gpar[:, 4:8] = f(inp["dt_bias"])[0][None, :]
    k1 = f(inp["peer_keys1"])[0]
    k2 = f(inp["peer_keys2"])[0]
    keysT = np.zeros((128, 16, 128), np.float32)
    for h in range(8):
        keysT[:, 2 * h, :] = k1[h].T
        keysT[:, 2 * h + 1, :] = k2[h].T
    keysT = keysT.reshape(128, 16 * 128)
    UT = np.ascontiguousarray(f(inp["peer_u"])[0].T)
    Vv = f(inp["peer_v"])[0]
    c = f(inp["c"])[0]
    shared = {
        "c_col": np.ascontiguousarray(c.reshape(8, 128).T),
        "w_ada": f(inp["w_ada"])[0], "b_ada": f(inp["b_ada"])[0][None, :],
        "n1w": rep(f(inp["norm1_w"])[0]), "n2w": rep(f(inp["norm2_w"])[0]), "fnw": rep(f(inp["final_norm_w"])),
        "gmnw": rep(f(inp["gm_norm_w"])[0]), "onw": rep(f(inp["o_norm_w"])[0]),
        "w_tok": w_tok, "w_qkv": w_qkv, "w_ab": w_ab, "wsT": wsT, "bs_bc": bs_bc, "convw": convw, "gpar": gpar,
        "w_out": f(inp["w_out"])[0], "wq": f(inp["peer_wq"])[0], "keysT": keysT, "UT": UT, "V": Vv,
        "consts": make_consts(),
    }
    maps = []
    for ci in range(n_cores):
        m = dict(shared)
        s0 = ci * NT * 128
        m["x_own"] = np.ascontiguousarray(x[s0:s0 + NT * 128])
        xp = np.zeros((max(NPREV, 1) * 128, 1024), np.float32)
        pm = np.zeros((128, max(NPREV, 1)), np.float32)
        if NPREV > 0:
            lo = s0 - NPREV * 128
            for t in range(NPREV):
                g0 = lo + t * 128
                if g0 >= 0:
                    xp[t * 128:(t + 1) * 128] = x[g0:g0 + 128]
                    pm[:, t] = 1.0
        m["x_prev"] = xp
        m["pmask"] = pm
        maps.append(m)
    return maps


_CACHE = {}


def kernel(**inputs):
    n_cores, NT = 8, 16
    maps = prep_inputs(inputs, n_cores, NT)
    key = (n_cores, NT)
    if key not in _CACHE:
        _CACHE[key] = build(n_cores, NT)
    nc = _CACHE[key]
    res = run_bass_kernel_spmd(nc, maps, core_ids=list(range(n_cores)))
    out = np.concatenate([np.asarray(r["y"], dtype=np.float32) for r in res.results], axis=0)
    return out[None, :, :].astype(np.float32)
```
